# Optimizing a Trainium2 kernel written in Bass

```python
import math
import jax, jax.numpy as jnp
from jax import lax
import numpy as np

D_MODEL = 2048
BATCH = 4
SEQ = 8192
DEPTH = 1

N_META = 16
LN_EPS = 1e-5
CONV_CH = D_MODEL
CONV_TAPS = 31
N_HEADS = 16
HEAD_DIM = D_MODEL // N_HEADS
ATTN_WIDTH = N_HEADS * HEAD_DIM
Q_BLOCK = 128
PEER_HEADS = 8
PEER_NKEYS = 128
PEER_EXPERTS = PEER_NKEYS * PEER_NKEYS
PEER_DQ = 256
PEER_DSUB = PEER_DQ // 2
PEER_TOPK = 16
PEER_CHUNK = 128
DN_ALPHA = (2.0 * DEPTH) ** 0.25
DN_BETA = (8.0 * DEPTH) ** -0.25

OFF_CONV_A = 0
OFF_CONV_G = OFF_CONV_A + CONV_CH
OFF_Q = OFF_CONV_G + CONV_CH
OFF_K = OFF_Q + ATTN_WIDTH
OFF_V = OFF_K + ATTN_WIDTH
OFF_F = OFF_V + ATTN_WIDTH
OFF_GATE_C = OFF_F + N_HEADS
OFF_GATE_A = OFF_GATE_C + D_MODEL
N_IN = OFF_GATE_A + D_MODEL

kernel_name = "hybrid_conformer_fox_peer_block"


def layer_norm(x, g, b):
    xf = x.astype(jnp.float32)
    mu = jnp.mean(xf, axis=-1, keepdims=True)
    var = jnp.mean(jnp.square(xf - mu), axis=-1, keepdims=True)
    return ((xf - mu) * lax.rsqrt(var + LN_EPS)).astype(x.dtype) * g + b


def conformer_conv(a, gate, dw_w, dw_b, ln_g, ln_b, w_o):
    h = a * jax.nn.sigmoid(gate)
    h = lax.conv_general_dilated(
        h, dw_w[:, None, :].astype(h.dtype), window_strides=(1,),
        padding=((CONV_TAPS - 1, 0),),
        dimension_numbers=('NWC', 'WIO', 'NWC'),
        feature_group_count=CONV_CH) + dw_b
    h = jax.nn.silu(layer_norm(h, ln_g, ln_b))
    return h @ w_o


def forgetting_attention(q, k, v, logf, w_o):
    B, L = q.shape[0], q.shape[1]
    scale = HEAD_DIM ** -0.5
    c = jnp.cumsum(logf, axis=1)
    c_k = jnp.transpose(c, (0, 2, 1))
    kpos = jnp.arange(L)

    def attend(q_blk, c_blk, qpos):
        s = jnp.einsum('bqhd,bkhd->bhqk', q_blk, k,
                       preferred_element_type=jnp.float32) * scale
        s = s + (jnp.transpose(c_blk, (0, 2, 1))[:, :, :, None] - c_k[:, :, None, :])
        s = jnp.where(kpos[None, None, None, :] <= qpos[None, None, :, None], s, -jnp.inf)
        p = jax.nn.softmax(s, axis=-1).astype(v.dtype)
        return jnp.einsum('bhqk,bkhd->bqhd', p, v)

    o_meta = attend(q[:, :N_META], c[:, :N_META], jnp.arange(N_META))
    n_real = L - N_META
    n_blk = n_real // Q_BLOCK
    qr = q[:, N_META:].reshape(B, n_blk, Q_BLOCK, N_HEADS, HEAD_DIM).transpose(1, 0, 2, 3, 4)
    cr = c[:, N_META:].reshape(B, n_blk, Q_BLOCK, N_HEADS).transpose(1, 0, 2, 3)
    pr = (N_META + jnp.arange(n_real)).reshape(n_blk, Q_BLOCK)
    o_r = lax.map(lambda a: attend(a[0], a[1], a[2]), (qr, cr, pr))
    o_r = o_r.transpose(1, 0, 2, 3, 4).reshape(B, n_real, N_HEADS, HEAD_DIM)
    o = jnp.concatenate([o_meta, o_r], axis=1).reshape(B, L, ATTN_WIDTH)
    return o @ w_o


def peer(x, w_q, subkeys, u_tab, v_tab):
    B, L, D = x.shape
    T = B * L
    xt = x.reshape(T, D)
    q = (xt @ w_q).reshape(T, PEER_HEADS, 2, PEER_DSUB)
    s = jnp.einsum('thpd,hpnd->thpn', q, subkeys, preferred_element_type=jnp.float32)
    sv, si = lax.top_k(s, PEER_TOPK)
    cand = sv[:, :, 0, :, None] + sv[:, :, 1, None, :]
    cand_id = si[:, :, 0, :, None] * PEER_NKEYS + si[:, :, 1, None, :]
    cand = cand.reshape(T, PEER_HEADS, PEER_TOPK * PEER_TOPK)
    cand_id = cand_id.reshape(T, PEER_HEADS, PEER_TOPK * PEER_TOPK)
    top_s, top_pos = lax.top_k(cand, PEER_TOPK)
    ids = jnp.take_along_axis(cand_id, top_pos, axis=-1)
    g = jax.nn.softmax(top_s, axis=-1).astype(x.dtype)

    n_chunk = -(-T // PEER_CHUNK)
    pad = n_chunk * PEER_CHUNK - T
    xc_all = jnp.pad(xt, ((0, pad), (0, 0))).reshape(n_chunk, PEER_CHUNK, D)
    ic_all = jnp.pad(ids, ((0, pad), (0, 0), (0, 0))).reshape(n_chunk, PEER_CHUNK, PEER_HEADS, PEER_TOPK)
    gc_all = jnp.pad(g, ((0, pad), (0, 0), (0, 0))).reshape(n_chunk, PEER_CHUNK, PEER_HEADS, PEER_TOPK)

    def expert_chunk(a):
        xc, ic, gc = a
        u = u_tab[ic]
        act = jax.nn.gelu(jnp.einsum('cd,chkd->chk', xc, u), approximate=False)
        vv = v_tab[ic]
        return jnp.einsum('chk,chkd->cd', gc * act, vv)

    out = lax.map(expert_chunk, (xc_all, ic_all, gc_all))
    return out.reshape(n_chunk * PEER_CHUNK, D)[:T].reshape(B, L, D)


def setup_inputs(seed: int = 0) -> dict:
    key = jax.random.key(seed)
    ks = jax.random.split(key, 24)
    f32 = jnp.float32
    nrm = lambda k, shp: jax.random.normal(k, shp, f32)
    x = nrm(ks[0], (BATCH, SEQ, D_MODEL))
    meta_tokens = nrm(ks[1], (N_META, D_MODEL))
    ln_in_g = 1.0 + 0.02 * nrm(ks[2], (D_MODEL,))
    ln_in_b = 0.02 * nrm(ks[3], (D_MODEL,))
    col_scale = np.ones((N_IN,), np.float32)
    col_scale[OFF_V:OFF_F] = DN_BETA
    w_in = nrm(ks[4], (DEPTH, D_MODEL, N_IN)) * (D_MODEL ** -0.5) * jnp.asarray(col_scale)
    b_in = 0.02 * nrm(ks[5], (DEPTH, N_IN))
    b_in = b_in.at[:, OFF_F:OFF_GATE_C].set(
        jax.random.uniform(ks[6], (DEPTH, N_HEADS), f32, minval=1.0, maxval=6.0))
    conv_dw_w = nrm(ks[7], (DEPTH, CONV_TAPS, CONV_CH)) * (CONV_TAPS ** -0.5)
    conv_dw_b = 0.02 * nrm(ks[8], (DEPTH, CONV_CH))
    conv_ln_g = 1.0 + 0.02 * nrm(ks[9], (DEPTH, CONV_CH))
    conv_ln_b = 0.02 * nrm(ks[10], (DEPTH, CONV_CH))
    w_conv_out = nrm(ks[11], (DEPTH, CONV_CH, D_MODEL)) * (CONV_CH ** -0.5) * DN_BETA
    w_attn_out = nrm(ks[12], (DEPTH, ATTN_WIDTH, D_MODEL)) * (ATTN_WIDTH ** -0.5) * DN_BETA
    w_out = nrm(ks[13], (DEPTH, D_MODEL, D_MODEL)) * (D_MODEL ** -0.5) * DN_BETA
    ln1_g = 1.0 + 0.02 * nrm(ks[14], (DEPTH, D_MODEL))
    ln1_b = 0.02 * nrm(ks[15], (DEPTH, D_MODEL))
    peer_w_q = nrm(ks[16], (DEPTH, D_MODEL, PEER_HEADS * PEER_DQ)) * (D_MODEL ** -0.5)
    peer_subkeys = nrm(ks[17], (DEPTH, PEER_HEADS, 2, PEER_NKEYS, PEER_DSUB)) * (PEER_DSUB ** -0.5)
    peer_u = nrm(ks[18], (DEPTH, PEER_EXPERTS, D_MODEL)) * (D_MODEL ** -0.5) * DN_BETA
    peer_v = nrm(ks[19], (DEPTH, PEER_EXPERTS, D_MODEL)) * DN_BETA
    ln2_g = 1.0 + 0.02 * nrm(ks[20], (DEPTH, D_MODEL))
    ln2_b = 0.02 * nrm(ks[21], (DEPTH, D_MODEL))
    return {"x": x, "meta_tokens": meta_tokens, "ln_in_g": ln_in_g, "ln_in_b": ln_in_b,
            "w_in": w_in, "b_in": b_in, "conv_dw_w": conv_dw_w, "conv_dw_b": conv_dw_b,
            "conv_ln_g": conv_ln_g, "conv_ln_b": conv_ln_b, "w_conv_out": w_conv_out,
            "w_attn_out": w_attn_out, "w_out": w_out, "ln1_g": ln1_g, "ln1_b": ln1_b,
            "peer_w_q": peer_w_q, "peer_subkeys": peer_subkeys, "peer_u": peer_u,
            "peer_v": peer_v, "ln2_g": ln2_g, "ln2_b": ln2_b}


def reference(x, meta_tokens, ln_in_g, ln_in_b, w_in, b_in, conv_dw_w, conv_dw_b,
              conv_ln_g, conv_ln_b, w_conv_out, w_attn_out, w_out, ln1_g, ln1_b,
              peer_w_q, peer_subkeys, peer_u, peer_v, ln2_g, ln2_b):
    B = x.shape[0]
    meta = jnp.broadcast_to(meta_tokens[None].astype(x.dtype), (B, N_META, D_MODEL))
    h = jnp.concatenate([meta, x], axis=1)
    h = layer_norm(h, ln_in_g, ln_in_b)
    L = h.shape[1]
    for l in range(DEPTH):
        z = h @ w_in[l] + b_in[l]
        y_conv = conformer_conv(z[..., OFF_CONV_A:OFF_CONV_G], z[..., OFF_CONV_G:OFF_Q],
                                conv_dw_w[l], conv_dw_b[l], conv_ln_g[l], conv_ln_b[l],
                                w_conv_out[l])
        q = z[..., OFF_Q:OFF_K].reshape(B, L, N_HEADS, HEAD_DIM)
        k = z[..., OFF_K:OFF_V].reshape(B, L, N_HEADS, HEAD_DIM)
        v = z[..., OFF_V:OFF_F].reshape(B, L, N_HEADS, HEAD_DIM)
        logf = jax.nn.log_sigmoid(z[..., OFF_F:OFF_GATE_C].astype(jnp.float32))
        y_attn = forgetting_attention(q, k, v, logf, w_attn_out[l])
        g_c = jax.nn.sigmoid(z[..., OFF_GATE_C:OFF_GATE_A])
        g_a = jax.nn.sigmoid(z[..., OFF_GATE_A:N_IN])
        mix = (g_c * y_conv + g_a * y_attn) @ w_out[l]
        h = layer_norm(DN_ALPHA * h + mix, ln1_g[l], ln1_b[l])
        ffn = peer(h, peer_w_q[l], peer_subkeys[l], peer_u[l], peer_v[l])
        h = layer_norm(DN_ALPHA * h + ffn, ln2_g[l], ln2_b[l])
    return h[:, N_META:]
```

```python
import contextlib
import numpy as np
import concourse.bass as bass
import concourse.mybir as mybir
from concourse.bass_utils import run_bass_kernel_spmd

F32 = mybir.dt.float32
BF16 = mybir.dt.bfloat16
U32 = mybir.dt.uint32
I32 = mybir.dt.int32
ALU = mybir.AluOpType
AF = mybir.ActivationFunctionType
AX = mybir.AxisListType

D = 2048
KC = 16
NH = 16
HD = 128
N_META = 16
TAPS = 31
N_IN = 14352
OFF_A, OFF_G, OFF_Q, OFF_K, OFF_V, OFF_F, OFF_GC, OFF_GA = 0, 2048, 4096, 6144, 8192, 10240, 10256, 12304
LN_EPS = 1e-5
DN_ALPHA = 2.0 ** 0.25
NEXP = 16384
NEG = -30000.0


class Buf:
    __slots__ = ("name", "w", "r", "sem")

    def __init__(self, name):
        self.name = name
        self.w = {}
        self.r = {}
        self.sem = None


class Sched:
    def __init__(self, nc, stack):
        self.nc = nc
        self.stack = stack
        self.names = ["pe", "act", "dve", "pool", "sp"]
        self.eng = {"pe": nc.tensor, "act": nc.scalar, "dve": nc.vector, "pool": nc.gpsimd, "sp": nc.sync}
        self.esem = {e: stack.enter_context(nc.semaphore("s_" + e)) for e in self.names}
        self.ecount = {e: 0 for e in self.names}
        self.waited = {e: {} for e in self.names}
        self.semcount = {}
        self.nsem = 0
        self.ninstr = 0

    def _wait(self, eng, deps):
        wd = self.waited[eng]
        for sem, val in deps.items():
            if eng == "pe" and sem is self.esem["pe"]:
                continue
            if wd.get(sem, 0) >= val:
                continue
            wd[sem] = val
            self.eng[eng].wait_ge(sem, val)

    @staticmethod
    def _merge(dst, src):
        for s, v in src.items():
            if dst.get(s, 0) < v:
                dst[s] = v

    def op(self, eng, fn, reads=(), writes=()):
        deps = {}
        for b in reads:
            self._merge(deps, b.w)
        for b in writes:
            self._merge(deps, b.w)
            self._merge(deps, b.r)
        self._wait(eng, deps)
        self.ecount[eng] += 1
        self.ninstr += 1
        sem = self.esem[eng]
        tok = {sem: self.ecount[eng]}
        fn(self.eng[eng]).then_inc(sem, 1)
        for b in reads:
            self._merge(b.r, tok)
        for b in writes:
            b.w = dict(tok)
            b.r = {}

    def _slot_sem(self, b):
        if b.sem is None:
            b.sem = self.stack.enter_context(self.nc.semaphore("d%d" % self.nsem))
            self.nsem += 1
            self.semcount[b.sem] = 0
        return b.sem

    def dma(self, q, fn, src, dst, slot, extra_reads=(), append=False):
        deps = {}
        self._merge(deps, src.w)
        for b in extra_reads:
            self._merge(deps, b.w)
        if not append:
            self._merge(deps, dst.w)
            self._merge(deps, dst.r)
        self._wait(q, deps)
        sem = self._slot_sem(slot)
        self.semcount[sem] += 16
        self.ninstr += 1
        tok = {sem: self.semcount[sem]}
        fn(self.eng[q]).then_inc(sem, 16)
        self._merge(src.r, tok)
        for b in extra_reads:
            self._merge(b.r, tok)
        if append:
            self._merge(dst.w, tok)
        else:
            dst.w = dict(tok)
            dst.r = {}

    def final_wait(self, eng, bufs):
        deps = {}
        for b in bufs:
            self._merge(deps, b.w)
        self._wait(eng, deps)

    def barrier(self):
        deps = {self.esem[e]: self.ecount[e] for e in self.names if self.ecount[e] > 0}
        for sem, v in self.semcount.items():
            if v > 0:
                deps[sem] = v
        for e in self.names:
            self._wait(e, dict(deps))


class Ring:
    def __init__(self, tiles, name):
        self.tiles = tiles
        self.bufs = [Buf("%s%d" % (name, i)) for i in range(len(tiles))]
        self.i = -1

    def next(self):
        self.i = (self.i + 1) % len(self.tiles)
        return self.tiles[self.i], self.bufs[self.i]


def build(SEQ, dbg=False, stop_after="F"):
    AT = SEQ // 128
    NB = AT // 2
    NG = AT // 4
    NTOK = AT * 128
    NOWN = NB * 128
    LK = N_META + NTOK
    NOG = NOWN // 512
    assert AT % 8 == 0
    PH = "0 AB B2 C D E1 E2 F".split()
    nph = PH.index(stop_after)

    nc = bass.Bass("TRN2", target_bir_lowering=False)
    stack = contextlib.ExitStack()

    def din(name, shape, dt=F32):
        return nc.dram_tensor(name, list(shape), dt, kind="ExternalInput").ap()

    def dscr(name, shape, dt):
        return nc.dram_tensor(name, list(shape), dt,
                              kind="ExternalOutput" if dbg else "Internal").ap()

    def dint(name, shape, dt):
        return nc.dram_tensor(name, list(shape), dt, kind="Internal").ap()

    xa = din("xa", [NTOK, D])
    xmeta = din("xmeta", [N_META, D])
    valid16 = din("valid16", [16, NTOK])
    kb0 = din("kb0", [128, 1])
    hmask = din("hmask", [128, 32])
    lnin_g = din("lnin_g", [128, D])
    lnin_b = din("lnin_b", [128, D])
    w_in = din("w_in", [D, N_IN])
    b_fm = din("b_fm", [128, 113])
    bv_b = din("bv_b", [128, D])
    dww = din("dww", [128, KC, TAPS])
    dwb = din("dwb", [128, KC])
    cln_g = din("cln_g", [128, KC])
    cln_b = din("cln_b", [128, KC])
    w_co = din("w_co", [D, D])
    w_ao = din("w_ao", [D, D])
    w_o = din("w_o", [D, D])
    ln1_g = din("ln1_g", [128, D])
    ln1_b = din("ln1_b", [128, D])
    w_q = din("w_q", [D, D])
    subk = din("subk", [16, 128, 128])
    tab_u = din("tab_u", [NEXP, D])
    tab_v = din("tab_v", [NEXP, D])
    ln2_g = din("ln2_g", [128, D])
    ln2_b = din("ln2_b", [128, D])
    ident_in = din("ident", [128, 128])
    tri_in = din("tri", [128, 128])
    iota16_in = din("iota16", [128, 16])
    out = nc.dram_tensor("out", [NOWN, D], F32, kind="ExternalOutput").ap()

    win_bf = dint("win_bf", [KC, 128, N_IN], BF16)
    wco_bf = dint("wco_bf", [KC, 128, D], BF16)
    wao_bf = dint("wao_bf", [KC, 128, D], BF16)
    wo_bf = dint("wo_bf", [KC, 128, D], BF16)
    wq_bf = dint("wq_bf", [KC, 128, D], BF16)
    kT_d = dscr("kT_d", [NH, 128, LK], BF16)
    v_d = dscr("v_d", [LK, D], BF16)
    qT_d = dscr("qT_d", [NH, 128, NOWN], BF16)
    uT_d = dscr("uT_d", [KC, 128, NB, 160], BF16)
    gcT_d = dscr("gcT_d", [KC, 128, NOWN], BF16)
    gaT_d = dscr("gaT_d", [KC, 128, NOWN], BF16)
    logf_d = dscr("logf_d", [16, LK], F32)
    cT_d = dscr("cT_d", [16, LK], F32)
    ycg_d = dscr("ycg_d", [KC, 128, NOWN], BF16)
    o_d = dscr("o_d", [NOWN, D], BF16)
    h0_d = dscr("h0_d", [NOWN, D], F32)
    h1_d = dscr("h1_d", [NOWN, D], F32)
    s_d = dscr("s_d", [NOWN, D], F32)
    uv_bf = dint("uv_bf", [NEXP, 2 * D], BF16)

    S = Sched(nc, stack)
    B_ext = Buf("ext")

    def sb(name, shape, dt, st=None):
        return (st or stack).enter_context(nc.sbuf_tensor("t_" + name, list(shape), dt))

    def ps(name, shape, dt=F32):
        return stack.enter_context(nc.psum_tensor(name, list(shape), dt))

    P = {}
    pscount = [0]

    def mkpsum(st, nf, no, nb):
        assert nf + no + nb <= 8
        pscount[0] += 1
        tag = "p%d_" % pscount[0]
        P["f"] = Ring([st.enter_context(nc.psum_tensor(tag + "f%d" % i, [128, 512], F32)) for i in range(nf)], "psf")
        P["o"] = Ring([st.enter_context(nc.psum_tensor(tag + "o%d" % i, [128, 512], F32)) for i in range(no)], "pso")
        P["b"] = Ring([st.enter_context(nc.psum_tensor(tag + "b%d" % i, [128, 1024], BF16)) for i in range(nb)], "psb")

    B_c = Buf("consts")
    ident_f = sb("ident_f", [128, 128], F32)
    ident_b = sb("ident_b", [128, 128], BF16)
    tri_b = sb("tri_b", [128, 128], BF16)
    tri_f = sb("tri_f", [128, 128], F32)
    iota16 = sb("iota16", [128, 16], F32)
    thr16 = sb("thr16", [128, 16], F32)
    bfm = sb("bfm", [128, 113], F32)
    nbf = sb("nbf", [16, 1], F32)
    kb0_t = sb("kb0_t", [128, 1], F32)
    hmask_t = sb("hmask_t", [128, 32], F32)
    eps_t = sb("eps_t", [128, 1], F32)
    one_t = sb("one_t", [128, 1], F32)
    onesm = sb("onesm", [128, 128], BF16)
    dww_t = sb("dww_t", [128, KC, TAPS], F32)
    dwb_t = sb("dwb_t", [128, KC], F32)
    clng_t = sb("clng_t", [128, KC], F32)
    clnb_t = sb("clnb_t", [128, KC], F32)
    negck = sb("negck", [128, AT, NH], F32)
    negckm = sb("negckm", [16, NH], F32)
    st_r = Ring([sb("lnst%d" % i, [128, 4, 6], F32) for i in range(2)], "lnst")
    mv_r = Ring([sb("lnmv%d" % i, [128, 4], F32) for i in range(2)], "lnmv")

    def cload(dst, src):
        S.dma("sp", lambda e: e.dma_start(out=dst, in_=src), B_ext, B_c, B_c)

    cload(ident_f[:], ident_in)
    cload(tri_f[:], tri_in)
    cload(iota16[:], iota16_in)
    cload(bfm[:], b_fm)
    cload(kb0_t[:], kb0)
    cload(hmask_t[:], hmask)
    cload(dww_t[:], dww)
    cload(dwb_t[:], dwb)
    cload(clng_t[:], cln_g)
    cload(clnb_t[:], cln_b)
    B_c2 = Buf("consts2")
    S.op("dve", lambda e: e.tensor_copy(out=ident_b[:], in_=ident_f[:]), [B_c], [B_c2])
    S.op("dve", lambda e: e.tensor_copy(out=tri_b[:], in_=tri_f[:]), [B_c], [B_c2])
    S.op("dve", lambda e: e.memset(eps_t[:], LN_EPS), [], [B_c2])
    S.op("dve", lambda e: e.memset(one_t[:], 1.0), [], [B_c2])
    S.op("dve", lambda e: e.memset(onesm[:], 1.0 / D), [], [B_c2])
    S.op("dve", lambda e: e.tensor_scalar(out=nbf[:], in0=bfm[0:16, 112:113], scalar1=-1.0, scalar2=None,
                                          op0=ALU.mult), [B_c], [B_c2])
    S.op("dve", lambda e: e.tensor_scalar(out=thr16[:], in0=iota16[:], scalar1=16.0, scalar2=16.0,
                                          op0=ALU.mult, op1=ALU.add), [B_c], [B_c2])
    CONST = [B_c, B_c2]

    (B_win, B_wco, B_wao, B_wo, B_wq, B_kT, B_v, B_qT, B_uT, B_gc, B_ga, B_lf, B_cT, B_h0,
     B_ycg, B_oT, B_h1, B_s, B_out, B_nck, B_uv) = (Buf(n) for n in (
         "win wco wao wo wq kT v qT uT gc ga lf cT h0 ycg oT h1 s out nck uv").split())

    def layer_norm_tile(xt, bx, rows, g_t, b_t, gb_bufs, out_f32, b_out):
        st, bst = st_r.next()
        mv, bmv = mv_r.next()
        for j in range(4):
            S.op("dve", lambda e, j=j: e.bn_stats(out=st[:rows, j, :], in_=xt[:rows, j * 512:(j + 1) * 512]),
                 [bx], [bst])
        S.op("dve", lambda e: e.bn_aggr(out=mv[:rows, 0:2], in_=st[:rows].rearrange("p a b -> p (a b)")),
             [bst], [bmv])
        S.op("act", lambda e: e.activation(out=mv[:rows, 2:3], in_=mv[:rows, 1:2], func=AF.Sqrt,
                                           bias=eps_t[:rows, :], scale=1.0), [bmv] + CONST, [bmv])
        S.op("dve", lambda e: e.reciprocal(out=mv[:rows, 3:4], in_=mv[:rows, 2:3]), [bmv], [bmv])
        S.op("dve", lambda e: e.tensor_scalar(out=out_f32[:rows], in0=xt[:rows], scalar1=mv[:rows, 0:1],
                                              scalar2=mv[:rows, 3:4], op0=ALU.subtract, op1=ALU.mult),
             [bx, bmv], [b_out])
        S.op("pool", lambda e: e.tensor_tensor(out=out_f32[:rows], in0=out_f32[:rows], in1=g_t[:rows],
                                               op=ALU.mult), [b_out] + gb_bufs, [b_out])
        S.op("dve", lambda e: e.tensor_tensor(out=out_f32[:rows], in0=out_f32[:rows], in1=b_t[:rows],
                                              op=ALU.add), [b_out] + gb_bufs, [b_out])

    def transpose_to(xb, bxb, rows, dstT, bdst, col0):
        for half in range(2):
            pt, bpt = P["b"].next()
            for j in range(8):
                kc = half * 8 + j
                S.op("pe", lambda e, kc=kc, j=j, pt=pt: e.transpose(
                    out=pt[:, j * 128:j * 128 + rows], in_=xb[:rows, kc * 128:(kc + 1) * 128],
                    identity=ident_b[:rows, :rows]), [bxb] + CONST, [bpt])
            S.op("act", lambda e, half=half, pt=pt: e.activation(
                out=dstT[:, half * 8:(half + 1) * 8, col0:col0 + rows],
                in_=pt[:].rearrange("p (j t) -> p j t", t=128)[:, :, 0:rows], func=AF.Copy),
                [bpt], [bdst])

    def mm_acc(pt_ap, bpt, w, bw, wc, rhsT, brhs, c0, n):
        for kc in range(KC):
            S.op("pe", lambda e, kc=kc: e.matmul(
                pt_ap, lhsT=w[:, kc, wc * 128:(wc + 1) * 128], rhs=rhsT[:, kc, c0:c0 + n],
                start=(kc == 0), stop=(kc == KC - 1)), [bw, brhs], [bpt])

    def mm_own(pt, bpt, w, bw, wc, rhsT, brhs, c0, n):
        for kc in range(KC):
            S.op("pe", lambda e: e.matmul(
                pt[:, 0:2 * n].rearrange("p (a b) -> p a b", b=n), lhsT=w[:, kc, wc * 128:(wc + 1) * 128],
                rhs=rhsT[:, kc, :].rearrange("p (a b) -> p a b", b=256)[:, :, c0:c0 + n],
                start=(kc == 0), stop=(kc == KC - 1)), [bw, brhs], [bpt])

    def load_w(w_r, src_bf, bsrc, col0, ncols=512):
        w, bw = w_r.next()
        S.dma("sp", lambda e: e.dma_start(
            out=w[:, :, 0:ncols], in_=src_bf[:, :, col0:col0 + ncols].rearrange("k p c -> p k c")),
            bsrc, bw, bw)
        return w, bw

    with contextlib.ExitStack() as ph:
        stage_r = Ring([sb("cst%d" % i, [128, 2048], BF16, ph) for i in range(3)], "cst")

        def precast(src, dst, ncols, dbuf):
            for kc in range(KC):
                for c0 in range(0, ncols, 2048):
                    cw = min(2048, ncols - c0)
                    t, b = stage_r.next()
                    S.dma("pool", lambda e: e.dma_start(
                        out=t[:, 0:cw], in_=src[kc * 128:(kc + 1) * 128, c0:c0 + cw]), B_ext, b, b)
                    S.dma("sp", lambda e: e.dma_start(
                        out=dst[kc, :, c0:c0 + cw], in_=t[:, 0:cw]), b, dbuf, b, append=True)

        precast(w_in, win_bf, N_IN, B_win)
        precast(w_co, wco_bf, D, B_wco)
        if nph < 4:
            precast(w_ao, wao_bf, D, B_wao)
            precast(w_o, wo_bf, D, B_wo)
            precast(w_q, wq_bf, D, B_wq)
        S.barrier()

    if nph >= 1:
      with contextlib.ExitStack() as ph:
        lng_t = sb("lng_t", [128, D], F32, ph)
        mkpsum(ph, 5, 0, 2)
        lnb_t = sb("lnb_t", [128, D], F32, ph)
        bvb_t = sb("bvb_t", [128, D], F32, ph)
        B_ln = Buf("lnconst")
        for dst_, src_ in ((lng_t, lnin_g), (lnb_t, lnin_b), (bvb_t, bv_b)):
            S.dma("sp", lambda e: e.dma_start(out=dst_[:], in_=src_), B_ext, B_ln, B_ln)
        x_r = Ring([sb("xt%d" % i, [128, D], F32, ph) for i in range(2)], "xt")
        xn_r = Ring([sb("xn%d" % i, [128, D], F32, ph) for i in range(2)], "xn")
        xb_r = Ring([sb("xb%d" % i, [128, D], BF16, ph) for i in range(2)], "xb")
        h0T_r = Ring([sb("h0T%d" % i, [128, KC, 512], BF16, ph) for i in range(2)], "h0T")
        w_r = Ring([sb("wst%d" % i, [128, KC, 512], BF16, ph) for i in range(2)], "wst")
        wf_t = sb("wf_t", [128, KC, 16], BF16, ph)
        B_wf = Buf("wf")
        S.dma("sp", lambda e: e.dma_start(out=wf_t[:], in_=win_bf[:, :, OFF_F:OFF_F + 16].rearrange("k p c -> p k c")),
              B_win, B_wf, B_wf)
        sig_r = Ring([sb("sig%d" % i, [128, 4, 320], F32, ph) for i in range(2)], "sig")
        ev_r = Ring([sb("ev%d" % i, [128, 4, 512], BF16, ph) for i in range(3)], "ev")
        lf_r = Ring([sb("lft%d" % i, [16, 512], F32, ph) for i in range(2)], "lft")

        def group_proj(h0T, bh, gi, ntok, meta):
            ktok0 = 0 if meta else N_META + gi * 512
            own = [] if meta else [(2 * gi, 1), (2 * gi + 1, 3)]
            if not meta:
                for j in range(4):
                    wg, bwg = load_w(w_r, win_bf, B_win, OFF_G + j * 512)
                    sg, bsg = sig_r.next()
                    for c in range(4):
                        pt, bpt = P["f"].next()
                        mm_own(pt, bpt, wg, bwg, c, h0T, bh, 96, 160)
                        bc = 16 + j * 4 + c
                        S.op("act", lambda e: e.activation(
                            out=sg[:, c, :], in_=pt[:, 0:320], func=AF.Sigmoid,
                            bias=bfm[:, bc:bc + 1], scale=1.0), [bpt] + CONST, [bsg])
                    wa, bwa = load_w(w_r, win_bf, B_win, OFF_A + j * 512)
                    ev, bev = ev_r.next()
                    for c in range(4):
                        pt, bpt = P["f"].next()
                        mm_own(pt, bpt, wa, bwa, c, h0T, bh, 96, 160)
                        bc = j * 4 + c
                        S.op("dve", lambda e: e.scalar_tensor_tensor(
                            out=ev[:, c, 0:320], in0=pt[:, 0:320], scalar=bfm[:, bc:bc + 1],
                            in1=sg[:, c, :], op0=ALU.add, op1=ALU.mult), [bpt, bsg] + CONST, [bev])
                    if gi == 0:
                        S.op("pool", lambda e: e.tensor_tensor(
                            out=ev[:, :, 0:32], in0=ev[:, :, 0:32],
                            in1=hmask_t[:].unsqueeze(1).to_broadcast([128, 4, 32]), op=ALU.mult),
                            [bev] + CONST, [bev])
                    for (ob, _), o in zip(own, (0, 160)):
                        S.dma("pool", lambda e: e.dma_start(
                            out=uT_d[j * 4:(j + 1) * 4, :, ob, :].rearrange("c p t -> p c t"),
                            in_=ev[:, :, o:o + 160]), bev, B_uT, bev, append=True)
                for (off, dst, bdst, func, bcol0) in ((OFF_Q, qT_d, B_qT, AF.Identity, 32),
                                                       (OFF_GC, gcT_d, B_gc, AF.Sigmoid, 80),
                                                       (OFF_GA, gaT_d, B_ga, AF.Sigmoid, 96)):
                    for j in range(4):
                        w, bw = load_w(w_r, win_bf, B_win, off + j * 512)
                        ev, bev = ev_r.next()
                        for c in range(4):
                            pt, bpt = P["f"].next()
                            mm_own(pt, bpt, w, bw, c, h0T, bh, 128, 128)
                            bc = bcol0 + j * 4 + c
                            S.op("act", lambda e: e.activation(
                                out=ev[:, c, 0:256], in_=pt[:, 0:256], func=func,
                                bias=bfm[:, bc:bc + 1], scale=1.0), [bpt] + CONST, [bev])
                        S.dma("pool", lambda e: e.dma_start(
                            out=dst[j * 4:(j + 1) * 4, :, gi * 256:(gi + 1) * 256].rearrange("c p t -> p c t"),
                            in_=ev[:, :, 0:256]), bev, bdst, bev, append=True)
            for j in range(4):
                w, bw = load_w(w_r, win_bf, B_win, OFF_K + j * 512)
                ev, bev = ev_r.next()
                for c in range(4):
                    pt, bpt = P["f"].next()
                    mm_acc(pt[:, 0:ntok], bpt, w, bw, c, h0T, bh, 0, ntok)
                    bc = 48 + j * 4 + c
                    S.op("dve", lambda e: e.tensor_scalar(
                        out=ev[:, c, 0:ntok], in0=pt[:, 0:ntok], scalar1=bfm[:, bc:bc + 1], scalar2=None,
                        op0=ALU.add), [bpt] + CONST, [bev])
                S.dma("pool", lambda e: e.dma_start(
                    out=kT_d[j * 4:(j + 1) * 4, :, ktok0:ktok0 + ntok].rearrange("c p t -> p c t"),
                    in_=ev[:, :, 0:ntok]), bev, B_kT, bev, append=True)
            ntile = 1 if meta else 4
            rows = ntok if meta else 128
            for j in range(4):
                w, bw = load_w(w_r, win_bf, B_win, OFF_V + j * 512)
                ev, bev = ev_r.next()
                for t in range(ntile):
                    pt, bpt = P["f"].next()
                    for kc in range(KC):
                        S.op("pe", lambda e: e.matmul(
                            pt[:rows, :], lhsT=h0T[:, kc, t * 128:t * 128 + rows], rhs=w[:, kc, :],
                            start=(kc == 0), stop=(kc == KC - 1)), [bw, bh], [bpt])
                    S.op("dve", lambda e: e.tensor_tensor(
                        out=ev[:rows, t, :], in0=pt[:rows, :], in1=bvb_t[:rows, j * 512:(j + 1) * 512],
                        op=ALU.add), [bpt, B_ln], [bev])
                if meta:
                    S.dma("pool", lambda e: e.dma_start(
                        out=v_d[0:rows, j * 512:(j + 1) * 512], in_=ev[:rows, 0, :]), bev, B_v, bev, append=True)
                else:
                    S.dma("pool", lambda e: e.dma_start(
                        out=v_d[ktok0:ktok0 + 512, j * 512:(j + 1) * 512].rearrange("(t p) c -> p t c", p=128),
                        in_=ev[:, :, :]), bev, B_v, bev, append=True)
            pt, bpt = P["f"].next()
            for kc in range(KC):
                S.op("pe", lambda e: e.matmul(
                    pt[:16, 0:ntok], lhsT=wf_t[:, kc, :], rhs=h0T[:, kc, 0:ntok],
                    start=(kc == 0), stop=(kc == KC - 1)), [B_wf, bh], [bpt])
            lt, blt = lf_r.next()
            S.op("act", lambda e: e.activation(out=lt[:, 0:ntok], in_=pt[:16, 0:ntok], func=AF.Exp,
                                               bias=nbf[:, :], scale=-1.0), [bpt] + CONST, [blt])
            S.op("act", lambda e: e.activation(out=lt[:, 0:ntok], in_=lt[:, 0:ntok], func=AF.Ln,
                                               bias=one_t[:16, :], scale=1.0), [blt] + CONST, [blt])
            S.op("dve", lambda e: e.tensor_scalar(out=lt[:, 0:ntok], in0=lt[:, 0:ntok],
                                                  scalar1=-1.0, scalar2=None, op0=ALU.mult), [blt], [blt])
            S.dma("pool", lambda e: e.dma_start(out=logf_d[:, ktok0:ktok0 + ntok], in_=lt[:, 0:ntok]),
                  blt, B_lf, blt, append=True)

        def ln_in_tile(src_ap, rows, h0T, bh, col0, own_block):
            xt, bx = x_r.next()
            S.dma("sp", lambda e: e.dma_start(out=xt[:rows], in_=src_ap), B_ext, bx, bx)
            xn, bxn = xn_r.next()
            layer_norm_tile(xt, bx, rows, lng_t, lnb_t, [B_ln], xn, bxn)
            if own_block is not None:
                S.dma("pool", lambda e: e.dma_start(out=h0_d[own_block * 128:(own_block + 1) * 128, :],
                                                    in_=xn[:]), bxn, B_h0, bxn, append=True)
            xb, bxb = xb_r.next()
            S.op("act", lambda e: e.activation(out=xb[:rows], in_=xn[:rows], func=AF.Copy), [bxn], [bxb])
            transpose_to(xb, bxb, rows, h0T, bh, col0)

        h0T, bh = h0T_r.next()
        ln_in_tile(xmeta, N_META, h0T, bh, 0, None)
        group_proj(h0T, bh, 0, N_META, True)
        for gi in range(NG):
            h0T, bh = h0T_r.next()
            for t in range(4):
                at = gi * 4 + t
                ln_in_tile(xa[at * 128:(at + 1) * 128, :], 128, h0T, bh, t * 128,
                           (at // 2) if (at % 2 == 1) else None)
            group_proj(h0T, bh, gi, 512, False)
        S.barrier()

    if nph >= 2:
      with contextlib.ExitStack() as ph:
        CH = 2048
        mkpsum(ph, 4, 0, 0)
        zeros16 = sb("zeros16", [16, CH], F32, ph)
        B_z16 = Buf("z16")
        S.op("pool", lambda e: e.memset(zeros16[:], 0.0), [], [B_z16])
        lf_r2 = Ring([sb("lfc%d" % i, [16, CH], F32, ph) for i in range(2)], "lfc")
        val_r = Ring([sb("val%d" % i, [16, CH], F32, ph) for i in range(2)], "val")
        ct_r = Ring([sb("ctc%d" % i, [16, CH], F32, ph) for i in range(2)], "ctc")
        lfm, blfm = lf_r2.next()
        S.dma("sp", lambda e: e.dma_start(out=lfm[:, 0:N_META], in_=logf_d[:, 0:N_META]), B_lf, blfm, blfm)
        ctp, bctp = ct_r.next()
        S.op("dve", lambda e: e.tensor_tensor_scan(out=ctp[:, 0:N_META], data0=lfm[:, 0:N_META],
                                                   data1=zeros16[:, 0:N_META], initial=0.0,
                                                   op0=ALU.add, op1=ALU.add), [blfm, B_z16], [bctp])
        S.dma("pool", lambda e: e.dma_start(out=cT_d[:, 0:N_META], in_=ctp[:, 0:N_META]), bctp, B_cT, bctp,
              append=True)
        pt, bpt = P["f"].next()
        S.op("pe", lambda e: e.matmul(pt[:16, 0:16], lhsT=ctp[:, 0:N_META], rhs=ident_f[0:16, 0:16],
                                      start=True, stop=True), [bctp] + CONST, [bpt])
        S.op("dve", lambda e: e.tensor_scalar(out=negckm[:, :], in0=pt[:16, 0:16], scalar1=-1.0,
                                              scalar2=None, op0=ALU.mult), [bpt], [B_nck])
        prev_last = ctp[:, N_META - 1:N_META]
        bprev = bctp
        for c0 in range(0, NTOK, CH):
            cw = min(CH, NTOK - c0)
            a0 = N_META + c0
            lf, blf = lf_r2.next()
            S.dma("sp", lambda e: e.dma_start(out=lf[:, 0:cw], in_=logf_d[:, a0:a0 + cw]), B_lf, blf, blf)
            vt, bvt = val_r.next()
            S.dma("sp", lambda e: e.dma_start(out=vt[:, 0:cw], in_=valid16[:, c0:c0 + cw]), B_ext, bvt, bvt)
            S.op("dve", lambda e: e.tensor_tensor(out=lf[:, 0:cw], in0=lf[:, 0:cw], in1=vt[:, 0:cw],
                                                  op=ALU.mult), [bvt, blf], [blf])
            ct, bct = ct_r.next()
            S.op("dve", lambda e: e.tensor_tensor_scan(
                out=ct[:, 0:cw], data0=lf[:, 0:cw], data1=zeros16[:, 0:cw], initial=prev_last,
                op0=ALU.add, op1=ALU.add), [blf, B_z16, bprev], [bct])
            S.dma("pool", lambda e: e.dma_start(out=cT_d[:, a0:a0 + cw], in_=ct[:, 0:cw]), bct, B_cT, bct,
                  append=True)
            nt = cw // 128
            pt, bpt = P["f"].next()
            for t in range(nt):
                S.op("pe", lambda e: e.matmul(
                    pt[:, t * 16:(t + 1) * 16], lhsT=ct[:, t * 128:(t + 1) * 128],
                    rhs=ident_f[0:16, 0:16], start=True, stop=True), [bct] + CONST, [bpt])
            t0 = c0 // 128
            S.op("dve", lambda e: e.tensor_scalar(
                out=negck[:, t0:t0 + nt, :].rearrange("p t h -> p (t h)"), in0=pt[:, 0:nt * 16], scalar1=-1.0,
                scalar2=None, op0=ALU.mult), [bpt], [B_nck])
            prev_last = ct[:, cw - 1:cw]
            bprev = bct
        S.op("dve", lambda e: e.tensor_scalar(out=negck[:, 0, :], in0=negck[:, 0, :], scalar1=kb0_t[:, 0:1],
                                              scalar2=None, op0=ALU.add), [B_nck] + CONST, [B_nck])
        S.barrier()

    if nph >= 3:
      with contextlib.ExitStack() as ph:
        convT = sb("convT", [128, KC, 512], F32, ph)
        mkpsum(ph, 5, 2, 0)
        cb = sb("cb", [128, KC, 512], BF16, ph)
        sq = sb("sq", [128, KC, 512], BF16, ph)
        aT = sb("aT", [128, KC, 512], BF16, ph)
        B_conv, B_cb, B_sq, B_aT = Buf("convT"), Buf("cb"), Buf("sq"), Buf("aT")
        ut_r = Ring([sb("ut%d" % i, [128, 4, 160], BF16, ph) for i in range(2)], "ut")
        dg_r = Ring([sb("dg%d" % i, [128, TAPS, 128], BF16, ph) for i in range(2)], "dg")
        w_r = Ring([sb("wstc%d" % i, [128, KC, 512], BF16, ph) for i in range(2)], "wstc")
        gc_r = Ring([sb("gct%d" % i, [128, 4, 512], BF16, ph) for i in range(2)], "gct")
        ev_r = Ring([sb("evc%d" % i, [128, 4, 512], BF16, ph) for i in range(2)], "evc")
        mean_sb = sb("mean_sb", [128, 512], F32, ph)
        rstd_sb = sb("rstd_sb", [128, 512], F32, ph)
        m2_sb = sb("m2_sb", [128, 512], F32, ph)
        B_mean, B_rstd, B_m2 = Buf("mean"), Buf("rstd"), Buf("m2")
        xc_r = Ring([sb("xc%d" % i, [128, 512], F32, ph) for i in range(3)], "xc")
        for og in range(NOG):
            pmean, bpmean = P["o"].next()
            pex2, bpex2 = P["o"].next()
            for cc in range(KC):
                ut, but = ut_r.next()
                S.dma("sp", lambda e: e.dma_start(out=ut[:], in_=uT_d[cc, :, og * 4:(og + 1) * 4, :]),
                      B_uT, but, but)
                dg, bdg = dg_r.next()
                S.op("pool", lambda e: e.tensor_tensor(
                    out=dg[:], in0=ident_b[:].unsqueeze(1).to_broadcast([128, TAPS, 128]),
                    in1=dww_t[:, cc, :].unsqueeze(2).to_broadcast([128, TAPS, 128]), op=ALU.mult),
                    CONST, [bdg])
                pt, bpt = P["f"].next()
                for tap in range(TAPS):
                    S.op("pe", lambda e: e.matmul(
                        pt[:].rearrange("p (a b) -> p a b", b=128), lhsT=dg[:, tap, :],
                        rhs=ut[:, :, 2 + tap:2 + tap + 128], start=(tap == 0), stop=(tap == TAPS - 1)),
                        [bdg, but], [bpt])
                S.op("act", lambda e: e.activation(out=convT[:, cc, :], in_=pt[:], func=AF.Identity,
                                                   bias=dwb_t[:, cc:cc + 1], scale=1.0),
                     [bpt] + CONST, [B_conv])
                S.op("dve", lambda e: e.tensor_copy(out=cb[:, cc, :], in_=convT[:, cc, :]), [B_conv], [B_cb])
                S.op("act", lambda e: e.activation(out=sq[:, cc, :], in_=pt[:], func=AF.Square,
                                                   bias=dwb_t[:, cc:cc + 1], scale=1.0),
                     [bpt] + CONST, [B_sq])
                S.op("pe", lambda e: e.matmul(pmean[:], lhsT=onesm[:], rhs=cb[:, cc, :],
                                              start=(cc == 0), stop=(cc == KC - 1)), [B_cb] + CONST, [bpmean])
                S.op("pe", lambda e: e.matmul(pex2[:], lhsT=onesm[:], rhs=sq[:, cc, :],
                                              start=(cc == 0), stop=(cc == KC - 1)), [B_sq] + CONST, [bpex2])
            S.op("act", lambda e: e.activation(out=mean_sb[:], in_=pmean[:], func=AF.Copy), [bpmean], [B_mean])
            S.op("dve", lambda e: e.tensor_tensor(out=m2_sb[:], in0=mean_sb[:], in1=mean_sb[:], op=ALU.mult),
                 [B_mean], [B_m2])
            S.op("dve", lambda e: e.tensor_tensor(out=m2_sb[:], in0=pex2[:], in1=m2_sb[:], op=ALU.subtract),
                 [bpex2, B_m2], [B_m2])
            S.op("act", lambda e: e.activation(out=m2_sb[:], in_=m2_sb[:], func=AF.Sqrt, bias=eps_t[:, :],
                                               scale=1.0), [B_m2] + CONST, [B_m2])
            S.op("dve", lambda e: e.reciprocal(out=rstd_sb[:], in_=m2_sb[:]), [B_m2], [B_rstd])
            for cc in range(KC):
                xc, bxc = xc_r.next()
                S.op("dve", lambda e: e.tensor_tensor(out=xc[:], in0=convT[:, cc, :], in1=mean_sb[:],
                                                      op=ALU.subtract), [B_conv, B_mean], [bxc])
                S.op("dve", lambda e: e.tensor_tensor(out=xc[:], in0=xc[:], in1=rstd_sb[:], op=ALU.mult),
                     [bxc, B_rstd], [bxc])
                S.op("act", lambda e: e.activation(out=aT[:, cc, :], in_=xc[:], func=AF.Silu,
                                                   bias=clnb_t[:, cc:cc + 1], scale=clng_t[:, cc:cc + 1]),
                     [bxc] + CONST, [B_aT])
            for j in range(4):
                w, bw = load_w(w_r, wco_bf, B_wco, j * 512)
                gct, bgc = gc_r.next()
                S.dma("sp", lambda e: e.dma_start(
                    out=gct[:], in_=gcT_d[j * 4:(j + 1) * 4, :, og * 512:(og + 1) * 512].rearrange("c p t -> p c t")),
                    B_gc, bgc, bgc)
                ev, bev = ev_r.next()
                for c in range(4):
                    pt, bpt = P["f"].next()
                    mm_acc(pt[:], bpt, w, bw, c, aT, B_aT, 0, 512)
                    S.op("dve", lambda e: e.tensor_tensor(out=ev[:, c, :], in0=pt[:], in1=gct[:, c, :],
                                                          op=ALU.mult), [bpt, bgc], [bev])
                S.dma("pool", lambda e: e.dma_start(
                    out=ycg_d[j * 4:(j + 1) * 4, :, og * 512:(og + 1) * 512].rearrange("c p t -> p c t"),
                    in_=ev[:]), bev, B_ycg, bev, append=True)
        S.barrier()

    if nph >= 4:
      with contextlib.ExitStack() as ph:
        LA = 3
        mkpsum(ph, LA + 1, 4, 0)
        kT_r = Ring([sb("kTh%d" % i, [128, LK], BF16, ph) for i in range(2)], "kTh")
        v_r = Ring([sb("vh%d" % i, [128, AT, 129], BF16, ph) for i in range(2)], "vh")
        vm_r = Ring([sb("vm%d" % i, [16, 129], BF16, ph) for i in range(2)], "vm")
        q_r = Ring([sb("qTh%d" % i, [128, NOWN], BF16, ph) for i in range(2)], "qTh")
        cq_r = Ring([sb("cqb%d" % i, [128, NB, 128], F32, ph) for i in range(2)], "cqb")
        o_r = Ring([sb("oTh%d" % i, [128, NB, 128], BF16, ph) for i in range(2)], "oTh")
        tmp_r = Ring([sb("atmp%d" % i, [128, 512], F32, ph) for i in range(6)], "atmp")
        pT_r = Ring([sb("pT%d" % i, [128, 512], BF16, ph) for i in range(7)], "pT")
        rc_r = Ring([sb("rc%d" % i, [128, 1], F32, ph) for i in range(3)], "rc")
        for i in range(2):
            S.op("pool", lambda e: e.memset(v_r.tiles[i][:, :, 128:129], 1.0), [], [v_r.bufs[i]])
            S.op("pool", lambda e: e.memset(vm_r.tiles[i][:, 128:129], 1.0), [], [vm_r.bufs[i]])
        scale = float(HD) ** -0.5
        NGQ = NB // 4
        dst_r = Ring([sb("dcst%d" % i, [128, 2 * D], BF16, ph) for i in range(2)], "dcst")
        tasks = []

        def mk_w(src, dst, dbuf, kc):
            def f():
                t, b = dst_r.next()
                S.dma("pool", lambda e: e.dma_start(out=t[:, 0:D], in_=src[kc * 128:(kc + 1) * 128, :]), B_ext, b, b)
                S.dma("sp", lambda e: e.dma_start(out=dst[kc, :, :], in_=t[:, 0:D]), b, dbuf, b, append=True)
            return f

        def mk_uv(r0):
            def f():
                t, b = dst_r.next()
                S.dma("pool", lambda e: e.dma_start(out=t[:, 0:D], in_=tab_u[r0:r0 + 128, :]), B_ext, b, b)
                S.dma("pool", lambda e: e.dma_start(out=t[:, D:2 * D], in_=tab_v[r0:r0 + 128, :]), B_ext, b, b)
                S.dma("sp", lambda e: e.dma_start(out=uv_bf[r0:r0 + 128, :], in_=t[:]), b, B_uv, b, append=True)
            return f

        for (src, dst, dbuf) in ((w_ao, wao_bf, B_wao), (w_o, wo_bf, B_wo), (w_q, wq_bf, B_wq)):
            for kc in range(KC):
                tasks.append(mk_w(src, dst, dbuf, kc))
        if nph >= 7:
            for r0 in range(0, NEXP, 128):
                tasks.append(mk_uv(r0))
        items = []
        for h in range(NH):
            for g in range(NGQ):
                items.append((h, g, -1))
                for kt in range(8 * g + 8):
                    items.append((h, g, kt))
        HS = {}
        SP_ = {}
        ACC = {}

        def head_load(h):
            kT, bk = kT_r.next()
            S.dma("sp", lambda e: e.dma_start(out=kT[:], in_=kT_d[h]), B_kT, bk, bk)
            vh, bvh = v_r.next()
            for t0 in range(0, AT, 8):
                S.dma("sp", lambda e: e.dma_start(
                    out=vh[:, t0:t0 + 8, 0:128],
                    in_=v_d[N_META + t0 * 128:N_META + (t0 + 8) * 128, h * 128:(h + 1) * 128]
                    .rearrange("(t p) c -> p t c", p=128)), B_v, bvh, bvh)
            vm, bvm = vm_r.next()
            S.dma("sp", lambda e: e.dma_start(out=vm[:, 0:128], in_=v_d[0:N_META, h * 128:(h + 1) * 128]),
                  B_v, bvm, bvm)
            qT, bq = q_r.next()
            S.dma("sp", lambda e: e.dma_start(out=qT[:], in_=qT_d[h]), B_qT, bq, bq)
            cq, bcq = cq_r.next()
            for b0 in range(0, NB, 16):
                b1 = min(NB, b0 + 16)
                S.dma("sp", lambda e: e.dma_start(
                    out=cq[:, b0:b1, :],
                    in_=cT_d[h, N_META:LK].rearrange("(b two q) -> b two q", two=2, q=128)[b0:b1, 1, :]
                    .partition_broadcast(128)), B_cT, bcq, bcq)
            oT, bo = o_r.next()
            HS[h] = (kT, bk, vh, bvh, vm, bvm, qT, bq, cq, bcq, oT, bo)

        def jmin_of(g, kt):
            return 0 if kt < 0 else max(0, (kt - 8 * g) // 2)

        def emit_S(n):
            h, g, kt = items[n]
            if h not in HS:
                head_load(h)
            kT, bk, vh, bvh, vm, bvm, qT, bq, cq, bcq, oT, bo = HS[h]
            pt, bpt = P["f"].next()
            SP_[n] = (pt, bpt)
            c0 = jmin_of(g, kt) * 128
            if kt < 0:
                S.op("pe", lambda e: e.matmul(pt[:16, 0:512], lhsT=kT[:, 0:N_META],
                                              rhs=qT[:, g * 512:(g + 1) * 512], start=True, stop=True),
                     [bk, bq], [bpt])
            else:
                S.op("pe", lambda e: e.matmul(
                    pt[:, c0:512], lhsT=kT[:, N_META + kt * 128:N_META + (kt + 1) * 128],
                    rhs=qT[:, g * 512 + c0:(g + 1) * 512], start=True, stop=True), [bk, bq], [bpt])

        def emit_post(n):
            h, g, kt = items[n]
            kT, bk, vh, bvh, vm, bvm, qT, bq, cq, bcq, oT, bo = HS[h]
            pt, bpt = SP_.pop(n)
            rows = 16 if kt < 0 else 128
            jm = jmin_of(g, kt)
            c0 = jm * 128
            cqg = cq[:, 4 * g:4 * g + 4, :].rearrange("p a b -> p (a b)")
            tmp, btmp = tmp_r.next()
            S.op("dve", lambda e: e.scalar_tensor_tensor(
                out=tmp[:rows, c0:512], in0=pt[:rows, c0:512], scalar=scale, in1=cqg[:rows, c0:512],
                op0=ALU.mult, op1=ALU.add), [bpt, bcq], [btmp])
            pT, bpT = pT_r.next()
            bias = negckm[:, h:h + 1] if kt < 0 else negck[:, kt, h:h + 1]
            S.op("act", lambda e: e.activation(out=pT[:rows, c0:512], in_=tmp[:rows, c0:512], func=AF.Exp,
                                               bias=bias, scale=1.0), [btmp, B_nck], [bpT])
            if kt < 0:
                ACC[(h, g)] = [P["o"].next() for _ in range(4)]
            accs = ACC[(h, g)]
            for j in range(jm, 4):
                i = 4 * g + j
                po, bpo = accs[j]
                last = (kt == 2 * i + 1)
                if last:
                    S.op("pool", lambda e: e.tensor_tensor(out=pT[:, j * 128:(j + 1) * 128],
                                                           in0=pT[:, j * 128:(j + 1) * 128],
                                                           in1=tri_b[:], op=ALU.mult), [bpT] + CONST, [bpT])
                if kt < 0:
                    S.op("pe", lambda e: e.matmul(po[:, 0:129], lhsT=pT[:16, j * 128:(j + 1) * 128],
                                                  rhs=vm[:, :], start=True, stop=False), [bpT, bvm], [bpo])
                else:
                    S.op("pe", lambda e: e.matmul(po[:, 0:129], lhsT=pT[:, j * 128:(j + 1) * 128],
                                                  rhs=vh[:, kt, :], start=False, stop=last), [bpT, bvh], [bpo])
                if last:
                    rc, brc = rc_r.next()
                    S.op("dve", lambda e: e.reciprocal(out=rc[:], in_=po[:, 128:129]), [bpo], [brc])
                    S.op("dve", lambda e: e.tensor_scalar(out=oT[:, i, :], in0=po[:, 0:128], scalar1=rc[:, 0:1],
                                                          scalar2=None, op0=ALU.mult), [bpo, brc], [bo])
            if g == NGQ - 1 and kt == 8 * g + 7:
                for b0 in range(0, NB, 8):
                    b1 = min(NB, b0 + 8)
                    S.dma("pool", lambda e: e.dma_start(
                        out=o_d[b0 * 128:b1 * 128, h * 128:(h + 1) * 128].rearrange("(b p) c -> p b c", p=128),
                        in_=oT[:, b0:b1, :]), bo, B_oT, bo, append=True)
                del HS[h]

        NI = len(items)
        every = max(1, (NI * 3 // 4) // max(1, len(tasks)))
        for n in range(NI + LA):
            if n < NI:
                emit_S(n)
            if n >= LA:
                emit_post(n - LA)
            if tasks and n % every == 0:
                tasks.pop(0)()
        while tasks:
            tasks.pop(0)()
        S.barrier()

    if nph >= 5:
      with contextlib.ExitStack() as ph:
        l1g = sb("l1g", [128, D], F32, ph)
        mkpsum(ph, 5, 0, 2)
        ot_r = Ring([sb("otk%d" % i, [128, D], BF16, ph) for i in range(2)], "otk")
        l1b = sb("l1b", [128, D], F32, ph)
        B_l1 = Buf("l1")
        for dst_, src_ in ((l1g, ln1_g), (l1b, ln1_b)):
            S.dma("sp", lambda e: e.dma_start(out=dst_[:], in_=src_), B_ext, B_l1, B_l1)
        oT_r = Ring([sb("oTg%d" % i, [128, KC, 512], BF16, ph) for i in range(1)], "oTg")
        mixT = sb("mixT", [128, KC, 512], BF16, ph)
        B_mix = Buf("mixT")
        w_r = Ring([sb("wste%d" % i, [128, KC, 512], BF16, ph) for i in range(2)], "wste")
        ga_r = Ring([sb("gat%d" % i, [128, 4, 512], BF16, ph) for i in range(2)], "gat")
        yc_r = Ring([sb("yct%d" % i, [128, 4, 512], BF16, ph) for i in range(2)], "yct")
        tf_r = Ring([sb("tfe%d" % i, [128, 512], F32, ph) for i in range(2)], "tfe")
        h0_r = Ring([sb("h0t%d" % i, [128, D], F32, ph) for i in range(2)], "h0t")
        hp_r = Ring([sb("h1p%d" % i, [128, D], F32, ph) for i in range(2)], "h1p")
        h1_r = Ring([sb("h1t%d" % i, [128, D], F32, ph) for i in range(2)], "h1t")
        for og in range(NOG):
            oTg, boT = oT_r.next()
            for t in range(4):
                ob = og * 4 + t
                otk, botk = ot_r.next()
                S.dma("sp", lambda e: e.dma_start(out=otk[:], in_=o_d[ob * 128:(ob + 1) * 128, :]),
                      B_oT, botk, botk)
                transpose_to(otk, botk, 128, oTg, boT, t * 128)
            for j in range(4):
                w, bw = load_w(w_r, wao_bf, B_wao, j * 512)
                gat, bga = ga_r.next()
                S.dma("sp", lambda e: e.dma_start(
                    out=gat[:], in_=gaT_d[j * 4:(j + 1) * 4, :, og * 512:(og + 1) * 512].rearrange("c p t -> p c t")),
                    B_ga, bga, bga)
                yct, byc = yc_r.next()
                S.dma("sp", lambda e: e.dma_start(
                    out=yct[:], in_=ycg_d[j * 4:(j + 1) * 4, :, og * 512:(og + 1) * 512].rearrange("c p t -> p c t")),
                    B_ycg, byc, byc)
                for c in range(4):
                    pt, bpt = P["f"].next()
                    mm_acc(pt[:], bpt, w, bw, c, oTg, boT, 0, 512)
                    tf, btf = tf_r.next()
                    S.op("dve", lambda e: e.tensor_tensor(out=tf[:], in0=pt[:], in1=gat[:, c, :], op=ALU.mult),
                         [bpt, bga], [btf])
                    S.op("pool", lambda e: e.tensor_tensor(out=mixT[:, j * 4 + c, :], in0=tf[:],
                                                           in1=yct[:, c, :], op=ALU.add), [btf, byc], [B_mix])
            for t in range(4):
                ob = og * 4 + t
                h0t, bh0 = h0_r.next()
                S.dma("sp", lambda e: e.dma_start(out=h0t[:], in_=h0_d[ob * 128:(ob + 1) * 128, :]),
                      B_h0, bh0, bh0)
                hp, bhp = hp_r.next()
                for j in range(4):
                    w, bw = load_w(w_r, wo_bf, B_wo, j * 512)
                    pt, bpt = P["f"].next()
                    for kc in range(KC):
                        S.op("pe", lambda e: e.matmul(
                            pt[:], lhsT=mixT[:, kc, t * 128:(t + 1) * 128], rhs=w[:, kc, :],
                            start=(kc == 0), stop=(kc == KC - 1)), [bw, B_mix], [bpt])
                    S.op("dve", lambda e: e.scalar_tensor_tensor(
                        out=hp[:, j * 512:(j + 1) * 512], in0=h0t[:, j * 512:(j + 1) * 512], scalar=DN_ALPHA,
                        in1=pt[:], op0=ALU.mult, op1=ALU.add), [bpt, bh0], [bhp])
                h1t, bh1 = h1_r.next()
                layer_norm_tile(hp, bhp, 128, l1g, l1b, [B_l1], h1t, bh1)
                S.dma("pool", lambda e: e.dma_start(out=h1_d[ob * 128:(ob + 1) * 128, :], in_=h1t[:]),
                      bh1, B_h1, bh1, append=True)
        S.barrier()

    if nph >= 6:
      with contextlib.ExitStack() as ph:
        skT = sb("skT", [128, 16, 128], BF16, ph)
        mkpsum(ph, 5, 0, 2)
        B_sk = Buf("skT")
        sk_r = Ring([sb("skl%d" % i, [128, 128], F32, ph) for i in range(2)], "skl")
        for hp_ in range(16):
            skl, bskl = sk_r.next()
            S.dma("sp", lambda e: e.dma_start(out=skl[:], in_=subk[hp_]), B_ext, bskl, bskl)
            pt, bpt = P["f"].next()
            S.op("pe", lambda e: e.transpose(out=pt[:, 0:128], in_=skl[:], identity=ident_f[:]),
                 [bskl] + CONST, [bpt])
            S.op("act", lambda e: e.activation(out=skT[:, hp_, :], in_=pt[:, 0:128], func=AF.Copy),
                 [bpt], [B_sk])
        h1_r = Ring([sb("h1l%d" % i, [128, D], F32, ph) for i in range(2)], "h1l")
        xb_r = Ring([sb("h1b%d" % i, [128, D], BF16, ph) for i in range(2)], "h1b")
        h1T = sb("h1T", [128, KC, 512], BF16, ph)
        B_h1T = Buf("h1T")
        w_r = Ring([sb("wstq%d" % i, [128, KC, 512], BF16, ph) for i in range(2)], "wstq")
        qpT = sb("qpT", [128, 16, 512], BF16, ph)
        B_qp = Buf("qpT")
        s_r = Ring([sb("st%d" % i, [128, D], F32, ph) for i in range(2)], "st")
        for og in range(NOG):
            for t in range(4):
                ob = og * 4 + t
                h1l, bh1l = h1_r.next()
                S.dma("sp", lambda e: e.dma_start(out=h1l[:], in_=h1_d[ob * 128:(ob + 1) * 128, :]),
                      B_h1, bh1l, bh1l)
                xb, bxb = xb_r.next()
                S.op("act", lambda e: e.activation(out=xb[:], in_=h1l[:], func=AF.Copy), [bh1l], [bxb])
                transpose_to(xb, bxb, 128, h1T, B_h1T, t * 128)
            for j in range(4):
                w, bw = load_w(w_r, wq_bf, B_wq, j * 512)
                for c in range(4):
                    pt, bpt = P["f"].next()
                    mm_acc(pt[:], bpt, w, bw, c, h1T, B_h1T, 0, 512)
                    S.op("act", lambda e: e.activation(out=qpT[:, j * 4 + c, :], in_=pt[:], func=AF.Copy),
                         [bpt], [B_qp])
            for t in range(4):
                ob = og * 4 + t
                stl, bst_ = s_r.next()
                for hq in range(4):
                    pt, bpt = P["f"].next()
                    for c in range(4):
                        hp_ = hq * 4 + c
                        S.op("pe", lambda e: e.matmul(
                            pt[:, c * 128:(c + 1) * 128], lhsT=qpT[:, hp_, t * 128:(t + 1) * 128],
                            rhs=skT[:, hp_, :], start=True, stop=True), [B_qp, B_sk], [bpt])
                    S.op("act", lambda e: e.activation(out=stl[:, hq * 512:(hq + 1) * 512], in_=pt[:],
                                                       func=AF.Copy), [bpt], [bst_])
                S.dma("pool", lambda e: e.dma_start(out=s_d[ob * 128:(ob + 1) * 128, :], in_=stl[:]),
                      bst_, B_s, bst_, append=True)
        S.barrier()

    if nph >= 7:
      with contextlib.ExitStack() as ph:
        l2g = sb("l2g", [128, D], F32, ph)
        l2b = sb("l2b", [128, D], F32, ph)
        B_l2 = Buf("l2")
        for dst_, src_ in ((l2g, ln2_g), (l2b, ln2_b)):
            S.dma("sp", lambda e: e.dma_start(out=dst_[:], in_=src_), B_ext, B_l2, B_l2)
        s_r = Ring([sb("sf%d" % i, [128, 16, 128], F32, ph) for i in range(1)], "sf")
        h1_r = Ring([sb("h1f%d" % i, [128, D], F32, ph) for i in range(2)], "h1f")
        tkbuf = sb("tkbuf", [128, 2048], F32, ph)
        s2 = tkbuf[:].rearrange("p (a b) -> p a b", b=128)
        m16 = sb("m16", [128, 16, 16], F32, ph)
        ix16 = sb("ix16", [128, 16, 16], U32, ph)
        ixf = sb("ixf", [128, 16, 16], F32, ph)
        cand = sb("cand", [128, 8, 256], F32, ph)
        cand2 = tkbuf[:].rearrange("p (a b) -> p a b", b=256)
        vals = sb("vals", [128, 8, 16], F32, ph)
        posu = sb("posu", [128, 8, 16], U32, ph)
        posf = sb("posf", [128, 128], F32, ph)
        rf = sb("rf", [128, 128], F32, ph)
        cf = sb("cf", [128, 128], F32, ph)
        oh = tkbuf[:].rearrange("p (a b) -> p a b", b=16)
        If = sb("If", [128, 128], F32, ph)
        Jf = sb("Jf", [128, 128], F32, ph)
        idf = sb("idf", [128, 128], F32, ph)
        ids_r = Ring([sb("ids%d" % i, [128, 128], I32, ph) for i in range(2)], "ids")
        ev8 = sb("ev8", [128, 8, 16], F32, ph)
        z8 = sb("z8", [128, 8], F32, ph)
        g_r = Ring([sb("gk%d" % i, [128, 128], F32, ph) for i in range(2)], "gk")
        dots = sb("dots", [128, 128], F32, ph)
        wk_r = Ring([sb("wk%d" % i, [128, 128], F32, ph) for i in range(2)], "wk")
        mkpsum(ph, 0, 4, 0)
        gbuf_r = Ring([sb("gb%d" % i, [128, 2 * D], BF16, ph) for i in range(8)], "gb")
        junk_r = Ring([sb("junk%d" % i, [128, D // 2], BF16, ph) for i in range(2)], "junk")
        pr_r = Ring([sb("prd%d" % i, [128, D // 2], BF16, ph) for i in range(3)], "prd")
        jk2_r = Ring([sb("jk2_%d" % i, [128, D // 2], BF16, ph) for i in range(2)], "jk2")
        dots2 = sb("dots2", [128, 128], F32, ph)
        dg_r = Ring([sb("dgf%d" % i, [128, 128], BF16, ph) for i in range(4)], "dgf")
        dg1_r = Ring([sb("dgg%d" % i, [128, 128], F32, ph) for i in range(4)], "dgg")
        ak = sb("ak", [128, 128], F32, ph)
        wkt = sb("wkt", [128, 128], F32, ph)
        pre_r = Ring([sb("pre%d" % i, [128, D], F32, ph) for i in range(1)], "pre")
        o_r = Ring([sb("of%d" % i, [128, D], F32, ph) for i in range(1)], "of")
        colb = [Buf("col%d" % i) for i in range(4)]
        h1b_r = Ring([sb("h1bf%d" % i, [128, D], BF16, ph) for i in range(1)], "h1bf")
        B_tk, B_junk, B_dots = Buf("topk"), Buf("junk"), Buf("dots")
        TK = [B_tk]
        RES = {}

        def topk_gen(ob):
                sf, bsf = s_r.next()
                S.dma("sp", lambda e: e.dma_start(
                    out=sf[:], in_=s_d[ob * 128:(ob + 1) * 128, :].rearrange("p (a b) -> p a b", b=128)),
                    B_s, bsf, bsf)
                yield
                for hp_ in range(16):
                    S.op("dve", lambda e: e.max(out=m16[:, hp_, 0:8], in_=sf[:, hp_, :]), [bsf], TK)
                    S.op("dve", lambda e: e.match_replace(out=s2[:, hp_, :], in_to_replace=m16[:, hp_, 0:8],
                                                          in_values=sf[:, hp_, :], imm_value=-1e30), [bsf] + TK, TK)
                    S.op("dve", lambda e: e.max(out=m16[:, hp_, 8:16], in_=s2[:, hp_, :]), TK, TK)
                    S.op("dve", lambda e: e.max_index(out=ix16[:, hp_, 0:8], in_max=m16[:, hp_, 0:8],
                                                      in_values=sf[:, hp_, :]), [bsf] + TK, TK)
                    S.op("dve", lambda e: e.max_index(out=ix16[:, hp_, 8:16], in_max=m16[:, hp_, 8:16],
                                                      in_values=sf[:, hp_, :]), [bsf] + TK, TK)
                    yield
                S.op("dve", lambda e: e.tensor_copy(out=ixf[:], in_=ix16[:]), TK, TK)
                yield
                m4 = m16[:].rearrange("p (h s) k -> p h s k", s=2)
                ix4 = ixf[:].rearrange("p (h s) k -> p h s k", s=2)
                for h in range(8):
                    S.op("dve", lambda e: e.tensor_tensor(
                        out=cand[:, h, :].rearrange("p (r c) -> p r c", c=16),
                        in0=m4[:, h, 0, :].unsqueeze(2).to_broadcast([128, 16, 16]),
                        in1=m4[:, h, 1, :].unsqueeze(1).to_broadcast([128, 16, 16]), op=ALU.add), TK, TK)
                    yield
                for h in range(8):
                    S.op("dve", lambda e: e.max(out=vals[:, h, 0:8], in_=cand[:, h, :]), TK, TK)
                    S.op("dve", lambda e: e.match_replace(out=cand2[:, h, :], in_to_replace=vals[:, h, 0:8],
                                                          in_values=cand[:, h, :], imm_value=-1e30), TK, TK)
                    S.op("dve", lambda e: e.max(out=vals[:, h, 8:16], in_=cand2[:, h, :]), TK, TK)
                    S.op("dve", lambda e: e.max_index(out=posu[:, h, 0:8], in_max=vals[:, h, 0:8],
                                                      in_values=cand[:, h, :]), TK, TK)
                    S.op("dve", lambda e: e.max_index(out=posu[:, h, 8:16], in_max=vals[:, h, 8:16],
                                                      in_values=cand[:, h, :]), TK, TK)
                    yield
                S.op("dve", lambda e: e.tensor_copy(out=posf[:], in_=posu[:].rearrange("p h k -> p (h k)")), TK, TK)
                yield
                S.op("dve", lambda e: e.tensor_tensor(
                    out=oh[:], in0=posf[:].unsqueeze(2).to_broadcast([128, 128, 16]),
                    in1=thr16[:].unsqueeze(1).to_broadcast([128, 128, 16]), op=ALU.is_ge), TK + CONST, TK)
                yield
                S.op("dve", lambda e: e.reduce_sum(out=rf[:], in_=oh[:], axis=AX.X), TK, TK)
                yield
                S.op("dve", lambda e: e.scalar_tensor_tensor(out=cf[:], in0=rf[:], scalar=-16.0, in1=posf[:],
                                                             op0=ALU.mult, op1=ALU.add), TK, TK)
                yield
                for (src_rc, side, dstI) in ((rf, 0, If), (cf, 1, Jf)):
                    S.op("dve", lambda e: e.tensor_tensor(
                        out=oh[:], in0=src_rc[:].unsqueeze(2).to_broadcast([128, 128, 16]),
                        in1=iota16[:].unsqueeze(1).to_broadcast([128, 128, 16]), op=ALU.is_equal), TK + CONST, TK)
                    for h in range(8):
                        S.op("dve", lambda e: e.tensor_tensor(
                            out=oh[:, h * 16:(h + 1) * 16, :], in0=oh[:, h * 16:(h + 1) * 16, :],
                            in1=ix4[:, h, side, :].unsqueeze(1).to_broadcast([128, 16, 16]), op=ALU.mult), TK, TK)
                    yield
                    S.op("dve", lambda e: e.reduce_sum(out=dstI[:], in_=oh[:], axis=AX.X), TK, TK)
                    yield
                S.op("dve", lambda e: e.scalar_tensor_tensor(out=idf[:], in0=If[:], scalar=128.0, in1=Jf[:],
                                                             op0=ALU.mult, op1=ALU.add), TK, TK)
                yield
                ids, bids = ids_r.next()
                S.op("dve", lambda e: e.tensor_copy(out=ids[:], in_=idf[:]), TK, [bids])
                yield
                S.op("dve", lambda e: e.tensor_tensor(
                    out=ev8[:], in0=vals[:], in1=vals[:, :, 0:1].to_broadcast([128, 8, 16]), op=ALU.subtract),
                    TK, TK)
                yield
                S.op("act", lambda e: e.activation(out=ev8[:], in_=ev8[:], func=AF.Exp), TK, TK)
                yield
                S.op("dve", lambda e: e.reduce_sum(out=z8[:], in_=ev8[:], axis=AX.X), TK, TK)
                yield
                S.op("dve", lambda e: e.reciprocal(out=z8[:], in_=z8[:]), TK, TK)
                yield
                gk, bgk = g_r.next()
                S.op("dve", lambda e: e.tensor_tensor(
                    out=gk[:].rearrange("p (h k) -> p h k", k=16), in0=ev8[:],
                    in1=z8[:].unsqueeze(2).to_broadcast([128, 8, 16]), op=ALU.mult), TK, [bgk])
                yield
                RES[ob] = (ids, bids, gk, bgk)
                yield

        for _ in topk_gen(0):
            pass
        for ob in range(NB):
            ids, bids, gk, bgk = RES.pop(ob)
            nxt = topk_gen(ob + 1) if ob + 1 < NB else None
            h1f, bh1f = h1_r.next()
            S.dma("sp", lambda e: e.dma_start(out=h1f[:], in_=h1_d[ob * 128:(ob + 1) * 128, :]),
                  B_h1, bh1f, bh1f)
            h1b, bh1b = h1b_r.next()
            S.op("act", lambda e: e.activation(out=h1b[:], in_=h1f[:], func=AF.Copy), [bh1f], [bh1b])
            accs = [P["o"].next() for _ in range(4)]
            for k in range(128):
                gb, bgb = gbuf_r.next()
                S.dma("pool", lambda e: e.indirect_dma_start(
                    out=gb[:], out_offset=None, in_=uv_bf,
                    in_offset=bass.IndirectOffsetOnAxis(ap=ids[:, k:k + 1], axis=0)),
                    B_uv, bgb, bgb, extra_reads=[bids])
                jk, bjk = junk_r.next()
                cb_ = colb[k % 4]
                H2 = D // 2
                S.op("dve", lambda e: e.scalar_tensor_tensor(
                    out=jk[:], in0=gb[:, 0:H2], scalar=1.0, in1=h1b[:, 0:H2], op0=ALU.mult, op1=ALU.mult,
                    accum_out=dots[:, k:k + 1]), [bgb, bh1b], [bjk, cb_])
                pr, bpr = pr_r.next()
                S.op("dve", lambda e: e.tensor_tensor(out=pr[:], in0=gb[:, H2:D], in1=h1b[:, H2:D], op=ALU.mult),
                     [bgb, bh1b], [bpr])
                jk2, bjk2 = jk2_r.next()
                S.op("act", lambda e: e.activation(out=jk2[:], in_=pr[:], func=AF.Identity,
                                                   accum_out=dots2[:, k:k + 1]), [bpr], [bjk2, cb_])
                S.op("act", lambda e: e.activation(out=ak[:, k:k + 1], in_=dots[:, k:k + 1], func=AF.Gelu,
                                                   bias=dots2[:, k:k + 1], scale=1.0), [cb_], [cb_])
                dg1, bdg1 = dg1_r.next()
                S.op("act", lambda e: e.activation(out=dg1[:], in_=ident_f[:], func=AF.Identity,
                                                   scale=ak[:, k:k + 1]), [cb_] + CONST, [bdg1])
                dg, bdg = dg_r.next()
                S.op("act", lambda e: e.activation(out=dg[:], in_=dg1[:], func=AF.Identity,
                                                   scale=gk[:, k:k + 1]), [bdg1, bgk], [bdg])
                for j in range(4):
                    po, bpo = accs[j]
                    S.op("pe", lambda e: e.matmul(po[:, 0:512], lhsT=dg[:],
                                                  rhs=gb[:, D + j * 512:D + (j + 1) * 512],
                                                  start=(k == 0), stop=(k == 127)), [bdg, bgb], [bpo])
                if nxt is not None:
                    next(nxt, None)
            if nxt is not None:
                for _ in nxt:
                    pass
            pre, bpre = pre_r.next()
            for j in range(4):
                po, bpo = accs[j]
                S.op("dve", lambda e: e.scalar_tensor_tensor(
                    out=pre[:, j * 512:(j + 1) * 512], in0=h1f[:, j * 512:(j + 1) * 512], scalar=DN_ALPHA,
                    in1=po[:, 0:512], op0=ALU.mult, op1=ALU.add), [bh1f, bpo], [bpre])
            of, bof = o_r.next()
            layer_norm_tile(pre, bpre, 128, l2g, l2b, [B_l2], of, bof)
            S.dma("pool", lambda e: e.dma_start(out=out[ob * 128:(ob + 1) * 128, :], in_=of[:]),
                  bof, B_out, bof, append=True)
        S.barrier()

    S.final_wait("sp", [B_out])
    print("kernel build: %d instructions, %d dma sems" % (S.ninstr, S.nsem))
    stack.close()
    return nc


def host_inputs(SEQ, inputs):
    AT = SEQ // 128
    NTOK = AT * 128
    f32 = np.float32
    x = np.asarray(inputs["x"], f32)
    meta = np.ascontiguousarray(np.asarray(inputs["meta_tokens"], f32))
    b_in = np.asarray(inputs["b_in"], f32)[0]
    b_fm = np.zeros((128, 113), f32)
    cols = np.concatenate([b_in[OFF_A:OFF_V], b_in[OFF_GC:N_IN]])
    b_fm[:, :96] = cols.reshape(96, 128).T
    bf2 = np.zeros((128, 113), f32)
    bf2[:, 0:64] = b_fm[:, 0:64]
    bf2[:, 80:112] = b_fm[:, 64:96]
    bf2[:16, 112] = b_in[OFF_F:OFF_F + 16]

    def bc(v):
        return np.ascontiguousarray(np.broadcast_to(np.asarray(v, f32).reshape(1, -1), (128, D)))

    def fm(v):
        return np.ascontiguousarray(np.asarray(v, f32).reshape(KC, 128).T)

    common = {
        "xmeta": meta,
        "lnin_g": bc(inputs["ln_in_g"]), "lnin_b": bc(inputs["ln_in_b"]),
        "w_in": np.ascontiguousarray(np.asarray(inputs["w_in"], f32)[0]),
        "b_fm": bf2, "bv_b": bc(b_in[OFF_V:OFF_F]),
        "dww": np.ascontiguousarray(np.asarray(inputs["conv_dw_w"], f32)[0].T.reshape(KC, 128, TAPS).transpose(1, 0, 2)),
        "dwb": fm(inputs["conv_dw_b"]), "cln_g": fm(inputs["conv_ln_g"]), "cln_b": fm(inputs["conv_ln_b"]),
        "w_co": np.ascontiguousarray(np.asarray(inputs["w_conv_out"], f32)[0]),
        "w_ao": np.ascontiguousarray(np.asarray(inputs["w_attn_out"], f32)[0]),
        "w_o": np.ascontiguousarray(np.asarray(inputs["w_out"], f32)[0]),
        "ln1_g": bc(inputs["ln1_g"]), "ln1_b": bc(inputs["ln1_b"]),
        "w_q": np.ascontiguousarray(np.asarray(inputs["peer_w_q"], f32)[0]),
        "subk": np.ascontiguousarray(np.asarray(inputs["peer_subkeys"], f32)[0].reshape(16, 128, 128)),
        "tab_u": np.ascontiguousarray(np.asarray(inputs["peer_u"], f32)[0]),
        "tab_v": np.ascontiguousarray(np.asarray(inputs["peer_v"], f32)[0]),
        "ln2_g": bc(inputs["ln2_g"]), "ln2_b": bc(inputs["ln2_b"]),
        "ident": np.eye(128, dtype=f32),
        "tri": np.triu(np.ones((128, 128), f32)),
        "iota16": np.ascontiguousarray(np.broadcast_to(np.arange(16, dtype=f32), (128, 16))),
    }
    maps = []
    for core in range(8):
        b, p = core // 2, core % 2
        xa = np.zeros((NTOK, D), f32)
        valid = np.ones((NTOK,), f32)
        kb0 = np.zeros((128, 1), f32)
        hmask = np.ones((128, 32), f32)
        if p == 0:
            xa[128:] = x[b, :NTOK - 128]
            xa[112:128] = meta
            valid[:128] = 0.0
            kb0[:] = NEG
            hmask[:, :16] = 0.0
        else:
            xa[:] = x[b, :NTOK]
        m = dict(common)
        m.update({"xa": xa, "valid16": np.ascontiguousarray(np.broadcast_to(valid, (16, NTOK))),
                  "kb0": kb0, "hmask": hmask})
        maps.append(m)
    return maps


def assemble(SEQ, results):
    AT = SEQ // 128
    NB = AT // 2
    out = np.zeros((4, SEQ, D), np.float32)
    for core in range(8):
        b, p = core // 2, core % 2
        o = results[core]["out"].reshape(NB, 128, D)
        for i in range(NB):
            rt = 2 * i + p
            out[b, rt * 128:(rt + 1) * 128] = o[i]
    return out


_NC_CACHE = {}


def kernel(**inputs):
    SEQ = int(np.asarray(inputs["x"]).shape[1])
    if SEQ not in _NC_CACHE:
        _NC_CACHE[SEQ] = build(SEQ)
    nc = _NC_CACHE[SEQ]
    maps = host_inputs(SEQ, inputs)
    res = run_bass_kernel_spmd(nc, maps, core_ids=list(range(8)))
    return assemble(SEQ, res.results)
```

```python
import contextlib
import numpy as np
import concourse.bass as bass
import concourse.mybir as mybir
from concourse.bass_utils import run_bass_kernel_spmd

F32 = mybir.dt.float32
BF16 = mybir.dt.bfloat16
U32 = mybir.dt.uint32
I32 = mybir.dt.int32
ALU = mybir.AluOpType
AF = mybir.ActivationFunctionType
AX = mybir.AxisListType

D = 2048
KC = 16
NH = 16
HD = 128
N_META = 16
TAPS = 31
N_IN = 14352
OFF_A, OFF_G, OFF_Q, OFF_K, OFF_V, OFF_F, OFF_GC, OFF_GA = 0, 2048, 4096, 6144, 8192, 10240, 10256, 12304
LN_EPS = 1e-5
DN_ALPHA = 2.0 ** 0.25
NEXP = 16384
NEG = -30000.0


class Buf:
    __slots__ = ("name", "w", "r", "sem")

    def __init__(self, name):
        self.name = name
        self.w = {}
        self.r = {}
        self.sem = None


class Sched:
    def __init__(self, nc, stack):
        self.nc = nc
        self.stack = stack
        self.names = ["pe", "act", "dve", "pool", "sp"]
        self.eng = {"pe": nc.tensor, "act": nc.scalar, "dve": nc.vector, "pool": nc.gpsimd, "sp": nc.sync}
        self.esem = {e: stack.enter_context(nc.semaphore("s_" + e)) for e in self.names}
        self.ecount = {e: 0 for e in self.names}
        self.waited = {e: {} for e in self.names}
        self.semcount = {}
        self.nsem = 0
        self.ninstr = 0

    def _wait(self, eng, deps):
        wd = self.waited[eng]
        for sem, val in deps.items():
            if eng == "pe" and sem is self.esem["pe"]:
                continue
            if wd.get(sem, 0) >= val:
                continue
            wd[sem] = val
            self.eng[eng].wait_ge(sem, val)

    @staticmethod
    def _merge(dst, src):
        for s, v in src.items():
            if dst.get(s, 0) < v:
                dst[s] = v

    def op(self, eng, fn, reads=(), writes=()):
        deps = {}
        for b in reads:
            self._merge(deps, b.w)
        for b in writes:
            self._merge(deps, b.w)
            self._merge(deps, b.r)
        self._wait(eng, deps)
        self.ecount[eng] += 1
        self.ninstr += 1
        sem = self.esem[eng]
        tok = {sem: self.ecount[eng]}
        fn(self.eng[eng]).then_inc(sem, 1)
        for b in reads:
            self._merge(b.r, tok)
        for b in writes:
            b.w = dict(tok)
            b.r = {}

    def _slot_sem(self, b):
        if b.sem is None:
            b.sem = self.stack.enter_context(self.nc.semaphore("d%d" % self.nsem))
            self.nsem += 1
            self.semcount[b.sem] = 0
        return b.sem

    def dma(self, q, fn, src, dst, slot, extra_reads=(), append=False):
        deps = {}
        self._merge(deps, src.w)
        for b in extra_reads:
            self._merge(deps, b.w)
        if not append:
            self._merge(deps, dst.w)
            self._merge(deps, dst.r)
        self._wait(q, deps)
        sem = self._slot_sem(slot)
        self.semcount[sem] += 16
        self.ninstr += 1
        tok = {sem: self.semcount[sem]}
        fn(self.eng[q]).then_inc(sem, 16)
        self._merge(src.r, tok)
        for b in extra_reads:
            self._merge(b.r, tok)
        if append:
            self._merge(dst.w, tok)
        else:
            dst.w = dict(tok)
            dst.r = {}

    def final_wait(self, eng, bufs):
        deps = {}
        for b in bufs:
            self._merge(deps, b.w)
        self._wait(eng, deps)

    def barrier(self):
        deps = {self.esem[e]: self.ecount[e] for e in self.names if self.ecount[e] > 0}
        for sem, v in self.semcount.items():
            if v > 0:
                deps[sem] = v
        for e in self.names:
            self._wait(e, dict(deps))


class Ring:
    def __init__(self, tiles, name):
        self.tiles = tiles
        self.bufs = [Buf("%s%d" % (name, i)) for i in range(len(tiles))]
        self.i = -1

    def next(self):
        self.i = (self.i + 1) % len(self.tiles)
        return self.tiles[self.i], self.bufs[self.i]


def build(SEQ, dbg=False, stop_after="F"):
    AT = SEQ // 128
    NB = AT // 2
    NG = AT // 4
    NTOK = AT * 128
    NOWN = NB * 128
    LK = N_META + NTOK
    NOG = NOWN // 512
    assert AT % 8 == 0
    PH = "0 AB B2 C D E1 E2 F".split()
    nph = PH.index(stop_after)

    nc = bass.Bass("TRN2", target_bir_lowering=False)
    stack = contextlib.ExitStack()

    def din(name, shape, dt=F32):
        return nc.dram_tensor(name, list(shape), dt, kind="ExternalInput").ap()

    def dscr(name, shape, dt):
        return nc.dram_tensor(name, list(shape), dt,
                              kind="ExternalOutput" if dbg else "Internal").ap()

    def dint(name, shape, dt):
        return nc.dram_tensor(name, list(shape), dt, kind="Internal").ap()

    xa = din("xa", [NTOK, D])
    xmeta = din("xmeta", [N_META, D])
    valid16 = din("valid16", [16, NTOK])
    kb0 = din("kb0", [128, 1])
    hmask = din("hmask", [128, 32])
    lnin_g = din("lnin_g", [128, D])
    lnin_b = din("lnin_b", [128, D])
    w_in = din("w_in", [D, N_IN])
    b_fm = din("b_fm", [128, 113])
    bv_b = din("bv_b", [128, D])
    dww = din("dww", [128, KC, TAPS])
    dwb = din("dwb", [128, KC])
    cln_g = din("cln_g", [128, KC])
    cln_b = din("cln_b", [128, KC])
    w_co = din("w_co", [D, D])
    w_ao = din("w_ao", [D, D])
    w_o = din("w_o", [D, D])
    ln1_g = din("ln1_g", [128, D])
    ln1_b = din("ln1_b", [128, D])
    w_q = din("w_q", [D, D])
    subk = din("subk", [16, 128, 128])
    tab_u = din("tab_u", [NEXP, D])
    tab_v = din("tab_v", [NEXP, D])
    ln2_g = din("ln2_g", [128, D])
    ln2_b = din("ln2_b", [128, D])
    ident_in = din("ident", [128, 128])
    tri_in = din("tri", [128, 128])
    iota16_in = din("iota16", [128, 16])
    out = nc.dram_tensor("out", [NOWN, D], F32, kind="ExternalOutput").ap()

    win_bf = dint("win_bf", [KC, 128, N_IN], BF16)
    wco_bf = dint("wco_bf", [KC, 128, D], BF16)
    wao_bf = dint("wao_bf", [KC, 128, D], BF16)
    wo_bf = dint("wo_bf", [KC, 128, D], BF16)
    wq_bf = dint("wq_bf", [KC, 128, D], BF16)
    kT_d = dscr("kT_d", [NH, 128, LK], BF16)
    v_d = dscr("v_d", [LK, D], BF16)
    qT_d = dscr("qT_d", [NH, 128, NOWN], BF16)
    uT_d = dscr("uT_d", [KC, 128, NB, 160], BF16)
    gcT_d = dscr("gcT_d", [KC, 128, NOWN], BF16)
    gaT_d = dscr("gaT_d", [KC, 128, NOWN], BF16)
    logf_d = dscr("logf_d", [16, LK], F32)
    cT_d = dscr("cT_d", [16, LK], F32)
    ycg_d = dscr("ycg_d", [KC, 128, NOWN], BF16)
    o_d = dscr("o_d", [NOWN, D], BF16)
    h0_d = dscr("h0_d", [NOWN, D], F32)
    h1_d = dscr("h1_d", [NOWN, D], F32)
    s_d = dscr("s_d", [NOWN, D], F32)
    uv_bf = dint("uv_bf", [NEXP, 2 * D], BF16)

    S = Sched(nc, stack)
    B_ext = Buf("ext")

    def sb(name, shape, dt, st=None):
        return (st or stack).enter_context(nc.sbuf_tensor("t_" + name, list(shape), dt))

    def ps(name, shape, dt=F32):
        return stack.enter_context(nc.psum_tensor(name, list(shape), dt))

    P = {}
    pscount = [0]

    def mkpsum(st, nf, no, nb):
        assert nf + no + nb <= 8
        pscount[0] += 1
        tag = "p%d_" % pscount[0]
        P["f"] = Ring([st.enter_context(nc.psum_tensor(tag + "f%d" % i, [128, 512], F32)) for i in range(nf)], "psf")
        P["o"] = Ring([st.enter_context(nc.psum_tensor(tag + "o%d" % i, [128, 512], F32)) for i in range(no)], "pso")
        P["b"] = Ring([st.enter_context(nc.psum_tensor(tag + "b%d" % i, [128, 1024], BF16)) for i in range(nb)], "psb")

    B_c = Buf("consts")
    ident_f = sb("ident_f", [128, 128], F32)
    ident_b = sb("ident_b", [128, 128], BF16)
    tri_b = sb("tri_b", [128, 128], BF16)
    tri_f = sb("tri_f", [128, 128], F32)
    iota16 = sb("iota16", [128, 16], F32)
    thr16 = sb("thr16", [128, 16], F32)
    bfm = sb("bfm", [128, 113], F32)
    nbf = sb("nbf", [16, 1], F32)
    kb0_t = sb("kb0_t", [128, 1], F32)
    hmask_t = sb("hmask_t", [128, 32], F32)
    eps_t = sb("eps_t", [128, 1], F32)
    one_t = sb("one_t", [128, 1], F32)
    onesm = sb("onesm", [128, 128], BF16)
    dww_t = sb("dww_t", [128, KC, TAPS], F32)
    dwb_t = sb("dwb_t", [128, KC], F32)
    clng_t = sb("clng_t", [128, KC], F32)
    clnb_t = sb("clnb_t", [128, KC], F32)
    negck = sb("negck", [128, AT, NH], F32)
    negckm = sb("negckm", [16, NH], F32)
    st_r = Ring([sb("lnst%d" % i, [128, 4, 6], F32) for i in range(2)], "lnst")
    mv_r = Ring([sb("lnmv%d" % i, [128, 4], F32) for i in range(2)], "lnmv")

    def cload(dst, src):
        S.dma("sp", lambda e: e.dma_start(out=dst, in_=src), B_ext, B_c, B_c)

    cload(ident_f[:], ident_in)
    cload(tri_f[:], tri_in)
    cload(iota16[:], iota16_in)
    cload(bfm[:], b_fm)
    cload(kb0_t[:], kb0)
    cload(hmask_t[:], hmask)
    cload(dww_t[:], dww)
    cload(dwb_t[:], dwb)
    cload(clng_t[:], cln_g)
    cload(clnb_t[:], cln_b)
    B_c2 = Buf("consts2")
    S.op("dve", lambda e: e.tensor_copy(out=ident_b[:], in_=ident_f[:]), [B_c], [B_c2])
    S.op("dve", lambda e: e.tensor_copy(out=tri_b[:], in_=tri_f[:]), [B_c], [B_c2])
    S.op("dve", lambda e: e.memset(eps_t[:], LN_EPS), [], [B_c2])
    S.op("dve", lambda e: e.memset(one_t[:], 1.0), [], [B_c2])
    S.op("dve", lambda e: e.memset(onesm[:], 1.0 / D), [], [B_c2])
    S.op("dve", lambda e: e.tensor_scalar(out=nbf[:], in0=bfm[0:16, 112:113], scalar1=-1.0, scalar2=None,
                                          op0=ALU.mult), [B_c], [B_c2])
    S.op("dve", lambda e: e.tensor_scalar(out=thr16[:], in0=iota16[:], scalar1=16.0, scalar2=16.0,
                                          op0=ALU.mult, op1=ALU.add), [B_c], [B_c2])
    CONST = [B_c, B_c2]

    (B_win, B_wco, B_wao, B_wo, B_wq, B_kT, B_v, B_qT, B_uT, B_gc, B_ga, B_lf, B_cT, B_h0,
     B_ycg, B_oT, B_h1, B_s, B_out, B_nck, B_uv) = (Buf(n) for n in (
         "win wco wao wo wq kT v qT uT gc ga lf cT h0 ycg oT h1 s out nck uv").split())

    def layer_norm_tile(xt, bx, rows, g_t, b_t, gb_bufs, out_f32, b_out):
        st, bst = st_r.next()
        mv, bmv = mv_r.next()
        for j in range(4):
            S.op("dve", lambda e, j=j: e.bn_stats(out=st[:rows, j, :], in_=xt[:rows, j * 512:(j + 1) * 512]),
                 [bx], [bst])
        S.op("dve", lambda e: e.bn_aggr(out=mv[:rows, 0:2], in_=st[:rows].rearrange("p a b -> p (a b)")),
             [bst], [bmv])
        S.op("act", lambda e: e.activation(out=mv[:rows, 2:3], in_=mv[:rows, 1:2], func=AF.Sqrt,
                                           bias=eps_t[:rows, :], scale=1.0), [bmv] + CONST, [bmv])
        S.op("dve", lambda e: e.reciprocal(out=mv[:rows, 3:4], in_=mv[:rows, 2:3]), [bmv], [bmv])
        S.op("dve", lambda e: e.tensor_scalar(out=out_f32[:rows], in0=xt[:rows], scalar1=mv[:rows, 0:1],
                                              scalar2=mv[:rows, 3:4], op0=ALU.subtract, op1=ALU.mult),
             [bx, bmv], [b_out])
        S.op("pool", lambda e: e.tensor_tensor(out=out_f32[:rows], in0=out_f32[:rows], in1=g_t[:rows],
                                               op=ALU.mult), [b_out] + gb_bufs, [b_out])
        S.op("dve", lambda e: e.tensor_tensor(out=out_f32[:rows], in0=out_f32[:rows], in1=b_t[:rows],
                                              op=ALU.add), [b_out] + gb_bufs, [b_out])

    def transpose_to(xb, bxb, rows, dstT, bdst, col0):
        for half in range(2):
            pt, bpt = P["b"].next()
            for j in range(8):
                kc = half * 8 + j
                S.op("pe", lambda e, kc=kc, j=j, pt=pt: e.transpose(
                    out=pt[:, j * 128:j * 128 + rows], in_=xb[:rows, kc * 128:(kc + 1) * 128],
                    identity=ident_b[:rows, :rows]), [bxb] + CONST, [bpt])
            S.op("act", lambda e, half=half, pt=pt: e.activation(
                out=dstT[:, half * 8:(half + 1) * 8, col0:col0 + rows],
                in_=pt[:].rearrange("p (j t) -> p j t", t=128)[:, :, 0:rows], func=AF.Copy),
                [bpt], [bdst])

    def mm_acc(pt_ap, bpt, w, bw, wc, rhsT, brhs, c0, n):
        for kc in range(KC):
            S.op("pe", lambda e, kc=kc: e.matmul(
                pt_ap, lhsT=w[:, kc, wc * 128:(wc + 1) * 128], rhs=rhsT[:, kc, c0:c0 + n],
                start=(kc == 0), stop=(kc == KC - 1)), [bw, brhs], [bpt])

    def mm_own(pt, bpt, w, bw, wc, rhsT, brhs, c0, n):
        for kc in range(KC):
            S.op("pe", lambda e: e.matmul(
                pt[:, 0:2 * n].rearrange("p (a b) -> p a b", b=n), lhsT=w[:, kc, wc * 128:(wc + 1) * 128],
                rhs=rhsT[:, kc, :].rearrange("p (a b) -> p a b", b=256)[:, :, c0:c0 + n],
                start=(kc == 0), stop=(kc == KC - 1)), [bw, brhs], [bpt])

    def load_w(w_r, src_bf, bsrc, col0, ncols=512):
        w, bw = w_r.next()
        S.dma("sp", lambda e: e.dma_start(
            out=w[:, :, 0:ncols], in_=src_bf[:, :, col0:col0 + ncols].rearrange("k p c -> p k c")),
            bsrc, bw, bw)
        return w, bw

    with contextlib.ExitStack() as ph:
        stage_r = Ring([sb("cst%d" % i, [128, 2048], BF16, ph) for i in range(8)], "cst")

        def precast(src, dst, ncols, dbuf):
            for kc in range(KC):
                for c0 in range(0, ncols, 2048):
                    cw = min(2048, ncols - c0)
                    t, b = stage_r.next()
                    S.dma("pool", lambda e: e.dma_start(
                        out=t[:, 0:cw], in_=src[kc * 128:(kc + 1) * 128, c0:c0 + cw]), B_ext, b, b)
                    S.dma("sp", lambda e: e.dma_start(
                        out=dst[kc, :, c0:c0 + cw], in_=t[:, 0:cw]), b, dbuf, b, append=True)

        precast(w_in, win_bf, N_IN, B_win)
        precast(w_co, wco_bf, D, B_wco)
        if nph < 4:
            precast(w_ao, wao_bf, D, B_wao)
            precast(w_o, wo_bf, D, B_wo)
            precast(w_q, wq_bf, D, B_wq)
        S.barrier()

    if nph >= 1:
      with contextlib.ExitStack() as ph:
        lng_t = sb("lng_t", [128, D], F32, ph)
        mkpsum(ph, 5, 0, 2)
        lnb_t = sb("lnb_t", [128, D], F32, ph)
        bvb_t = sb("bvb_t", [128, D], F32, ph)
        B_ln = Buf("lnconst")
        for dst_, src_ in ((lng_t, lnin_g), (lnb_t, lnin_b), (bvb_t, bv_b)):
            S.dma("sp", lambda e: e.dma_start(out=dst_[:], in_=src_), B_ext, B_ln, B_ln)
        x_r = Ring([sb("xt%d" % i, [128, D], F32, ph) for i in range(2)], "xt")
        xn_r = Ring([sb("xn%d" % i, [128, D], F32, ph) for i in range(2)], "xn")
        xb_r = Ring([sb("xb%d" % i, [128, D], BF16, ph) for i in range(2)], "xb")
        h0T_r = Ring([sb("h0T%d" % i, [128, KC, 512], BF16, ph) for i in range(2)], "h0T")
        w_r = Ring([sb("wst%d" % i, [128, KC, 512], BF16, ph) for i in range(2)], "wst")
        wf_t = sb("wf_t", [128, KC, 16], BF16, ph)
        B_wf = Buf("wf")
        S.dma("sp", lambda e: e.dma_start(out=wf_t[:], in_=win_bf[:, :, OFF_F:OFF_F + 16].rearrange("k p c -> p k c")),
              B_win, B_wf, B_wf)
        sig_r = Ring([sb("sig%d" % i, [128, 4, 320], F32, ph) for i in range(2)], "sig")
        ev_r = Ring([sb("ev%d" % i, [128, 4, 512], BF16, ph) for i in range(3)], "ev")
        lf_r = Ring([sb("lft%d" % i, [16, 512], F32, ph) for i in range(2)], "lft")

        def group_proj(h0T, bh, gi, ntok, meta):
            ktok0 = 0 if meta else N_META + gi * 512
            own = [] if meta else [(2 * gi, 1), (2 * gi + 1, 3)]
            if not meta:
                for j in range(4):
                    wg, bwg = load_w(w_r, win_bf, B_win, OFF_G + j * 512)
                    sg, bsg = sig_r.next()
                    for c in range(4):
                        pt, bpt = P["f"].next()
                        mm_own(pt, bpt, wg, bwg, c, h0T, bh, 96, 160)
                        bc = 16 + j * 4 + c
                        S.op("act", lambda e: e.activation(
                            out=sg[:, c, :], in_=pt[:, 0:320], func=AF.Sigmoid,
                            bias=bfm[:, bc:bc + 1], scale=1.0), [bpt] + CONST, [bsg])
                    wa, bwa = load_w(w_r, win_bf, B_win, OFF_A + j * 512)
                    ev, bev = ev_r.next()
                    for c in range(4):
                        pt, bpt = P["f"].next()
                        mm_own(pt, bpt, wa, bwa, c, h0T, bh, 96, 160)
                        bc = j * 4 + c
                        S.op("dve", lambda e: e.scalar_tensor_tensor(
                            out=ev[:, c, 0:320], in0=pt[:, 0:320], scalar=bfm[:, bc:bc + 1],
                            in1=sg[:, c, :], op0=ALU.add, op1=ALU.mult), [bpt, bsg] + CONST, [bev])
                    if gi == 0:
                        S.op("pool", lambda e: e.tensor_tensor(
                            out=ev[:, :, 0:32], in0=ev[:, :, 0:32],
                            in1=hmask_t[:].unsqueeze(1).to_broadcast([128, 4, 32]), op=ALU.mult),
                            [bev] + CONST, [bev])
                    for (ob, _), o in zip(own, (0, 160)):
                        S.dma("pool", lambda e: e.dma_start(
                            out=uT_d[j * 4:(j + 1) * 4, :, ob, :].rearrange("c p t -> p c t"),
                            in_=ev[:, :, o:o + 160]), bev, B_uT, bev, append=True)
                for (off, dst, bdst, func, bcol0) in ((OFF_Q, qT_d, B_qT, AF.Identity, 32),
                                                       (OFF_GC, gcT_d, B_gc, AF.Sigmoid, 80),
                                                       (OFF_GA, gaT_d, B_ga, AF.Sigmoid, 96)):
                    for j in range(4):
                        w, bw = load_w(w_r, win_bf, B_win, off + j * 512)
                        ev, bev = ev_r.next()
                        for c in range(4):
                            pt, bpt = P["f"].next()
                            mm_own(pt, bpt, w, bw, c, h0T, bh, 128, 128)
                            bc = bcol0 + j * 4 + c
                            S.op("act", lambda e: e.activation(
                                out=ev[:, c, 0:256], in_=pt[:, 0:256], func=func,
                                bias=bfm[:, bc:bc + 1], scale=1.0), [bpt] + CONST, [bev])
                        S.dma("pool", lambda e: e.dma_start(
                            out=dst[j * 4:(j + 1) * 4, :, gi * 256:(gi + 1) * 256].rearrange("c p t -> p c t"),
                            in_=ev[:, :, 0:256]), bev, bdst, bev, append=True)
            for j in range(4):
                w, bw = load_w(w_r, win_bf, B_win, OFF_K + j * 512)
                ev, bev = ev_r.next()
                for c in range(4):
                    pt, bpt = P["f"].next()
                    mm_acc(pt[:, 0:ntok], bpt, w, bw, c, h0T, bh, 0, ntok)
                    bc = 48 + j * 4 + c
                    S.op("dve", lambda e: e.tensor_scalar(
                        out=ev[:, c, 0:ntok], in0=pt[:, 0:ntok], scalar1=bfm[:, bc:bc + 1], scalar2=None,
                        op0=ALU.add), [bpt] + CONST, [bev])
                S.dma("pool", lambda e: e.dma_start(
                    out=kT_d[j * 4:(j + 1) * 4, :, ktok0:ktok0 + ntok].rearrange("c p t -> p c t"),
                    in_=ev[:, :, 0:ntok]), bev, B_kT, bev, append=True)
            ntile = 1 if meta else 4
            rows = ntok if meta else 128
            for j in range(4):
                w, bw = load_w(w_r, win_bf, B_win, OFF_V + j * 512)
                ev, bev = ev_r.next()
                for t in range(ntile):
                    pt, bpt = P["f"].next()
                    for kc in range(KC):
                        S.op("pe", lambda e: e.matmul(
                            pt[:rows, :], lhsT=h0T[:, kc, t * 128:t * 128 + rows], rhs=w[:, kc, :],
                            start=(kc == 0), stop=(kc == KC - 1)), [bw, bh], [bpt])
                    S.op("dve", lambda e: e.tensor_tensor(
                        out=ev[:rows, t, :], in0=pt[:rows, :], in1=bvb_t[:rows, j * 512:(j + 1) * 512],
                        op=ALU.add), [bpt, B_ln], [bev])
                if meta:
                    S.dma("pool", lambda e: e.dma_start(
                        out=v_d[0:rows, j * 512:(j + 1) * 512], in_=ev[:rows, 0, :]), bev, B_v, bev, append=True)
                else:
                    S.dma("pool", lambda e: e.dma_start(
                        out=v_d[ktok0:ktok0 + 512, j * 512:(j + 1) * 512].rearrange("(t p) c -> p t c", p=128),
                        in_=ev[:, :, :]), bev, B_v, bev, append=True)
            pt, bpt = P["f"].next()
            for kc in range(KC):
                S.op("pe", lambda e: e.matmul(
                    pt[:16, 0:ntok], lhsT=wf_t[:, kc, :], rhs=h0T[:, kc, 0:ntok],
                    start=(kc == 0), stop=(kc == KC - 1)), [B_wf, bh], [bpt])
            lt, blt = lf_r.next()
            S.op("act", lambda e: e.activation(out=lt[:, 0:ntok], in_=pt[:16, 0:ntok], func=AF.Exp,
                                               bias=nbf[:, :], scale=-1.0), [bpt] + CONST, [blt])
            S.op("act", lambda e: e.activation(out=lt[:, 0:ntok], in_=lt[:, 0:ntok], func=AF.Ln,
                                               bias=one_t[:16, :], scale=1.0), [blt] + CONST, [blt])
            S.op("dve", lambda e: e.tensor_scalar(out=lt[:, 0:ntok], in0=lt[:, 0:ntok],
                                                  scalar1=-1.0, scalar2=None, op0=ALU.mult), [blt], [blt])
            S.dma("pool", lambda e: e.dma_start(out=logf_d[:, ktok0:ktok0 + ntok], in_=lt[:, 0:ntok]),
                  blt, B_lf, blt, append=True)

        def ln_in_tile(src_ap, rows, h0T, bh, col0, own_block):
            xt, bx = x_r.next()
            S.dma("sp", lambda e: e.dma_start(out=xt[:rows], in_=src_ap), B_ext, bx, bx)
            xn, bxn = xn_r.next()
            layer_norm_tile(xt, bx, rows, lng_t, lnb_t, [B_ln], xn, bxn)
            if own_block is not None:
                S.dma("pool", lambda e: e.dma_start(out=h0_d[own_block * 128:(own_block + 1) * 128, :],
                                                    in_=xn[:]), bxn, B_h0, bxn, append=True)
            xb, bxb = xb_r.next()
            S.op("act", lambda e: e.activation(out=xb[:rows], in_=xn[:rows], func=AF.Copy), [bxn], [bxb])
            transpose_to(xb, bxb, rows, h0T, bh, col0)

        h0T, bh = h0T_r.next()
        ln_in_tile(xmeta, N_META, h0T, bh, 0, None)
        group_proj(h0T, bh, 0, N_META, True)
        for gi in range(NG):
            h0T, bh = h0T_r.next()
            for t in range(4):
                at = gi * 4 + t
                ln_in_tile(xa[at * 128:(at + 1) * 128, :], 128, h0T, bh, t * 128,
                           (at // 2) if (at % 2 == 1) else None)
            group_proj(h0T, bh, gi, 512, False)
        S.barrier()

    if nph >= 2:
      with contextlib.ExitStack() as ph:
        CH = 2048
        mkpsum(ph, 4, 0, 0)
        zeros16 = sb("zeros16", [16, CH], F32, ph)
        B_z16 = Buf("z16")
        S.op("pool", lambda e: e.memset(zeros16[:], 0.0), [], [B_z16])
        lf_r2 = Ring([sb("lfc%d" % i, [16, CH], F32, ph) for i in range(2)], "lfc")
        val_r = Ring([sb("val%d" % i, [16, CH], F32, ph) for i in range(2)], "val")
        ct_r = Ring([sb("ctc%d" % i, [16, CH], F32, ph) for i in range(2)], "ctc")
        lfm, blfm = lf_r2.next()
        S.dma("sp", lambda e: e.dma_start(out=lfm[:, 0:N_META], in_=logf_d[:, 0:N_META]), B_lf, blfm, blfm)
        ctp, bctp = ct_r.next()
        S.op("dve", lambda e: e.tensor_tensor_scan(out=ctp[:, 0:N_META], data0=lfm[:, 0:N_META],
                                                   data1=zeros16[:, 0:N_META], initial=0.0,
                                                   op0=ALU.add, op1=ALU.add), [blfm, B_z16], [bctp])
        S.dma("pool", lambda e: e.dma_start(out=cT_d[:, 0:N_META], in_=ctp[:, 0:N_META]), bctp, B_cT, bctp,
              append=True)
        pt, bpt = P["f"].next()
        S.op("pe", lambda e: e.matmul(pt[:16, 0:16], lhsT=ctp[:, 0:N_META], rhs=ident_f[0:16, 0:16],
                                      start=True, stop=True), [bctp] + CONST, [bpt])
        S.op("dve", lambda e: e.tensor_scalar(out=negckm[:, :], in0=pt[:16, 0:16], scalar1=-1.0,
                                              scalar2=None, op0=ALU.mult), [bpt], [B_nck])
        prev_last = ctp[:, N_META - 1:N_META]
        bprev = bctp
        for c0 in range(0, NTOK, CH):
            cw = min(CH, NTOK - c0)
            a0 = N_META + c0
            lf, blf = lf_r2.next()
            S.dma("sp", lambda e: e.dma_start(out=lf[:, 0:cw], in_=logf_d[:, a0:a0 + cw]), B_lf, blf, blf)
            vt, bvt = val_r.next()
            S.dma("sp", lambda e: e.dma_start(out=vt[:, 0:cw], in_=valid16[:, c0:c0 + cw]), B_ext, bvt, bvt)
            S.op("dve", lambda e: e.tensor_tensor(out=lf[:, 0:cw], in0=lf[:, 0:cw], in1=vt[:, 0:cw],
                                                  op=ALU.mult), [bvt, blf], [blf])
            ct, bct = ct_r.next()
            S.op("dve", lambda e: e.tensor_tensor_scan(
                out=ct[:, 0:cw], data0=lf[:, 0:cw], data1=zeros16[:, 0:cw], initial=prev_last,
                op0=ALU.add, op1=ALU.add), [blf, B_z16, bprev], [bct])
            S.dma("pool", lambda e: e.dma_start(out=cT_d[:, a0:a0 + cw], in_=ct[:, 0:cw]), bct, B_cT, bct,
                  append=True)
            nt = cw // 128
            pt, bpt = P["f"].next()
            for t in range(nt):
                S.op("pe", lambda e: e.matmul(
                    pt[:, t * 16:(t + 1) * 16], lhsT=ct[:, t * 128:(t + 1) * 128],
                    rhs=ident_f[0:16, 0:16], start=True, stop=True), [bct] + CONST, [bpt])
            t0 = c0 // 128
            S.op("dve", lambda e: e.tensor_scalar(
                out=negck[:, t0:t0 + nt, :].rearrange("p t h -> p (t h)"), in0=pt[:, 0:nt * 16], scalar1=-1.0,
                scalar2=None, op0=ALU.mult), [bpt], [B_nck])
            prev_last = ct[:, cw - 1:cw]
            bprev = bct
        S.op("dve", lambda e: e.tensor_scalar(out=negck[:, 0, :], in0=negck[:, 0, :], scalar1=kb0_t[:, 0:1],
                                              scalar2=None, op0=ALU.add), [B_nck] + CONST, [B_nck])
        S.barrier()

    if nph >= 3:
      with contextlib.ExitStack() as ph:
        convT = sb("convT", [128, KC, 512], F32, ph)
        mkpsum(ph, 5, 2, 0)
        cb = sb("cb", [128, KC, 512], BF16, ph)
        sq = sb("sq", [128, KC, 512], BF16, ph)
        aT = sb("aT", [128, KC, 512], BF16, ph)
        B_conv, B_cb, B_sq, B_aT = Buf("convT"), Buf("cb"), Buf("sq"), Buf("aT")
        ut_r = Ring([sb("ut%d" % i, [128, 4, 160], BF16, ph) for i in range(2)], "ut")
        dg_r = Ring([sb("dg%d" % i, [128, TAPS, 128], BF16, ph) for i in range(2)], "dg")
        w_r = Ring([sb("wstc%d" % i, [128, KC, 512], BF16, ph) for i in range(2)], "wstc")
        gc_r = Ring([sb("gct%d" % i, [128, 4, 512], BF16, ph) for i in range(2)], "gct")
        ev_r = Ring([sb("evc%d" % i, [128, 4, 512], BF16, ph) for i in range(2)], "evc")
        mean_sb = sb("mean_sb", [128, 512], F32, ph)
        rstd_sb = sb("rstd_sb", [128, 512], F32, ph)
        m2_sb = sb("m2_sb", [128, 512], F32, ph)
        B_mean, B_rstd, B_m2 = Buf("mean"), Buf("rstd"), Buf("m2")
        xc_r = Ring([sb("xc%d" % i, [128, 512], F32, ph) for i in range(3)], "xc")
        for og in range(NOG):
            pmean, bpmean = P["o"].next()
            pex2, bpex2 = P["o"].next()
            for cc in range(KC):
                ut, but = ut_r.next()
                S.dma("sp", lambda e: e.dma_start(out=ut[:], in_=uT_d[cc, :, og * 4:(og + 1) * 4, :]),
                      B_uT, but, but)
                dg, bdg = dg_r.next()
                S.op("pool", lambda e: e.tensor_tensor(
                    out=dg[:], in0=ident_b[:].unsqueeze(1).to_broadcast([128, TAPS, 128]),
                    in1=dww_t[:, cc, :].unsqueeze(2).to_broadcast([128, TAPS, 128]), op=ALU.mult),
                    CONST, [bdg])
                pt, bpt = P["f"].next()
                for tap in range(TAPS):
                    S.op("pe", lambda e: e.matmul(
                        pt[:].rearrange("p (a b) -> p a b", b=128), lhsT=dg[:, tap, :],
                        rhs=ut[:, :, 2 + tap:2 + tap + 128], start=(tap == 0), stop=(tap == TAPS - 1)),
                        [bdg, but], [bpt])
                S.op("act", lambda e: e.activation(out=convT[:, cc, :], in_=pt[:], func=AF.Identity,
                                                   bias=dwb_t[:, cc:cc + 1], scale=1.0),
                     [bpt] + CONST, [B_conv])
                S.op("dve", lambda e: e.tensor_copy(out=cb[:, cc, :], in_=convT[:, cc, :]), [B_conv], [B_cb])
                S.op("act", lambda e: e.activation(out=sq[:, cc, :], in_=pt[:], func=AF.Square,
                                                   bias=dwb_t[:, cc:cc + 1], scale=1.0),
                     [bpt] + CONST, [B_sq])
                S.op("pe", lambda e: e.matmul(pmean[:], lhsT=onesm[:], rhs=cb[:, cc, :],
                                              start=(cc == 0), stop=(cc == KC - 1)), [B_cb] + CONST, [bpmean])
                S.op("pe", lambda e: e.matmul(pex2[:], lhsT=onesm[:], rhs=sq[:, cc, :],
                                              start=(cc == 0), stop=(cc == KC - 1)), [B_sq] + CONST, [bpex2])
            S.op("act", lambda e: e.activation(out=mean_sb[:], in_=pmean[:], func=AF.Copy), [bpmean], [B_mean])
            S.op("dve", lambda e: e.tensor_tensor(out=m2_sb[:], in0=mean_sb[:], in1=mean_sb[:], op=ALU.mult),
                 [B_mean], [B_m2])
            S.op("dve", lambda e: e.tensor_tensor(out=m2_sb[:], in0=pex2[:], in1=m2_sb[:], op=ALU.subtract),
                 [bpex2, B_m2], [B_m2])
            S.op("act", lambda e: e.activation(out=m2_sb[:], in_=m2_sb[:], func=AF.Sqrt, bias=eps_t[:, :],
                                               scale=1.0), [B_m2] + CONST, [B_m2])
            S.op("dve", lambda e: e.reciprocal(out=rstd_sb[:], in_=m2_sb[:]), [B_m2], [B_rstd])
            for cc in range(KC):
                xc, bxc = xc_r.next()
                S.op("dve", lambda e: e.tensor_tensor(out=xc[:], in0=convT[:, cc, :], in1=mean_sb[:],
                                                      op=ALU.subtract), [B_conv, B_mean], [bxc])
                S.op("dve", lambda e: e.tensor_tensor(out=xc[:], in0=xc[:], in1=rstd_sb[:], op=ALU.mult),
                     [bxc, B_rstd], [bxc])
                S.op("act", lambda e: e.activation(out=aT[:, cc, :], in_=xc[:], func=AF.Silu,
                                                   bias=clnb_t[:, cc:cc + 1], scale=clng_t[:, cc:cc + 1]),
                     [bxc] + CONST, [B_aT])
            for j in range(4):
                w, bw = load_w(w_r, wco_bf, B_wco, j * 512)
                gct, bgc = gc_r.next()
                S.dma("sp", lambda e: e.dma_start(
                    out=gct[:], in_=gcT_d[j * 4:(j + 1) * 4, :, og * 512:(og + 1) * 512].rearrange("c p t -> p c t")),
                    B_gc, bgc, bgc)
                ev, bev = ev_r.next()
                for c in range(4):
                    pt, bpt = P["f"].next()
                    mm_acc(pt[:], bpt, w, bw, c, aT, B_aT, 0, 512)
                    S.op("dve", lambda e: e.tensor_tensor(out=ev[:, c, :], in0=pt[:], in1=gct[:, c, :],
                                                          op=ALU.mult), [bpt, bgc], [bev])
                S.dma("pool", lambda e: e.dma_start(
                    out=ycg_d[j * 4:(j + 1) * 4, :, og * 512:(og + 1) * 512].rearrange("c p t -> p c t"),
                    in_=ev[:]), bev, B_ycg, bev, append=True)
        S.barrier()

    if nph >= 4:
      with contextlib.ExitStack() as ph:
        LA = 3
        mkpsum(ph, LA + 1, 4, 0)
        kT_r = Ring([sb("kTh%d" % i, [128, LK], BF16, ph) for i in range(2)], "kTh")
        v_r = Ring([sb("vh%d" % i, [128, AT, 129], BF16, ph) for i in range(2)], "vh")
        vm_r = Ring([sb("vm%d" % i, [16, 129], BF16, ph) for i in range(2)], "vm")
        q_r = Ring([sb("qTh%d" % i, [128, NOWN], BF16, ph) for i in range(2)], "qTh")
        cq_r = Ring([sb("cqb%d" % i, [128, NB, 128], F32, ph) for i in range(2)], "cqb")
        o_r = Ring([sb("oTh%d" % i, [128, NB, 128], BF16, ph) for i in range(2)], "oTh")
        tmp_r = Ring([sb("atmp%d" % i, [128, 512], F32, ph) for i in range(6)], "atmp")
        pT_r = Ring([sb("pT%d" % i, [128, 512], BF16, ph) for i in range(7)], "pT")
        rc_r = Ring([sb("rc%d" % i, [128, 1], F32, ph) for i in range(3)], "rc")
        for i in range(2):
            S.op("pool", lambda e: e.memset(v_r.tiles[i][:, :, 128:129], 1.0), [], [v_r.bufs[i]])
            S.op("pool", lambda e: e.memset(vm_r.tiles[i][:, 128:129], 1.0), [], [vm_r.bufs[i]])
        scale = float(HD) ** -0.5
        NGQ = NB // 4
        dst_r = Ring([sb("dcst%d" % i, [128, 2 * D], BF16, ph) for i in range(2)], "dcst")
        tasks = []

        def mk_w(src, dst, dbuf, kc):
            def f():
                t, b = dst_r.next()
                S.dma("pool", lambda e: e.dma_start(out=t[:, 0:D], in_=src[kc * 128:(kc + 1) * 128, :]), B_ext, b, b)
                S.dma("sp", lambda e: e.dma_start(out=dst[kc, :, :], in_=t[:, 0:D]), b, dbuf, b, append=True)
            return f

        def mk_uv(r0):
            def f():
                t, b = dst_r.next()
                S.dma("pool", lambda e: e.dma_start(out=t[:, 0:D], in_=tab_u[r0:r0 + 128, :]), B_ext, b, b)
                S.dma("pool", lambda e: e.dma_start(out=t[:, D:2 * D], in_=tab_v[r0:r0 + 128, :]), B_ext, b, b)
                S.dma("sp", lambda e: e.dma_start(out=uv_bf[r0:r0 + 128, :], in_=t[:]), b, B_uv, b, append=True)
            return f

        for (src, dst, dbuf) in ((w_ao, wao_bf, B_wao), (w_o, wo_bf, B_wo), (w_q, wq_bf, B_wq)):
            for kc in range(KC):
                tasks.append(mk_w(src, dst, dbuf, kc))
        if nph >= 7:
            for r0 in range(0, NEXP, 128):
                tasks.append(mk_uv(r0))
        items = []
        for h in range(NH):
            for g in range(NGQ):
                items.append((h, g, -1))
                for kt in range(8 * g + 8):
                    items.append((h, g, kt))
        HS = {}
        SP_ = {}
        ACC = {}

        def head_load(h):
            kT, bk = kT_r.next()
            S.dma("sp", lambda e: e.dma_start(out=kT[:], in_=kT_d[h]), B_kT, bk, bk)
            vh, bvh = v_r.next()
            for t0 in range(0, AT, 8):
                S.dma("sp", lambda e: e.dma_start(
                    out=vh[:, t0:t0 + 8, 0:128],
                    in_=v_d[N_META + t0 * 128:N_META + (t0 + 8) * 128, h * 128:(h + 1) * 128]
                    .rearrange("(t p) c -> p t c", p=128)), B_v, bvh, bvh)
            vm, bvm = vm_r.next()
            S.dma("sp", lambda e: e.dma_start(out=vm[:, 0:128], in_=v_d[0:N_META, h * 128:(h + 1) * 128]),
                  B_v, bvm, bvm)
            qT, bq = q_r.next()
            S.dma("sp", lambda e: e.dma_start(out=qT[:], in_=qT_d[h]), B_qT, bq, bq)
            cq, bcq = cq_r.next()
            for b0 in range(0, NB, 16):
                b1 = min(NB, b0 + 16)
                S.dma("sp", lambda e: e.dma_start(
                    out=cq[:, b0:b1, :],
                    in_=cT_d[h, N_META:LK].rearrange("(b two q) -> b two q", two=2, q=128)[b0:b1, 1, :]
                    .partition_broadcast(128)), B_cT, bcq, bcq)
            oT, bo = o_r.next()
            HS[h] = (kT, bk, vh, bvh, vm, bvm, qT, bq, cq, bcq, oT, bo)

        def jmin_of(g, kt):
            return 0 if kt < 0 else max(0, (kt - 8 * g) // 2)

        def emit_S(n):
            h, g, kt = items[n]
            if h not in HS:
                head_load(h)
            kT, bk, vh, bvh, vm, bvm, qT, bq, cq, bcq, oT, bo = HS[h]
            pt, bpt = P["f"].next()
            SP_[n] = (pt, bpt)
            c0 = jmin_of(g, kt) * 128
            if kt < 0:
                S.op("pe", lambda e: e.matmul(pt[:16, 0:512], lhsT=kT[:, 0:N_META],
                                              rhs=qT[:, g * 512:(g + 1) * 512], start=True, stop=True),
                     [bk, bq], [bpt])
            else:
                S.op("pe", lambda e: e.matmul(
                    pt[:, c0:512], lhsT=kT[:, N_META + kt * 128:N_META + (kt + 1) * 128],
                    rhs=qT[:, g * 512 + c0:(g + 1) * 512], start=True, stop=True), [bk, bq], [bpt])

        def emit_post(n):
            h, g, kt = items[n]
            kT, bk, vh, bvh, vm, bvm, qT, bq, cq, bcq, oT, bo = HS[h]
            pt, bpt = SP_.pop(n)
            rows = 16 if kt < 0 else 128
            jm = jmin_of(g, kt)
            c0 = jm * 128
            cqg = cq[:, 4 * g:4 * g + 4, :].rearrange("p a b -> p (a b)")
            tmp, btmp = tmp_r.next()
            S.op("dve", lambda e: e.scalar_tensor_tensor(
                out=tmp[:rows, c0:512], in0=pt[:rows, c0:512], scalar=scale, in1=cqg[:rows, c0:512],
                op0=ALU.mult, op1=ALU.add), [bpt, bcq], [btmp])
            pT, bpT = pT_r.next()
            bias = negckm[:, h:h + 1] if kt < 0 else negck[:, kt, h:h + 1]
            S.op("act", lambda e: e.activation(out=pT[:rows, c0:512], in_=tmp[:rows, c0:512], func=AF.Exp,
                                               bias=bias, scale=1.0), [btmp, B_nck], [bpT])
            if kt < 0:
                ACC[(h, g)] = [P["o"].next() for _ in range(4)]
            accs = ACC[(h, g)]
            for j in range(jm, 4):
                i = 4 * g + j
                po, bpo = accs[j]
                last = (kt == 2 * i + 1)
                if last:
                    S.op("pool", lambda e: e.tensor_tensor(out=pT[:, j * 128:(j + 1) * 128],
                                                           in0=pT[:, j * 128:(j + 1) * 128],
                                                           in1=tri_b[:], op=ALU.mult), [bpT] + CONST, [bpT])
                if kt < 0:
                    S.op("pe", lambda e: e.matmul(po[:, 0:129], lhsT=pT[:16, j * 128:(j + 1) * 128],
                                                  rhs=vm[:, :], start=True, stop=False), [bpT, bvm], [bpo])
                else:
                    S.op("pe", lambda e: e.matmul(po[:, 0:129], lhsT=pT[:, j * 128:(j + 1) * 128],
                                                  rhs=vh[:, kt, :], start=False, stop=last), [bpT, bvh], [bpo])
                if last:
                    rc, brc = rc_r.next()
                    S.op("dve", lambda e: e.reciprocal(out=rc[:], in_=po[:, 128:129]), [bpo], [brc])
                    S.op("dve", lambda e: e.tensor_scalar(out=oT[:, i, :], in0=po[:, 0:128], scalar1=rc[:, 0:1],
                                                          scalar2=None, op0=ALU.mult), [bpo, brc], [bo])
            if g == NGQ - 1 and kt == 8 * g + 7:
                for b0 in range(0, NB, 8):
                    b1 = min(NB, b0 + 8)
                    S.dma("pool", lambda e: e.dma_start(
                        out=o_d[b0 * 128:b1 * 128, h * 128:(h + 1) * 128].rearrange("(b p) c -> p b c", p=128),
                        in_=oT[:, b0:b1, :]), bo, B_oT, bo, append=True)
                del HS[h]

        NI = len(items)
        every = max(1, (NI * 3 // 4) // max(1, len(tasks)))
        for n in range(NI + LA):
            if n < NI:
                emit_S(n)
            if n >= LA:
                emit_post(n - LA)
            if tasks and n % every == 0:
                tasks.pop(0)()
        while tasks:
            tasks.pop(0)()
        S.barrier()

    if nph >= 5:
      with contextlib.ExitStack() as ph:
        l1g = sb("l1g", [128, D], F32, ph)
        mkpsum(ph, 5, 0, 2)
        ot_r = Ring([sb("otk%d" % i, [128, D], BF16, ph) for i in range(2)], "otk")
        l1b = sb("l1b", [128, D], F32, ph)
        B_l1 = Buf("l1")
        for dst_, src_ in ((l1g, ln1_g), (l1b, ln1_b)):
            S.dma("sp", lambda e: e.dma_start(out=dst_[:], in_=src_), B_ext, B_l1, B_l1)
        oT_r = Ring([sb("oTg%d" % i, [128, KC, 512], BF16, ph) for i in range(1)], "oTg")
        mixT = sb("mixT", [128, KC, 512], BF16, ph)
        B_mix = Buf("mixT")
        w_r = Ring([sb("wste%d" % i, [128, KC, 512], BF16, ph) for i in range(2)], "wste")
        ga_r = Ring([sb("gat%d" % i, [128, 4, 512], BF16, ph) for i in range(2)], "gat")
        yc_r = Ring([sb("yct%d" % i, [128, 4, 512], BF16, ph) for i in range(2)], "yct")
        tf_r = Ring([sb("tfe%d" % i, [128, 512], F32, ph) for i in range(2)], "tfe")
        h0_r = Ring([sb("h0t%d" % i, [128, D], F32, ph) for i in range(2)], "h0t")
        hp_r = Ring([sb("h1p%d" % i, [128, D], F32, ph) for i in range(2)], "h1p")
        h1_r = Ring([sb("h1t%d" % i, [128, D], F32, ph) for i in range(2)], "h1t")
        for og in range(NOG):
            oTg, boT = oT_r.next()
            for t in range(4):
                ob = og * 4 + t
                otk, botk = ot_r.next()
                S.dma("sp", lambda e: e.dma_start(out=otk[:], in_=o_d[ob * 128:(ob + 1) * 128, :]),
                      B_oT, botk, botk)
                transpose_to(otk, botk, 128, oTg, boT, t * 128)
            for j in range(4):
                w, bw = load_w(w_r, wao_bf, B_wao, j * 512)
                gat, bga = ga_r.next()
                S.dma("sp", lambda e: e.dma_start(
                    out=gat[:], in_=gaT_d[j * 4:(j + 1) * 4, :, og * 512:(og + 1) * 512].rearrange("c p t -> p c t")),
                    B_ga, bga, bga)
                yct, byc = yc_r.next()
                S.dma("sp", lambda e: e.dma_start(
                    out=yct[:], in_=ycg_d[j * 4:(j + 1) * 4, :, og * 512:(og + 1) * 512].rearrange("c p t -> p c t")),
                    B_ycg, byc, byc)
                for c in range(4):
                    pt, bpt = P["f"].next()
                    mm_acc(pt[:], bpt, w, bw, c, oTg, boT, 0, 512)
                    tf, btf = tf_r.next()
                    S.op("dve", lambda e: e.tensor_tensor(out=tf[:], in0=pt[:], in1=gat[:, c, :], op=ALU.mult),
                         [bpt, bga], [btf])
                    S.op("pool", lambda e: e.tensor_tensor(out=mixT[:, j * 4 + c, :], in0=tf[:],
                                                           in1=yct[:, c, :], op=ALU.add), [btf, byc], [B_mix])
            for t in range(4):
                ob = og * 4 + t
                h0t, bh0 = h0_r.next()
                S.dma("sp", lambda e: e.dma_start(out=h0t[:], in_=h0_d[ob * 128:(ob + 1) * 128, :]),
                      B_h0, bh0, bh0)
                hp, bhp = hp_r.next()
                for j in range(4):
                    w, bw = load_w(w_r, wo_bf, B_wo, j * 512)
                    pt, bpt = P["f"].next()
                    for kc in range(KC):
                        S.op("pe", lambda e: e.matmul(
                            pt[:], lhsT=mixT[:, kc, t * 128:(t + 1) * 128], rhs=w[:, kc, :],
                            start=(kc == 0), stop=(kc == KC - 1)), [bw, B_mix], [bpt])
                    S.op("dve", lambda e: e.scalar_tensor_tensor(
                        out=hp[:, j * 512:(j + 1) * 512], in0=h0t[:, j * 512:(j + 1) * 512], scalar=DN_ALPHA,
                        in1=pt[:], op0=ALU.mult, op1=ALU.add), [bpt, bh0], [bhp])
                h1t, bh1 = h1_r.next()
                layer_norm_tile(hp, bhp, 128, l1g, l1b, [B_l1], h1t, bh1)
                S.dma("pool", lambda e: e.dma_start(out=h1_d[ob * 128:(ob + 1) * 128, :], in_=h1t[:]),
                      bh1, B_h1, bh1, append=True)
        S.barrier()

    if nph >= 6:
      with contextlib.ExitStack() as ph:
        skT = sb("skT", [128, 16, 128], BF16, ph)
        mkpsum(ph, 5, 0, 2)
        B_sk = Buf("skT")
        sk_r = Ring([sb("skl%d" % i, [128, 128], F32, ph) for i in range(2)], "skl")
        for hp_ in range(16):
            skl, bskl = sk_r.next()
            S.dma("sp", lambda e: e.dma_start(out=skl[:], in_=subk[hp_]), B_ext, bskl, bskl)
            pt, bpt = P["f"].next()
            S.op("pe", lambda e: e.transpose(out=pt[:, 0:128], in_=skl[:], identity=ident_f[:]),
                 [bskl] + CONST, [bpt])
            S.op("act", lambda e: e.activation(out=skT[:, hp_, :], in_=pt[:, 0:128], func=AF.Copy),
                 [bpt], [B_sk])
        h1_r = Ring([sb("h1l%d" % i, [128, D], F32, ph) for i in range(2)], "h1l")
        xb_r = Ring([sb("h1b%d" % i, [128, D], BF16, ph) for i in range(2)], "h1b")
        h1T = sb("h1T", [128, KC, 512], BF16, ph)
        B_h1T = Buf("h1T")
        w_r = Ring([sb("wstq%d" % i, [128, KC, 512], BF16, ph) for i in range(2)], "wstq")
        qpT = sb("qpT", [128, 16, 512], BF16, ph)
        B_qp = Buf("qpT")
        s_r = Ring([sb("st%d" % i, [128, D], F32, ph) for i in range(2)], "st")
        for og in range(NOG):
            for t in range(4):
                ob = og * 4 + t
                h1l, bh1l = h1_r.next()
                S.dma("sp", lambda e: e.dma_start(out=h1l[:], in_=h1_d[ob * 128:(ob + 1) * 128, :]),
                      B_h1, bh1l, bh1l)
                xb, bxb = xb_r.next()
                S.op("act", lambda e: e.activation(out=xb[:], in_=h1l[:], func=AF.Copy), [bh1l], [bxb])
                transpose_to(xb, bxb, 128, h1T, B_h1T, t * 128)
            for j in range(4):
                w, bw = load_w(w_r, wq_bf, B_wq, j * 512)
                for c in range(4):
                    pt, bpt = P["f"].next()
                    mm_acc(pt[:], bpt, w, bw, c, h1T, B_h1T, 0, 512)
                    S.op("act", lambda e: e.activation(out=qpT[:, j * 4 + c, :], in_=pt[:], func=AF.Copy),
                         [bpt], [B_qp])
            for t in range(4):
                ob = og * 4 + t
                stl, bst_ = s_r.next()
                for hq in range(4):
                    pt, bpt = P["f"].next()
                    for c in range(4):
                        hp_ = hq * 4 + c
                        S.op("pe", lambda e: e.matmul(
                            pt[:, c * 128:(c + 1) * 128], lhsT=qpT[:, hp_, t * 128:(t + 1) * 128],
                            rhs=skT[:, hp_, :], start=True, stop=True), [B_qp, B_sk], [bpt])
                    S.op("act", lambda e: e.activation(out=stl[:, hq * 512:(hq + 1) * 512], in_=pt[:],
                                                       func=AF.Copy), [bpt], [bst_])
                S.dma("pool", lambda e: e.dma_start(out=s_d[ob * 128:(ob + 1) * 128, :], in_=stl[:]),
                      bst_, B_s, bst_, append=True)
        S.barrier()

    if nph >= 7:
      with contextlib.ExitStack() as ph:
        l2g = sb("l2g", [128, D], F32, ph)
        l2b = sb("l2b", [128, D], F32, ph)
        B_l2 = Buf("l2")
        for dst_, src_ in ((l2g, ln2_g), (l2b, ln2_b)):
            S.dma("sp", lambda e: e.dma_start(out=dst_[:], in_=src_), B_ext, B_l2, B_l2)
        s_r = Ring([sb("sf%d" % i, [128, 16, 128], F32, ph) for i in range(1)], "sf")
        h1_r = Ring([sb("h1f%d" % i, [128, D], F32, ph) for i in range(2)], "h1f")
        tkbuf = sb("tkbuf", [128, 2048], F32, ph)
        s2 = tkbuf[:].rearrange("p (a b) -> p a b", b=128)
        m16 = sb("m16", [128, 16, 16], F32, ph)
        ix16 = sb("ix16", [128, 16, 16], U32, ph)
        ixf = sb("ixf", [128, 16, 16], F32, ph)
        cand = sb("cand", [128, 8, 256], F32, ph)
        cand2 = tkbuf[:].rearrange("p (a b) -> p a b", b=256)
        vals = sb("vals", [128, 8, 16], F32, ph)
        posu = sb("posu", [128, 8, 16], U32, ph)
        posf = sb("posf", [128, 128], F32, ph)
        rf = sb("rf", [128, 128], F32, ph)
        cf = sb("cf", [128, 128], F32, ph)
        oh = tkbuf[:].rearrange("p (a b) -> p a b", b=16)
        If = sb("If", [128, 128], F32, ph)
        Jf = sb("Jf", [128, 128], F32, ph)
        idf = sb("idf", [128, 128], F32, ph)
        ids_r = Ring([sb("ids%d" % i, [128, 128], I32, ph) for i in range(2)], "ids")
        ev8 = sb("ev8", [128, 8, 16], F32, ph)
        z8 = sb("z8", [128, 8], F32, ph)
        g_r = Ring([sb("gk%d" % i, [128, 128], F32, ph) for i in range(2)], "gk")
        dots = sb("dots", [128, 128], F32, ph)
        wk_r = Ring([sb("wk%d" % i, [128, 128], F32, ph) for i in range(2)], "wk")
        mkpsum(ph, 0, 4, 0)
        gbuf_r = Ring([sb("gb%d" % i, [128, 2 * D], BF16, ph) for i in range(8)], "gb")
        junk_r = Ring([sb("junk%d" % i, [128, D // 2], BF16, ph) for i in range(2)], "junk")
        pr_r = Ring([sb("prd%d" % i, [128, D // 2], BF16, ph) for i in range(3)], "prd")
        jk2_r = Ring([sb("jk2_%d" % i, [128, D // 2], BF16, ph) for i in range(2)], "jk2")
        dots2 = sb("dots2", [128, 128], F32, ph)
        dg_r = Ring([sb("dgf%d" % i, [128, 128], BF16, ph) for i in range(4)], "dgf")
        dg1_r = Ring([sb("dgg%d" % i, [128, 128], F32, ph) for i in range(4)], "dgg")
        ak = sb("ak", [128, 128], F32, ph)
        wkt = sb("wkt", [128, 128], F32, ph)
        pre_r = Ring([sb("pre%d" % i, [128, D], F32, ph) for i in range(1)], "pre")
        o_r = Ring([sb("of%d" % i, [128, D], F32, ph) for i in range(1)], "of")
        colb = [Buf("col%d" % i) for i in range(4)]
        h1b_r = Ring([sb("h1bf%d" % i, [128, D], BF16, ph) for i in range(1)], "h1bf")
        B_tk, B_junk, B_dots = Buf("topk"), Buf("junk"), Buf("dots")
        TK = [B_tk]
        RES = {}

        def topk_gen(ob):
                sf, bsf = s_r.next()
                S.dma("sp", lambda e: e.dma_start(
                    out=sf[:], in_=s_d[ob * 128:(ob + 1) * 128, :].rearrange("p (a b) -> p a b", b=128)),
                    B_s, bsf, bsf)
                yield
                for hp_ in range(16):
                    S.op("dve", lambda e: e.max(out=m16[:, hp_, 0:8], in_=sf[:, hp_, :]), [bsf], TK)
                    S.op("dve", lambda e: e.match_replace(out=s2[:, hp_, :], in_to_replace=m16[:, hp_, 0:8],
                                                          in_values=sf[:, hp_, :], imm_value=-1e30), [bsf] + TK, TK)
                    S.op("dve", lambda e: e.max(out=m16[:, hp_, 8:16], in_=s2[:, hp_, :]), TK, TK)
                    S.op("dve", lambda e: e.max_index(out=ix16[:, hp_, 0:8], in_max=m16[:, hp_, 0:8],
                                                      in_values=sf[:, hp_, :]), [bsf] + TK, TK)
                    S.op("dve", lambda e: e.max_index(out=ix16[:, hp_, 8:16], in_max=m16[:, hp_, 8:16],
                                                      in_values=sf[:, hp_, :]), [bsf] + TK, TK)
                    yield
                S.op("dve", lambda e: e.tensor_copy(out=ixf[:], in_=ix16[:]), TK, TK)
                yield
                m4 = m16[:].rearrange("p (h s) k -> p h s k", s=2)
                ix4 = ixf[:].rearrange("p (h s) k -> p h s k", s=2)
                for h in range(8):
                    S.op("dve", lambda e: e.tensor_tensor(
                        out=cand[:, h, :].rearrange("p (r c) -> p r c", c=16),
                        in0=m4[:, h, 0, :].unsqueeze(2).to_broadcast([128, 16, 16]),
                        in1=m4[:, h, 1, :].unsqueeze(1).to_broadcast([128, 16, 16]), op=ALU.add), TK, TK)
                    yield
                for h in range(8):
                    S.op("dve", lambda e: e.max(out=vals[:, h, 0:8], in_=cand[:, h, :]), TK, TK)
                    S.op("dve", lambda e: e.match_replace(out=cand2[:, h, :], in_to_replace=vals[:, h, 0:8],
                                                          in_values=cand[:, h, :], imm_value=-1e30), TK, TK)
                    S.op("dve", lambda e: e.max(out=vals[:, h, 8:16], in_=cand2[:, h, :]), TK, TK)
                    S.op("dve", lambda e: e.max_index(out=posu[:, h, 0:8], in_max=vals[:, h, 0:8],
                                                      in_values=cand[:, h, :]), TK, TK)
                    S.op("dve", lambda e: e.max_index(out=posu[:, h, 8:16], in_max=vals[:, h, 8:16],
                                                      in_values=cand[:, h, :]), TK, TK)
                    yield
                S.op("dve", lambda e: e.tensor_copy(out=posf[:], in_=posu[:].rearrange("p h k -> p (h k)")), TK, TK)
                yield
                S.op("dve", lambda e: e.tensor_tensor(
                    out=oh[:], in0=posf[:].unsqueeze(2).to_broadcast([128, 128, 16]),
                    in1=thr16[:].unsqueeze(1).to_broadcast([128, 128, 16]), op=ALU.is_ge), TK + CONST, TK)
                yield
                S.op("dve", lambda e: e.reduce_sum(out=rf[:], in_=oh[:], axis=AX.X), TK, TK)
                yield
                S.op("dve", lambda e: e.scalar_tensor_tensor(out=cf[:], in0=rf[:], scalar=-16.0, in1=posf[:],
                                                             op0=ALU.mult, op1=ALU.add), TK, TK)
                yield
                for (src_rc, side, dstI) in ((rf, 0, If), (cf, 1, Jf)):
                    S.op("dve", lambda e: e.tensor_tensor(
                        out=oh[:], in0=src_rc[:].unsqueeze(2).to_broadcast([128, 128, 16]),
                        in1=iota16[:].unsqueeze(1).to_broadcast([128, 128, 16]), op=ALU.is_equal), TK + CONST, TK)
                    for h in range(8):
                        S.op("dve", lambda e: e.tensor_tensor(
                            out=oh[:, h * 16:(h + 1) * 16, :], in0=oh[:, h * 16:(h + 1) * 16, :],
                            in1=ix4[:, h, side, :].unsqueeze(1).to_broadcast([128, 16, 16]), op=ALU.mult), TK, TK)
                    yield
                    S.op("dve", lambda e: e.reduce_sum(out=dstI[:], in_=oh[:], axis=AX.X), TK, TK)
                    yield
                S.op("dve", lambda e: e.scalar_tensor_tensor(out=idf[:], in0=If[:], scalar=128.0, in1=Jf[:],
                                                             op0=ALU.mult, op1=ALU.add), TK, TK)
                yield
                ids, bids = ids_r.next()
                S.op("dve", lambda e: e.tensor_copy(out=ids[:], in_=idf[:]), TK, [bids])
                yield
                S.op("dve", lambda e: e.tensor_tensor(
                    out=ev8[:], in0=vals[:], in1=vals[:, :, 0:1].to_broadcast([128, 8, 16]), op=ALU.subtract),
                    TK, TK)
                yield
                S.op("act", lambda e: e.activation(out=ev8[:], in_=ev8[:], func=AF.Exp), TK, TK)
                yield
                S.op("dve", lambda e: e.reduce_sum(out=z8[:], in_=ev8[:], axis=AX.X), TK, TK)
                yield
                S.op("dve", lambda e: e.reciprocal(out=z8[:], in_=z8[:]), TK, TK)
                yield
                gk, bgk = g_r.next()
                S.op("dve", lambda e: e.tensor_tensor(
                    out=gk[:].rearrange("p (h k) -> p h k", k=16), in0=ev8[:],
                    in1=z8[:].unsqueeze(2).to_broadcast([128, 8, 16]), op=ALU.mult), TK, [bgk])
                yield
                RES[ob] = (ids, bids, gk, bgk)
                yield

        for _ in topk_gen(0):
            pass
        for ob in range(NB):
            ids, bids, gk, bgk = RES.pop(ob)
            nxt = topk_gen(ob + 1) if ob + 1 < NB else None
            h1f, bh1f = h1_r.next()
            S.dma("sp", lambda e: e.dma_start(out=h1f[:], in_=h1_d[ob * 128:(ob + 1) * 128, :]),
                  B_h1, bh1f, bh1f)
            h1b, bh1b = h1b_r.next()
            S.op("act", lambda e: e.activation(out=h1b[:], in_=h1f[:], func=AF.Copy), [bh1f], [bh1b])
            accs = [P["o"].next() for _ in range(4)]
            for k in range(128):
                gb, bgb = gbuf_r.next()
                S.dma("pool", lambda e: e.indirect_dma_start(
                    out=gb[:], out_offset=None, in_=uv_bf,
                    in_offset=bass.IndirectOffsetOnAxis(ap=ids[:, k:k + 1], axis=0)),
                    B_uv, bgb, bgb, extra_reads=[bids])
                jk, bjk = junk_r.next()
                cb_ = colb[k % 4]
                H2 = D // 2
                S.op("dve", lambda e: e.scalar_tensor_tensor(
                    out=jk[:], in0=gb[:, 0:H2], scalar=1.0, in1=h1b[:, 0:H2], op0=ALU.mult, op1=ALU.mult,
                    accum_out=dots[:, k:k + 1]), [bgb, bh1b], [bjk, cb_])
                pr, bpr = pr_r.next()
                S.op("dve", lambda e: e.tensor_tensor(out=pr[:], in0=gb[:, H2:D], in1=h1b[:, H2:D], op=ALU.mult),
                     [bgb, bh1b], [bpr])
                jk2, bjk2 = jk2_r.next()
                S.op("act", lambda e: e.activation(out=jk2[:], in_=pr[:], func=AF.Identity,
                                                   accum_out=dots2[:, k:k + 1]), [bpr], [bjk2, cb_])
                S.op("act", lambda e: e.activation(out=ak[:, k:k + 1], in_=dots[:, k:k + 1], func=AF.Gelu,
                                                   bias=dots2[:, k:k + 1], scale=1.0), [cb_], [cb_])
                S.op("act", lambda e: e.activation(out=wkt[:, k:k + 1], in_=ak[:, k:k + 1], func=AF.Identity,
                                                   scale=gk[:, k:k + 1]), [cb_, bgk], [cb_])
                dg, bdg = dg_r.next()
                S.op("act", lambda e: e.activation(out=dg[:], in_=ident_b[:], func=AF.Identity,
                                                   scale=wkt[:, k:k + 1]), [cb_] + CONST, [bdg])
                for j in range(4):
                    po, bpo = accs[j]
                    S.op("pe", lambda e: e.matmul(po[:, 0:512], lhsT=dg[:],
                                                  rhs=gb[:, D + j * 512:D + (j + 1) * 512],
                                                  start=(k == 0), stop=(k == 127)), [bdg, bgb], [bpo])
                if nxt is not None:
                    next(nxt, None)
            if nxt is not None:
                for _ in nxt:
                    pass
            pre, bpre = pre_r.next()
            for j in range(4):
                po, bpo = accs[j]
                S.op("dve", lambda e: e.scalar_tensor_tensor(
                    out=pre[:, j * 512:(j + 1) * 512], in0=h1f[:, j * 512:(j + 1) * 512], scalar=DN_ALPHA,
                    in1=po[:, 0:512], op0=ALU.mult, op1=ALU.add), [bh1f, bpo], [bpre])
            of, bof = o_r.next()
            layer_norm_tile(pre, bpre, 128, l2g, l2b, [B_l2], of, bof)
            S.dma("pool", lambda e: e.dma_start(out=out[ob * 128:(ob + 1) * 128, :], in_=of[:]),
                  bof, B_out, bof, append=True)
        S.barrier()

    S.final_wait("sp", [B_out])
    print("kernel build: %d instructions, %d dma sems" % (S.ninstr, S.nsem))
    stack.close()
    return nc


def host_inputs(SEQ, inputs):
    AT = SEQ // 128
    NTOK = AT * 128
    f32 = np.float32
    x = np.asarray(inputs["x"], f32)
    meta = np.ascontiguousarray(np.asarray(inputs["meta_tokens"], f32))
    b_in = np.asarray(inputs["b_in"], f32)[0]
    b_fm = np.zeros((128, 113), f32)
    cols = np.concatenate([b_in[OFF_A:OFF_V], b_in[OFF_GC:N_IN]])
    b_fm[:, :96] = cols.reshape(96, 128).T
    bf2 = np.zeros((128, 113), f32)
    bf2[:, 0:64] = b_fm[:, 0:64]
    bf2[:, 80:112] = b_fm[:, 64:96]
    bf2[:16, 112] = b_in[OFF_F:OFF_F + 16]

    def bc(v):
        return np.ascontiguousarray(np.broadcast_to(np.asarray(v, f32).reshape(1, -1), (128, D)))

    def fm(v):
        return np.ascontiguousarray(np.asarray(v, f32).reshape(KC, 128).T)

    common = {
        "xmeta": meta,
        "lnin_g": bc(inputs["ln_in_g"]), "lnin_b": bc(inputs["ln_in_b"]),
        "w_in": np.ascontiguousarray(np.asarray(inputs["w_in"], f32)[0]),
        "b_fm": bf2, "bv_b": bc(b_in[OFF_V:OFF_F]),
        "dww": np.ascontiguousarray(np.asarray(inputs["conv_dw_w"], f32)[0].T.reshape(KC, 128, TAPS).transpose(1, 0, 2)),
        "dwb": fm(inputs["conv_dw_b"]), "cln_g": fm(inputs["conv_ln_g"]), "cln_b": fm(inputs["conv_ln_b"]),
        "w_co": np.ascontiguousarray(np.asarray(inputs["w_conv_out"], f32)[0]),
        "w_ao": np.ascontiguousarray(np.asarray(inputs["w_attn_out"], f32)[0]),
        "w_o": np.ascontiguousarray(np.asarray(inputs["w_out"], f32)[0]),
        "ln1_g": bc(inputs["ln1_g"]), "ln1_b": bc(inputs["ln1_b"]),
        "w_q": np.ascontiguousarray(np.asarray(inputs["peer_w_q"], f32)[0]),
        "subk": np.ascontiguousarray(np.asarray(inputs["peer_subkeys"], f32)[0].reshape(16, 128, 128)),
        "tab_u": np.ascontiguousarray(np.asarray(inputs["peer_u"], f32)[0]),
        "tab_v": np.ascontiguousarray(np.asarray(inputs["peer_v"], f32)[0]),
        "ln2_g": bc(inputs["ln2_g"]), "ln2_b": bc(inputs["ln2_b"]),
        "ident": np.eye(128, dtype=f32),
        "tri": np.triu(np.ones((128, 128), f32)),
        "iota16": np.ascontiguousarray(np.broadcast_to(np.arange(16, dtype=f32), (128, 16))),
    }
    maps = []
    for core in range(8):
        b, p = core // 2, core % 2
        xa = np.zeros((NTOK, D), f32)
        valid = np.ones((NTOK,), f32)
        kb0 = np.zeros((128, 1), f32)
        hmask = np.ones((128, 32), f32)
        if p == 0:
            xa[128:] = x[b, :NTOK - 128]
            xa[112:128] = meta
            valid[:128] = 0.0
            kb0[:] = NEG
            hmask[:, :16] = 0.0
        else:
            xa[:] = x[b, :NTOK]
        m = dict(common)
        m.update({"xa": xa, "valid16": np.ascontiguousarray(np.broadcast_to(valid, (16, NTOK))),
                  "kb0": kb0, "hmask": hmask})
        maps.append(m)
    return maps


def assemble(SEQ, results):
    AT = SEQ // 128
    NB = AT // 2
    out = np.zeros((4, SEQ, D), np.float32)
    for core in range(8):
        b, p = core // 2, core % 2
        o = results[core]["out"].reshape(NB, 128, D)
        for i in range(NB):
            rt = 2 * i + p
            out[b, rt * 128:(rt + 1) * 128] = o[i]
    return out


_NC_CACHE = {}


def kernel(**inputs):
    SEQ = int(np.asarray(inputs["x"]).shape[1])
    if SEQ not in _NC_CACHE:
        _NC_CACHE[SEQ] = build(SEQ)
    nc = _NC_CACHE[SEQ]
    maps = host_inputs(SEQ, inputs)
    res = run_bass_kernel_spmd(nc, maps, core_ids=list(range(8)))
    return assemble(SEQ, res.results)
```

```python
import contextlib
import numpy as np
import concourse.bass as bass
import concourse.mybir as mybir
from concourse.bass_utils import run_bass_kernel_spmd

F32 = mybir.dt.float32
BF16 = mybir.dt.bfloat16
U32 = mybir.dt.uint32
I32 = mybir.dt.int32
ALU = mybir.AluOpType
AF = mybir.ActivationFunctionType
AX = mybir.AxisListType

D = 2048
KC = 16
NH = 16
HD = 128
N_META = 16
TAPS = 31
N_IN = 14352
OFF_A, OFF_G, OFF_Q, OFF_K, OFF_V, OFF_F, OFF_GC, OFF_GA = 0, 2048, 4096, 6144, 8192, 10240, 10256, 12304
LN_EPS = 1e-5
DN_ALPHA = 2.0 ** 0.25
NEXP = 16384
NEG = -30000.0


class Buf:
    __slots__ = ("name", "w", "r", "sem")

    def __init__(self, name):
        self.name = name
        self.w = {}
        self.r = {}
        self.sem = None


class Sched:
    def __init__(self, nc, stack):
        self.nc = nc
        self.stack = stack
        self.names = ["pe", "act", "dve", "pool", "sp"]
        self.eng = {"pe": nc.tensor, "act": nc.scalar, "dve": nc.vector, "pool": nc.gpsimd, "sp": nc.sync}
        self.esem = {e: stack.enter_context(nc.semaphore("s_" + e)) for e in self.names}
        self.ecount = {e: 0 for e in self.names}
        self.waited = {e: {} for e in self.names}
        self.semcount = {}
        self.nsem = 0
        self.ninstr = 0

    def _wait(self, eng, deps):
        wd = self.waited[eng]
        for sem, val in deps.items():
            if eng == "pe" and sem is self.esem["pe"]:
                continue
            if wd.get(sem, 0) >= val:
                continue
            wd[sem] = val
            self.eng[eng].wait_ge(sem, val)

    @staticmethod
    def _merge(dst, src):
        for s, v in src.items():
            if dst.get(s, 0) < v:
                dst[s] = v

    def op(self, eng, fn, reads=(), writes=()):
        deps = {}
        for b in reads:
            self._merge(deps, b.w)
        for b in writes:
            self._merge(deps, b.w)
            self._merge(deps, b.r)
        self._wait(eng, deps)
        self.ecount[eng] += 1
        self.ninstr += 1
        sem = self.esem[eng]
        tok = {sem: self.ecount[eng]}
        fn(self.eng[eng]).then_inc(sem, 1)
        for b in reads:
            self._merge(b.r, tok)
        for b in writes:
            b.w = dict(tok)
            b.r = {}

    def _slot_sem(self, b):
        if b.sem is None:
            b.sem = self.stack.enter_context(self.nc.semaphore("d%d" % self.nsem))
            self.nsem += 1
            self.semcount[b.sem] = 0
        return b.sem

    def dma(self, q, fn, src, dst, slot, extra_reads=(), append=False):
        deps = {}
        self._merge(deps, src.w)
        for b in extra_reads:
            self._merge(deps, b.w)
        if not append:
            self._merge(deps, dst.w)
            self._merge(deps, dst.r)
        self._wait(q, deps)
        sem = self._slot_sem(slot)
        self.semcount[sem] += 16
        self.ninstr += 1
        tok = {sem: self.semcount[sem]}
        fn(self.eng[q]).then_inc(sem, 16)
        self._merge(src.r, tok)
        for b in extra_reads:
            self._merge(b.r, tok)
        if append:
            self._merge(dst.w, tok)
        else:
            dst.w = dict(tok)
            dst.r = {}

    def final_wait(self, eng, bufs):
        deps = {}
        for b in bufs:
            self._merge(deps, b.w)
        self._wait(eng, deps)

    def barrier(self):
        deps = {self.esem[e]: self.ecount[e] for e in self.names if self.ecount[e] > 0}
        for sem, v in self.semcount.items():
            if v > 0:
                deps[sem] = v
        for e in self.names:
            self._wait(e, dict(deps))


class Ring:
    def __init__(self, tiles, name):
        self.tiles = tiles
        self.bufs = [Buf("%s%d" % (name, i)) for i in range(len(tiles))]
        self.i = -1

    def next(self):
        self.i = (self.i + 1) % len(self.tiles)
        return self.tiles[self.i], self.bufs[self.i]


def build(SEQ, dbg=False, stop_after="F"):
    AT = SEQ // 128
    NB = AT // 2
    NG = AT // 4
    NTOK = AT * 128
    NOWN = NB * 128
    LK = N_META + NTOK
    NOG = NOWN // 512
    assert AT % 8 == 0
    PH = "0 AB B2 C D E1 E2 F".split()
    nph = PH.index(stop_after)

    nc = bass.Bass("TRN2", target_bir_lowering=False)
    stack = contextlib.ExitStack()

    def din(name, shape, dt=F32):
        return nc.dram_tensor(name, list(shape), dt, kind="ExternalInput").ap()

    def dscr(name, shape, dt):
        return nc.dram_tensor(name, list(shape), dt,
                              kind="ExternalOutput" if dbg else "Internal").ap()

    def dint(name, shape, dt):
        return nc.dram_tensor(name, list(shape), dt, kind="Internal").ap()

    xa = din("xa", [NTOK, D])
    xmeta = din("xmeta", [N_META, D])
    valid16 = din("valid16", [16, NTOK])
    kb0 = din("kb0", [128, 1])
    hmask = din("hmask", [128, 32])
    lnin_g = din("lnin_g", [128, D])
    lnin_b = din("lnin_b", [128, D])
    w_in = din("w_in", [D, N_IN])
    b_fm = din("b_fm", [128, 113])
    bv_b = din("bv_b", [128, D])
    dww = din("dww", [128, KC, TAPS])
    dwb = din("dwb", [128, KC])
    cln_g = din("cln_g", [128, KC])
    cln_b = din("cln_b", [128, KC])
    w_co = din("w_co", [D, D])
    w_ao = din("w_ao", [D, D])
    w_o = din("w_o", [D, D])
    ln1_g = din("ln1_g", [128, D])
    ln1_b = din("ln1_b", [128, D])
    w_q = din("w_q", [D, D])
    subk = din("subk", [16, 128, 128])
    tab_u = din("tab_u", [NEXP, D])
    tab_v = din("tab_v", [NEXP, D])
    ln2_g = din("ln2_g", [128, D])
    ln2_b = din("ln2_b", [128, D])
    ident_in = din("ident", [128, 128])
    tri_in = din("tri", [128, 128])
    iota16_in = din("iota16", [128, 16])
    out = nc.dram_tensor("out", [NOWN, D], F32, kind="ExternalOutput").ap()

    win_bf = dint("win_bf", [KC, 128, N_IN], BF16)
    wco_bf = dint("wco_bf", [KC, 128, D], BF16)
    wao_bf = dint("wao_bf", [KC, 128, D], BF16)
    wo_bf = dint("wo_bf", [KC, 128, D], BF16)
    wq_bf = dint("wq_bf", [KC, 128, D], BF16)
    kT_d = dscr("kT_d", [NH, 128, LK], BF16)
    v_d = dscr("v_d", [LK, D], BF16)
    qT_d = dscr("qT_d", [NH, 128, NOWN], BF16)
    uT_d = dscr("uT_d", [KC, 128, NB, 160], BF16)
    gcT_d = dscr("gcT_d", [KC, 128, NOWN], BF16)
    gaT_d = dscr("gaT_d", [KC, 128, NOWN], BF16)
    logf_d = dscr("logf_d", [16, LK], F32)
    cT_d = dscr("cT_d", [16, LK], F32)
    ycg_d = dscr("ycg_d", [KC, 128, NOWN], BF16)
    o_d = dscr("o_d", [NOWN, D], BF16)
    h0_d = dscr("h0_d", [NOWN, D], F32)
    h1_d = dscr("h1_d", [NOWN, D], F32)
    s_d = dscr("s_d", [NOWN, D], F32)
    uv_bf = dint("uv_bf", [NEXP, 2 * D], BF16)

    S = Sched(nc, stack)
    B_ext = Buf("ext")

    def sb(name, shape, dt, st=None):
        return (st or stack).enter_context(nc.sbuf_tensor("t_" + name, list(shape), dt))

    def ps(name, shape, dt=F32):
        return stack.enter_context(nc.psum_tensor(name, list(shape), dt))

    P = {}
    pscount = [0]

    def mkpsum(st, nf, no, nb):
        assert nf + no + nb <= 8
        pscount[0] += 1
        tag = "p%d_" % pscount[0]
        P["f"] = Ring([st.enter_context(nc.psum_tensor(tag + "f%d" % i, [128, 512], F32)) for i in range(nf)], "psf")
        P["o"] = Ring([st.enter_context(nc.psum_tensor(tag + "o%d" % i, [128, 512], F32)) for i in range(no)], "pso")
        P["b"] = Ring([st.enter_context(nc.psum_tensor(tag + "b%d" % i, [128, 1024], BF16)) for i in range(nb)], "psb")

    B_c = Buf("consts")
    ident_f = sb("ident_f", [128, 128], F32)
    ident_b = sb("ident_b", [128, 128], BF16)
    tri_b = sb("tri_b", [128, 128], BF16)
    tri_f = sb("tri_f", [128, 128], F32)
    trineg = sb("trineg", [128, 128], F32)
    iota16 = sb("iota16", [128, 16], F32)
    thr16 = sb("thr16", [128, 16], F32)
    bfm = sb("bfm", [128, 113], F32)
    nbf = sb("nbf", [16, 1], F32)
    kb0_t = sb("kb0_t", [128, 1], F32)
    hmask_t = sb("hmask_t", [128, 32], F32)
    eps_t = sb("eps_t", [128, 1], F32)
    one_t = sb("one_t", [128, 1], F32)
    onesm = sb("onesm", [128, 128], BF16)
    dww_t = sb("dww_t", [128, KC, TAPS], F32)
    dwb_t = sb("dwb_t", [128, KC], F32)
    clng_t = sb("clng_t", [128, KC], F32)
    clnb_t = sb("clnb_t", [128, KC], F32)
    negck = sb("negck", [128, AT, NH], F32)
    negckm = sb("negckm", [16, NH], F32)
    st_r = Ring([sb("lnst%d" % i, [128, 4, 6], F32) for i in range(2)], "lnst")
    mv_r = Ring([sb("lnmv%d" % i, [128, 4], F32) for i in range(2)], "lnmv")

    def cload(dst, src):
        S.dma("sp", lambda e: e.dma_start(out=dst, in_=src), B_ext, B_c, B_c)

    cload(ident_f[:], ident_in)
    cload(tri_f[:], tri_in)
    cload(iota16[:], iota16_in)
    cload(bfm[:], b_fm)
    cload(kb0_t[:], kb0)
    cload(hmask_t[:], hmask)
    cload(dww_t[:], dww)
    cload(dwb_t[:], dwb)
    cload(clng_t[:], cln_g)
    cload(clnb_t[:], cln_b)
    B_c2 = Buf("consts2")
    S.op("dve", lambda e: e.tensor_copy(out=ident_b[:], in_=ident_f[:]), [B_c], [B_c2])
    S.op("dve", lambda e: e.tensor_copy(out=tri_b[:], in_=tri_f[:]), [B_c], [B_c2])
    S.op("dve", lambda e: e.memset(eps_t[:], LN_EPS), [], [B_c2])
    S.op("dve", lambda e: e.memset(one_t[:], 1.0), [], [B_c2])
    S.op("dve", lambda e: e.memset(onesm[:], 1.0 / D), [], [B_c2])
    S.op("dve", lambda e: e.tensor_scalar(out=nbf[:], in0=bfm[0:16, 112:113], scalar1=-1.0, scalar2=None,
                                          op0=ALU.mult), [B_c], [B_c2])
    S.op("dve", lambda e: e.tensor_scalar(out=thr16[:], in0=iota16[:], scalar1=16.0, scalar2=16.0,
                                          op0=ALU.mult, op1=ALU.add), [B_c], [B_c2])
    S.op("dve", lambda e: e.tensor_scalar(out=trineg[:], in0=tri_f[:], scalar1=-1.0, scalar2=-NEG,
                                          op0=ALU.add, op1=ALU.mult), [B_c], [B_c2])
    CONST = [B_c, B_c2]

    (B_win, B_wco, B_wao, B_wo, B_wq, B_kT, B_v, B_qT, B_uT, B_gc, B_ga, B_lf, B_cT, B_h0,
     B_ycg, B_oT, B_h1, B_s, B_out, B_nck, B_uv) = (Buf(n) for n in (
         "win wco wao wo wq kT v qT uT gc ga lf cT h0 ycg oT h1 s out nck uv").split())

    def layer_norm_tile(xt, bx, rows, g_t, b_t, gb_bufs, out_f32, b_out):
        st, bst = st_r.next()
        mv, bmv = mv_r.next()
        for j in range(4):
            S.op("dve", lambda e, j=j: e.bn_stats(out=st[:rows, j, :], in_=xt[:rows, j * 512:(j + 1) * 512]),
                 [bx], [bst])
        S.op("dve", lambda e: e.bn_aggr(out=mv[:rows, 0:2], in_=st[:rows].rearrange("p a b -> p (a b)")),
             [bst], [bmv])
        S.op("act", lambda e: e.activation(out=mv[:rows, 2:3], in_=mv[:rows, 1:2], func=AF.Sqrt,
                                           bias=eps_t[:rows, :], scale=1.0), [bmv] + CONST, [bmv])
        S.op("dve", lambda e: e.reciprocal(out=mv[:rows, 3:4], in_=mv[:rows, 2:3]), [bmv], [bmv])
        S.op("dve", lambda e: e.tensor_scalar(out=out_f32[:rows], in0=xt[:rows], scalar1=mv[:rows, 0:1],
                                              scalar2=mv[:rows, 3:4], op0=ALU.subtract, op1=ALU.mult),
             [bx, bmv], [b_out])
        S.op("pool", lambda e: e.tensor_tensor(out=out_f32[:rows], in0=out_f32[:rows], in1=g_t[:rows],
                                               op=ALU.mult), [b_out] + gb_bufs, [b_out])
        S.op("dve", lambda e: e.tensor_tensor(out=out_f32[:rows], in0=out_f32[:rows], in1=b_t[:rows],
                                              op=ALU.add), [b_out] + gb_bufs, [b_out])

    def transpose_to(xb, bxb, rows, dstT, bdst, col0):
        for half in range(2):
            pt, bpt = P["b"].next()
            for j in range(8):
                kc = half * 8 + j
                S.op("pe", lambda e, kc=kc, j=j, pt=pt: e.transpose(
                    out=pt[:, j * 128:j * 128 + rows], in_=xb[:rows, kc * 128:(kc + 1) * 128],
                    identity=ident_b[:rows, :rows]), [bxb] + CONST, [bpt])
            S.op("act", lambda e, half=half, pt=pt: e.activation(
                out=dstT[:, half * 8:(half + 1) * 8, col0:col0 + rows],
                in_=pt[:].rearrange("p (j t) -> p j t", t=128)[:, :, 0:rows], func=AF.Copy),
                [bpt], [bdst])

    def mm_acc(pt_ap, bpt, w, bw, wc, rhsT, brhs, c0, n):
        for kc in range(KC):
            S.op("pe", lambda e, kc=kc: e.matmul(
                pt_ap, lhsT=w[:, kc, wc * 128:(wc + 1) * 128], rhs=rhsT[:, kc, c0:c0 + n],
                start=(kc == 0), stop=(kc == KC - 1)), [bw, brhs], [bpt])

    def mm_own(pt, bpt, w, bw, wc, rhsT, brhs, c0, n):
        for kc in range(KC):
            S.op("pe", lambda e: e.matmul(
                pt[:, 0:2 * n].rearrange("p (a b) -> p a b", b=n), lhsT=w[:, kc, wc * 128:(wc + 1) * 128],
                rhs=rhsT[:, kc, :].rearrange("p (a b) -> p a b", b=256)[:, :, c0:c0 + n],
                start=(kc == 0), stop=(kc == KC - 1)), [bw, brhs], [bpt])

    def load_w(w_r, src_bf, bsrc, col0, ncols=512):
        w, bw = w_r.next()
        S.dma("sp", lambda e: e.dma_start(
            out=w[:, :, 0:ncols], in_=src_bf[:, :, col0:col0 + ncols].rearrange("k p c -> p k c")),
            bsrc, bw, bw)
        return w, bw

    with contextlib.ExitStack() as ph:
        stage_r = Ring([sb("cst%d" % i, [128, 2048], BF16, ph) for i in range(3)], "cst")

        def precast(src, dst, ncols, dbuf):
            for kc in range(KC):
                for c0 in range(0, ncols, 2048):
                    cw = min(2048, ncols - c0)
                    t, b = stage_r.next()
                    S.dma("pool", lambda e: e.dma_start(
                        out=t[:, 0:cw], in_=src[kc * 128:(kc + 1) * 128, c0:c0 + cw]), B_ext, b, b)
                    S.dma("sp", lambda e: e.dma_start(
                        out=dst[kc, :, c0:c0 + cw], in_=t[:, 0:cw]), b, dbuf, b, append=True)

        precast(w_in, win_bf, N_IN, B_win)
        precast(w_co, wco_bf, D, B_wco)
        if nph < 4:
            precast(w_ao, wao_bf, D, B_wao)
            precast(w_o, wo_bf, D, B_wo)
            precast(w_q, wq_bf, D, B_wq)
        S.barrier()

    if nph >= 1:
      with contextlib.ExitStack() as ph:
        lng_t = sb("lng_t", [128, D], F32, ph)
        mkpsum(ph, 5, 0, 2)
        lnb_t = sb("lnb_t", [128, D], F32, ph)
        bvb_t = sb("bvb_t", [128, D], F32, ph)
        B_ln = Buf("lnconst")
        for dst_, src_ in ((lng_t, lnin_g), (lnb_t, lnin_b), (bvb_t, bv_b)):
            S.dma("sp", lambda e: e.dma_start(out=dst_[:], in_=src_), B_ext, B_ln, B_ln)
        x_r = Ring([sb("xt%d" % i, [128, D], F32, ph) for i in range(2)], "xt")
        xn_r = Ring([sb("xn%d" % i, [128, D], F32, ph) for i in range(2)], "xn")
        xb_r = Ring([sb("xb%d" % i, [128, D], BF16, ph) for i in range(2)], "xb")
        h0T_r = Ring([sb("h0T%d" % i, [128, KC, 512], BF16, ph) for i in range(2)], "h0T")
        w_r = Ring([sb("wst%d" % i, [128, KC, 512], BF16, ph) for i in range(2)], "wst")
        wf_t = sb("wf_t", [128, KC, 16], BF16, ph)
        B_wf = Buf("wf")
        S.dma("sp", lambda e: e.dma_start(out=wf_t[:], in_=win_bf[:, :, OFF_F:OFF_F + 16].rearrange("k p c -> p k c")),
              B_win, B_wf, B_wf)
        sig_r = Ring([sb("sig%d" % i, [128, 4, 320], F32, ph) for i in range(2)], "sig")
        ev_r = Ring([sb("ev%d" % i, [128, 4, 512], BF16, ph) for i in range(3)], "ev")
        lf_r = Ring([sb("lft%d" % i, [16, 512], F32, ph) for i in range(2)], "lft")

        def group_proj(h0T, bh, gi, ntok, meta):
            ktok0 = 0 if meta else N_META + gi * 512
            own = [] if meta else [(2 * gi, 1), (2 * gi + 1, 3)]
            if not meta:
                for j in range(4):
                    wg, bwg = load_w(w_r, win_bf, B_win, OFF_G + j * 512)
                    sg, bsg = sig_r.next()
                    for c in range(4):
                        pt, bpt = P["f"].next()
                        mm_own(pt, bpt, wg, bwg, c, h0T, bh, 96, 160)
                        bc = 16 + j * 4 + c
                        S.op("act", lambda e: e.activation(
                            out=sg[:, c, :], in_=pt[:, 0:320], func=AF.Sigmoid,
                            bias=bfm[:, bc:bc + 1], scale=1.0), [bpt] + CONST, [bsg])
                    wa, bwa = load_w(w_r, win_bf, B_win, OFF_A + j * 512)
                    ev, bev = ev_r.next()
                    for c in range(4):
                        pt, bpt = P["f"].next()
                        mm_own(pt, bpt, wa, bwa, c, h0T, bh, 96, 160)
                        bc = j * 4 + c
                        S.op("dve", lambda e: e.scalar_tensor_tensor(
                            out=ev[:, c, 0:320], in0=pt[:, 0:320], scalar=bfm[:, bc:bc + 1],
                            in1=sg[:, c, :], op0=ALU.add, op1=ALU.mult), [bpt, bsg] + CONST, [bev])
                    if gi == 0:
                        S.op("pool", lambda e: e.tensor_tensor(
                            out=ev[:, :, 0:32], in0=ev[:, :, 0:32],
                            in1=hmask_t[:].unsqueeze(1).to_broadcast([128, 4, 32]), op=ALU.mult),
                            [bev] + CONST, [bev])
                    for (ob, _), o in zip(own, (0, 160)):
                        S.dma("pool", lambda e: e.dma_start(
                            out=uT_d[j * 4:(j + 1) * 4, :, ob, :].rearrange("c p t -> p c t"),
                            in_=ev[:, :, o:o + 160]), bev, B_uT, bev, append=True)
                for (off, dst, bdst, func, bcol0) in ((OFF_Q, qT_d, B_qT, AF.Identity, 32),
                                                       (OFF_GC, gcT_d, B_gc, AF.Sigmoid, 80),
                                                       (OFF_GA, gaT_d, B_ga, AF.Sigmoid, 96)):
                    for j in range(4):
                        w, bw = load_w(w_r, win_bf, B_win, off + j * 512)
                        ev, bev = ev_r.next()
                        for c in range(4):
                            pt, bpt = P["f"].next()
                            mm_own(pt, bpt, w, bw, c, h0T, bh, 128, 128)
                            bc = bcol0 + j * 4 + c
                            S.op("act", lambda e: e.activation(
                                out=ev[:, c, 0:256], in_=pt[:, 0:256], func=func,
                                bias=bfm[:, bc:bc + 1], scale=1.0), [bpt] + CONST, [bev])
                        S.dma("pool", lambda e: e.dma_start(
                            out=dst[j * 4:(j + 1) * 4, :, gi * 256:(gi + 1) * 256].rearrange("c p t -> p c t"),
                            in_=ev[:, :, 0:256]), bev, bdst, bev, append=True)
            for j in range(4):
                w, bw = load_w(w_r, win_bf, B_win, OFF_K + j * 512)
                ev, bev = ev_r.next()
                for c in range(4):
                    pt, bpt = P["f"].next()
                    mm_acc(pt[:, 0:ntok], bpt, w, bw, c, h0T, bh, 0, ntok)
                    bc = 48 + j * 4 + c
                    S.op("dve", lambda e: e.tensor_scalar(
                        out=ev[:, c, 0:ntok], in0=pt[:, 0:ntok], scalar1=bfm[:, bc:bc + 1], scalar2=None,
                        op0=ALU.add), [bpt] + CONST, [bev])
                S.dma("pool", lambda e: e.dma_start(
                    out=kT_d[j * 4:(j + 1) * 4, :, ktok0:ktok0 + ntok].rearrange("c p t -> p c t"),
                    in_=ev[:, :, 0:ntok]), bev, B_kT, bev, append=True)
            ntile = 1 if meta else 4
            rows = ntok if meta else 128
            for j in range(4):
                w, bw = load_w(w_r, win_bf, B_win, OFF_V + j * 512)
                ev, bev = ev_r.next()
                for t in range(ntile):
                    pt, bpt = P["f"].next()
                    for kc in range(KC):
                        S.op("pe", lambda e: e.matmul(
                            pt[:rows, :], lhsT=h0T[:, kc, t * 128:t * 128 + rows], rhs=w[:, kc, :],
                            start=(kc == 0), stop=(kc == KC - 1)), [bw, bh], [bpt])
                    S.op("dve", lambda e: e.tensor_tensor(
                        out=ev[:rows, t, :], in0=pt[:rows, :], in1=bvb_t[:rows, j * 512:(j + 1) * 512],
                        op=ALU.add), [bpt, B_ln], [bev])
                if meta:
                    S.dma("pool", lambda e: e.dma_start(
                        out=v_d[0:rows, j * 512:(j + 1) * 512], in_=ev[:rows, 0, :]), bev, B_v, bev, append=True)
                else:
                    S.dma("pool", lambda e: e.dma_start(
                        out=v_d[ktok0:ktok0 + 512, j * 512:(j + 1) * 512].rearrange("(t p) c -> p t c", p=128),
                        in_=ev[:, :, :]), bev, B_v, bev, append=True)
            pt, bpt = P["f"].next()
            for kc in range(KC):
                S.op("pe", lambda e: e.matmul(
                    pt[:16, 0:ntok], lhsT=wf_t[:, kc, :], rhs=h0T[:, kc, 0:ntok],
                    start=(kc == 0), stop=(kc == KC - 1)), [B_wf, bh], [bpt])
            lt, blt = lf_r.next()
            S.op("act", lambda e: e.activation(out=lt[:, 0:ntok], in_=pt[:16, 0:ntok], func=AF.Exp,
                                               bias=nbf[:, :], scale=-1.0), [bpt] + CONST, [blt])
            S.op("act", lambda e: e.activation(out=lt[:, 0:ntok], in_=lt[:, 0:ntok], func=AF.Ln,
                                               bias=one_t[:16, :], scale=1.0), [blt] + CONST, [blt])
            S.op("dve", lambda e: e.tensor_scalar(out=lt[:, 0:ntok], in0=lt[:, 0:ntok],
                                                  scalar1=-1.0, scalar2=None, op0=ALU.mult), [blt], [blt])
            S.dma("pool", lambda e: e.dma_start(out=logf_d[:, ktok0:ktok0 + ntok], in_=lt[:, 0:ntok]),
                  blt, B_lf, blt, append=True)

        def ln_in_tile(src_ap, rows, h0T, bh, col0, own_block):
            xt, bx = x_r.next()
            S.dma("sp", lambda e: e.dma_start(out=xt[:rows], in_=src_ap), B_ext, bx, bx)
            xn, bxn = xn_r.next()
            layer_norm_tile(xt, bx, rows, lng_t, lnb_t, [B_ln], xn, bxn)
            if own_block is not None:
                S.dma("pool", lambda e: e.dma_start(out=h0_d[own_block * 128:(own_block + 1) * 128, :],
                                                    in_=xn[:]), bxn, B_h0, bxn, append=True)
            xb, bxb = xb_r.next()
            S.op("act", lambda e: e.activation(out=xb[:rows], in_=xn[:rows], func=AF.Copy), [bxn], [bxb])
            transpose_to(xb, bxb, rows, h0T, bh, col0)

        h0T, bh = h0T_r.next()
        ln_in_tile(xmeta, N_META, h0T, bh, 0, None)
        group_proj(h0T, bh, 0, N_META, True)
        for gi in range(NG):
            h0T, bh = h0T_r.next()
            for t in range(4):
                at = gi * 4 + t
                ln_in_tile(xa[at * 128:(at + 1) * 128, :], 128, h0T, bh, t * 128,
                           (at // 2) if (at % 2 == 1) else None)
            group_proj(h0T, bh, gi, 512, False)
        S.barrier()

    if nph >= 2:
      with contextlib.ExitStack() as ph:
        CH = 2048
        mkpsum(ph, 4, 0, 0)
        zeros16 = sb("zeros16", [16, CH], F32, ph)
        B_z16 = Buf("z16")
        S.op("pool", lambda e: e.memset(zeros16[:], 0.0), [], [B_z16])
        lf_r2 = Ring([sb("lfc%d" % i, [16, CH], F32, ph) for i in range(2)], "lfc")
        val_r = Ring([sb("val%d" % i, [16, CH], F32, ph) for i in range(2)], "val")
        ct_r = Ring([sb("ctc%d" % i, [16, CH], F32, ph) for i in range(2)], "ctc")
        lfm, blfm = lf_r2.next()
        S.dma("sp", lambda e: e.dma_start(out=lfm[:, 0:N_META], in_=logf_d[:, 0:N_META]), B_lf, blfm, blfm)
        ctp, bctp = ct_r.next()
        S.op("dve", lambda e: e.tensor_tensor_scan(out=ctp[:, 0:N_META], data0=lfm[:, 0:N_META],
                                                   data1=zeros16[:, 0:N_META], initial=0.0,
                                                   op0=ALU.add, op1=ALU.add), [blfm, B_z16], [bctp])
        S.dma("pool", lambda e: e.dma_start(out=cT_d[:, 0:N_META], in_=ctp[:, 0:N_META]), bctp, B_cT, bctp,
              append=True)
        pt, bpt = P["f"].next()
        S.op("pe", lambda e: e.matmul(pt[:16, 0:16], lhsT=ctp[:, 0:N_META], rhs=ident_f[0:16, 0:16],
                                      start=True, stop=True), [bctp] + CONST, [bpt])
        S.op("dve", lambda e: e.tensor_scalar(out=negckm[:, :], in0=pt[:16, 0:16], scalar1=-1.0,
                                              scalar2=None, op0=ALU.mult), [bpt], [B_nck])
        prev_last = ctp[:, N_META - 1:N_META]
        bprev = bctp
        for c0 in range(0, NTOK, CH):
            cw = min(CH, NTOK - c0)
            a0 = N_META + c0
            lf, blf = lf_r2.next()
            S.dma("sp", lambda e: e.dma_start(out=lf[:, 0:cw], in_=logf_d[:, a0:a0 + cw]), B_lf, blf, blf)
            vt, bvt = val_r.next()
            S.dma("sp", lambda e: e.dma_start(out=vt[:, 0:cw], in_=valid16[:, c0:c0 + cw]), B_ext, bvt, bvt)
            S.op("dve", lambda e: e.tensor_tensor(out=lf[:, 0:cw], in0=lf[:, 0:cw], in1=vt[:, 0:cw],
                                                  op=ALU.mult), [bvt, blf], [blf])
            ct, bct = ct_r.next()
            S.op("dve", lambda e: e.tensor_tensor_scan(
                out=ct[:, 0:cw], data0=lf[:, 0:cw], data1=zeros16[:, 0:cw], initial=prev_last,
                op0=ALU.add, op1=ALU.add), [blf, B_z16, bprev], [bct])
            S.dma("pool", lambda e: e.dma_start(out=cT_d[:, a0:a0 + cw], in_=ct[:, 0:cw]), bct, B_cT, bct,
                  append=True)
            nt = cw // 128
            pt, bpt = P["f"].next()
            for t in range(nt):
                S.op("pe", lambda e: e.matmul(
                    pt[:, t * 16:(t + 1) * 16], lhsT=ct[:, t * 128:(t + 1) * 128],
                    rhs=ident_f[0:16, 0:16], start=True, stop=True), [bct] + CONST, [bpt])
            t0 = c0 // 128
            S.op("dve", lambda e: e.tensor_scalar(
                out=negck[:, t0:t0 + nt, :].rearrange("p t h -> p (t h)"), in0=pt[:, 0:nt * 16], scalar1=-1.0,
                scalar2=None, op0=ALU.mult), [bpt], [B_nck])
            prev_last = ct[:, cw - 1:cw]
            bprev = bct
        S.op("dve", lambda e: e.tensor_scalar(out=negck[:, 0, :], in0=negck[:, 0, :], scalar1=kb0_t[:, 0:1],
                                              scalar2=None, op0=ALU.add), [B_nck] + CONST, [B_nck])
        S.barrier()

    if nph >= 3:
      with contextlib.ExitStack() as ph:
        convT = sb("convT", [128, KC, 512], F32, ph)
        mkpsum(ph, 5, 2, 0)
        cb = sb("cb", [128, KC, 512], BF16, ph)
        sq = sb("sq", [128, KC, 512], BF16, ph)
        aT = sb("aT", [128, KC, 512], BF16, ph)
        B_conv, B_cb, B_sq, B_aT = Buf("convT"), Buf("cb"), Buf("sq"), Buf("aT")
        ut_r = Ring([sb("ut%d" % i, [128, 4, 160], BF16, ph) for i in range(2)], "ut")
        dg_r = Ring([sb("dg%d" % i, [128, TAPS, 128], BF16, ph) for i in range(2)], "dg")
        w_r = Ring([sb("wstc%d" % i, [128, KC, 512], BF16, ph) for i in range(2)], "wstc")
        gc_r = Ring([sb("gct%d" % i, [128, 4, 512], BF16, ph) for i in range(2)], "gct")
        ev_r = Ring([sb("evc%d" % i, [128, 4, 512], BF16, ph) for i in range(2)], "evc")
        mean_sb = sb("mean_sb", [128, 512], F32, ph)
        rstd_sb = sb("rstd_sb", [128, 512], F32, ph)
        m2_sb = sb("m2_sb", [128, 512], F32, ph)
        B_mean, B_rstd, B_m2 = Buf("mean"), Buf("rstd"), Buf("m2")
        xc_r = Ring([sb("xc%d" % i, [128, 512], F32, ph) for i in range(3)], "xc")
        for og in range(NOG):
            pmean, bpmean = P["o"].next()
            pex2, bpex2 = P["o"].next()
            for cc in range(KC):
                ut, but = ut_r.next()
                S.dma("sp", lambda e: e.dma_start(out=ut[:], in_=uT_d[cc, :, og * 4:(og + 1) * 4, :]),
                      B_uT, but, but)
                dg, bdg = dg_r.next()
                S.op("pool", lambda e: e.tensor_tensor(
                    out=dg[:], in0=ident_b[:].unsqueeze(1).to_broadcast([128, TAPS, 128]),
                    in1=dww_t[:, cc, :].unsqueeze(2).to_broadcast([128, TAPS, 128]), op=ALU.mult),
                    CONST, [bdg])
                pt, bpt = P["f"].next()
                for tap in range(TAPS):
                    S.op("pe", lambda e: e.matmul(
                        pt[:].rearrange("p (a b) -> p a b", b=128), lhsT=dg[:, tap, :],
                        rhs=ut[:, :, 2 + tap:2 + tap + 128], start=(tap == 0), stop=(tap == TAPS - 1)),
                        [bdg, but], [bpt])
                S.op("act", lambda e: e.activation(out=convT[:, cc, :], in_=pt[:], func=AF.Identity,
                                                   bias=dwb_t[:, cc:cc + 1], scale=1.0),
                     [bpt] + CONST, [B_conv])
                S.op("dve", lambda e: e.tensor_copy(out=cb[:, cc, :], in_=convT[:, cc, :]), [B_conv], [B_cb])
                S.op("act", lambda e: e.activation(out=sq[:, cc, :], in_=pt[:], func=AF.Square,
                                                   bias=dwb_t[:, cc:cc + 1], scale=1.0),
                     [bpt] + CONST, [B_sq])
                S.op("pe", lambda e: e.matmul(pmean[:], lhsT=onesm[:], rhs=cb[:, cc, :],
                                              start=(cc == 0), stop=(cc == KC - 1)), [B_cb] + CONST, [bpmean])
                S.op("pe", lambda e: e.matmul(pex2[:], lhsT=onesm[:], rhs=sq[:, cc, :],
                                              start=(cc == 0), stop=(cc == KC - 1)), [B_sq] + CONST, [bpex2])
            S.op("act", lambda e: e.activation(out=mean_sb[:], in_=pmean[:], func=AF.Copy), [bpmean], [B_mean])
            S.op("dve", lambda e: e.tensor_tensor(out=m2_sb[:], in0=mean_sb[:], in1=mean_sb[:], op=ALU.mult),
                 [B_mean], [B_m2])
            S.op("dve", lambda e: e.tensor_tensor(out=m2_sb[:], in0=pex2[:], in1=m2_sb[:], op=ALU.subtract),
                 [bpex2, B_m2], [B_m2])
            S.op("act", lambda e: e.activation(out=m2_sb[:], in_=m2_sb[:], func=AF.Sqrt, bias=eps_t[:, :],
                                               scale=1.0), [B_m2] + CONST, [B_m2])
            S.op("dve", lambda e: e.reciprocal(out=rstd_sb[:], in_=m2_sb[:]), [B_m2], [B_rstd])
            for cc in range(KC):
                xc, bxc = xc_r.next()
                S.op("dve", lambda e: e.tensor_tensor(out=xc[:], in0=convT[:, cc, :], in1=mean_sb[:],
                                                      op=ALU.subtract), [B_conv, B_mean], [bxc])
                S.op("dve", lambda e: e.tensor_tensor(out=xc[:], in0=xc[:], in1=rstd_sb[:], op=ALU.mult),
                     [bxc, B_rstd], [bxc])
                S.op("act", lambda e: e.activation(out=aT[:, cc, :], in_=xc[:], func=AF.Silu,
                                                   bias=clnb_t[:, cc:cc + 1], scale=clng_t[:, cc:cc + 1]),
                     [bxc] + CONST, [B_aT])
            for j in range(4):
                w, bw = load_w(w_r, wco_bf, B_wco, j * 512)
                gct, bgc = gc_r.next()
                S.dma("sp", lambda e: e.dma_start(
                    out=gct[:], in_=gcT_d[j * 4:(j + 1) * 4, :, og * 512:(og + 1) * 512].rearrange("c p t -> p c t")),
                    B_gc, bgc, bgc)
                ev, bev = ev_r.next()
                for c in range(4):
                    pt, bpt = P["f"].next()
                    mm_acc(pt[:], bpt, w, bw, c, aT, B_aT, 0, 512)
                    S.op("dve", lambda e: e.tensor_tensor(out=ev[:, c, :], in0=pt[:], in1=gct[:, c, :],
                                                          op=ALU.mult), [bpt, bgc], [bev])
                S.dma("pool", lambda e: e.dma_start(
                    out=ycg_d[j * 4:(j + 1) * 4, :, og * 512:(og + 1) * 512].rearrange("c p t -> p c t"),
                    in_=ev[:]), bev, B_ycg, bev, append=True)
        S.barrier()

    if nph >= 4:
      with contextlib.ExitStack() as ph:
        LA = 3
        mkpsum(ph, LA + 1, 4, 0)
        kT_r = Ring([sb("kTh%d" % i, [128, LK], BF16, ph) for i in range(2)], "kTh")
        v_r = Ring([sb("vh%d" % i, [128, AT, 129], BF16, ph) for i in range(2)], "vh")
        vm_r = Ring([sb("vm%d" % i, [16, 129], BF16, ph) for i in range(2)], "vm")
        q_r = Ring([sb("qTh%d" % i, [128, NOWN], BF16, ph) for i in range(2)], "qTh")
        cq_r = Ring([sb("cqb%d" % i, [128, NB, 128], F32, ph) for i in range(2)], "cqb")
        o_r = Ring([sb("oTh%d" % i, [128, NB, 128], BF16, ph) for i in range(2)], "oTh")
        tmp_r = Ring([sb("atmp%d" % i, [128, 512], F32, ph) for i in range(6)], "atmp")
        pT_r = Ring([sb("pT%d" % i, [128, 512], BF16, ph) for i in range(7)], "pT")
        rc_r = Ring([sb("rc%d" % i, [128, 1], F32, ph) for i in range(3)], "rc")
        for i in range(2):
            S.op("pool", lambda e: e.memset(v_r.tiles[i][:, :, 128:129], 1.0), [], [v_r.bufs[i]])
            S.op("pool", lambda e: e.memset(vm_r.tiles[i][:, 128:129], 1.0), [], [vm_r.bufs[i]])
        scale = float(HD) ** -0.5
        NGQ = NB // 4
        dst_r = Ring([sb("dcst%d" % i, [128, 2 * D], BF16, ph) for i in range(2)], "dcst")
        tasks = []

        def mk_w(src, dst, dbuf, kc):
            def f():
                t, b = dst_r.next()
                S.dma("pool", lambda e: e.dma_start(out=t[:, 0:D], in_=src[kc * 128:(kc + 1) * 128, :]), B_ext, b, b)
                S.dma("sp", lambda e: e.dma_start(out=dst[kc, :, :], in_=t[:, 0:D]), b, dbuf, b, append=True)
            return f

        def mk_uv(r0):
            def f():
                t, b = dst_r.next()
                S.dma("pool", lambda e: e.dma_start(out=t[:, 0:D], in_=tab_u[r0:r0 + 128, :]), B_ext, b, b)
                S.dma("pool", lambda e: e.dma_start(out=t[:, D:2 * D], in_=tab_v[r0:r0 + 128, :]), B_ext, b, b)
                S.dma("sp", lambda e: e.dma_start(out=uv_bf[r0:r0 + 128, :], in_=t[:]), b, B_uv, b, append=True)
            return f

        for (src, dst, dbuf) in ((w_ao, wao_bf, B_wao), (w_o, wo_bf, B_wo), (w_q, wq_bf, B_wq)):
            for kc in range(KC):
                tasks.append(mk_w(src, dst, dbuf, kc))
        if nph >= 7:
            for r0 in range(0, NEXP, 128):
                tasks.append(mk_uv(r0))
        items = []
        for h in range(NH):
            for g in range(NGQ):
                items.append((h, g, -1))
                for kt in range(8 * g + 8):
                    items.append((h, g, kt))
        HS = {}
        SP_ = {}
        ACC = {}

        def head_load(h):
            kT, bk = kT_r.next()
            S.dma("sp", lambda e: e.dma_start(out=kT[:], in_=kT_d[h]), B_kT, bk, bk)
            vh, bvh = v_r.next()
            for t0 in range(0, AT, 8):
                S.dma("sp", lambda e: e.dma_start(
                    out=vh[:, t0:t0 + 8, 0:128],
                    in_=v_d[N_META + t0 * 128:N_META + (t0 + 8) * 128, h * 128:(h + 1) * 128]
                    .rearrange("(t p) c -> p t c", p=128)), B_v, bvh, bvh)
            vm, bvm = vm_r.next()
            S.dma("sp", lambda e: e.dma_start(out=vm[:, 0:128], in_=v_d[0:N_META, h * 128:(h + 1) * 128]),
                  B_v, bvm, bvm)
            qT, bq = q_r.next()
            S.dma("sp", lambda e: e.dma_start(out=qT[:], in_=qT_d[h]), B_qT, bq, bq)
            cq, bcq = cq_r.next()
            for b0 in range(0, NB, 16):
                b1 = min(NB, b0 + 16)
                S.dma("sp", lambda e: e.dma_start(
                    out=cq[:, b0:b1, :],
                    in_=cT_d[h, N_META:LK].rearrange("(b two q) -> b two q", two=2, q=128)[b0:b1, 1, :]
                    .partition_broadcast(128)), B_cT, bcq, bcq)
            oT, bo = o_r.next()
            HS[h] = (kT, bk, vh, bvh, vm, bvm, qT, bq, cq, bcq, oT, bo)

        def jmin_of(g, kt):
            return 0 if kt < 0 else max(0, (kt - 8 * g) // 2)

        def emit_S(n):
            h, g, kt = items[n]
            if h not in HS:
                head_load(h)
            kT, bk, vh, bvh, vm, bvm, qT, bq, cq, bcq, oT, bo = HS[h]
            pt, bpt = P["f"].next()
            SP_[n] = (pt, bpt)
            c0 = jmin_of(g, kt) * 128
            if kt < 0:
                S.op("pe", lambda e: e.matmul(pt[:16, 0:512], lhsT=kT[:, 0:N_META],
                                              rhs=qT[:, g * 512:(g + 1) * 512], start=True, stop=True),
                     [bk, bq], [bpt])
            else:
                S.op("pe", lambda e: e.matmul(
                    pt[:, c0:512], lhsT=kT[:, N_META + kt * 128:N_META + (kt + 1) * 128],
                    rhs=qT[:, g * 512 + c0:(g + 1) * 512], start=True, stop=True), [bk, bq], [bpt])

        def emit_post(n):
            h, g, kt = items[n]
            kT, bk, vh, bvh, vm, bvm, qT, bq, cq, bcq, oT, bo = HS[h]
            pt, bpt = SP_.pop(n)
            rows = 16 if kt < 0 else 128
            jm = jmin_of(g, kt)
            c0 = jm * 128
            cqg = cq[:, 4 * g:4 * g + 4, :].rearrange("p a b -> p (a b)")
            tmp, btmp = tmp_r.next()
            S.op("dve", lambda e: e.scalar_tensor_tensor(
                out=tmp[:rows, c0:512], in0=pt[:rows, c0:512], scalar=scale, in1=cqg[:rows, c0:512],
                op0=ALU.mult, op1=ALU.add), [bpt, bcq], [btmp])
            if kt >= 8 * g and (kt - 8 * g) % 2 == 1:
                S.op("dve", lambda e: e.tensor_tensor(out=tmp[:, c0:c0 + 128], in0=tmp[:, c0:c0 + 128],
                                                      in1=trineg[:], op=ALU.add), [btmp] + CONST, [btmp])
            pT, bpT = pT_r.next()
            bias = negckm[:, h:h + 1] if kt < 0 else negck[:, kt, h:h + 1]
            S.op("act", lambda e: e.activation(out=pT[:rows, c0:512], in_=tmp[:rows, c0:512], func=AF.Exp,
                                               bias=bias, scale=1.0), [btmp, B_nck], [bpT])
            if kt < 0:
                ACC[(h, g)] = [P["o"].next() for _ in range(4)]
            accs = ACC[(h, g)]
            for j in range(jm, 4):
                i = 4 * g + j
                po, bpo = accs[j]
                last = (kt == 2 * i + 1)
                if kt < 0:
                    S.op("pe", lambda e: e.matmul(po[:, 0:129], lhsT=pT[:16, j * 128:(j + 1) * 128],
                                                  rhs=vm[:, :], start=True, stop=False), [bpT, bvm], [bpo])
                else:
                    S.op("pe", lambda e: e.matmul(po[:, 0:129], lhsT=pT[:, j * 128:(j + 1) * 128],
                                                  rhs=vh[:, kt, :], start=False, stop=last), [bpT, bvh], [bpo])
                if last:
                    rc, brc = rc_r.next()
                    S.op("dve", lambda e: e.reciprocal(out=rc[:], in_=po[:, 128:129]), [bpo], [brc])
                    S.op("dve", lambda e: e.tensor_scalar(out=oT[:, i, :], in0=po[:, 0:128], scalar1=rc[:, 0:1],
                                                          scalar2=None, op0=ALU.mult), [bpo, brc], [bo])
            if g == NGQ - 1 and kt == 8 * g + 7:
                for b0 in range(0, NB, 8):
                    b1 = min(NB, b0 + 8)
                    S.dma("pool", lambda e: e.dma_start(
                        out=o_d[b0 * 128:b1 * 128, h * 128:(h + 1) * 128].rearrange("(b p) c -> p b c", p=128),
                        in_=oT[:, b0:b1, :]), bo, B_oT, bo, append=True)
                del HS[h]

        NI = len(items)
        every = max(1, (NI * 3 // 4) // max(1, len(tasks)))
        for n in range(NI + LA):
            if n < NI:
                emit_S(n)
            if n >= LA:
                emit_post(n - LA)
            if tasks and n % every == 0:
                tasks.pop(0)()
        while tasks:
            tasks.pop(0)()
        S.barrier()

    if nph >= 5:
      with contextlib.ExitStack() as ph:
        l1g = sb("l1g", [128, D], F32, ph)
        mkpsum(ph, 5, 0, 2)
        ot_r = Ring([sb("otk%d" % i, [128, D], BF16, ph) for i in range(2)], "otk")
        l1b = sb("l1b", [128, D], F32, ph)
        B_l1 = Buf("l1")
        for dst_, src_ in ((l1g, ln1_g), (l1b, ln1_b)):
            S.dma("sp", lambda e: e.dma_start(out=dst_[:], in_=src_), B_ext, B_l1, B_l1)
        oT_r = Ring([sb("oTg%d" % i, [128, KC, 512], BF16, ph) for i in range(1)], "oTg")
        mixT = sb("mixT", [128, KC, 512], BF16, ph)
        B_mix = Buf("mixT")
        w_r = Ring([sb("wste%d" % i, [128, KC, 512], BF16, ph) for i in range(2)], "wste")
        ga_r = Ring([sb("gat%d" % i, [128, 4, 512], BF16, ph) for i in range(2)], "gat")
        yc_r = Ring([sb("yct%d" % i, [128, 4, 512], BF16, ph) for i in range(2)], "yct")
        tf_r = Ring([sb("tfe%d" % i, [128, 512], F32, ph) for i in range(2)], "tfe")
        h0_r = Ring([sb("h0t%d" % i, [128, D], F32, ph) for i in range(2)], "h0t")
        hp_r = Ring([sb("h1p%d" % i, [128, D], F32, ph) for i in range(2)], "h1p")
        h1_r = Ring([sb("h1t%d" % i, [128, D], F32, ph) for i in range(2)], "h1t")
        for og in range(NOG):
            oTg, boT = oT_r.next()
            for t in range(4):
                ob = og * 4 + t
                otk, botk = ot_r.next()
                S.dma("sp", lambda e: e.dma_start(out=otk[:], in_=o_d[ob * 128:(ob + 1) * 128, :]),
                      B_oT, botk, botk)
                transpose_to(otk, botk, 128, oTg, boT, t * 128)
            for j in range(4):
                w, bw = load_w(w_r, wao_bf, B_wao, j * 512)
                gat, bga = ga_r.next()
                S.dma("sp", lambda e: e.dma_start(
                    out=gat[:], in_=gaT_d[j * 4:(j + 1) * 4, :, og * 512:(og + 1) * 512].rearrange("c p t -> p c t")),
                    B_ga, bga, bga)
                yct, byc = yc_r.next()
                S.dma("sp", lambda e: e.dma_start(
                    out=yct[:], in_=ycg_d[j * 4:(j + 1) * 4, :, og * 512:(og + 1) * 512].rearrange("c p t -> p c t")),
                    B_ycg, byc, byc)
                for c in range(4):
                    pt, bpt = P["f"].next()
                    mm_acc(pt[:], bpt, w, bw, c, oTg, boT, 0, 512)
                    tf, btf = tf_r.next()
                    S.op("dve", lambda e: e.tensor_tensor(out=tf[:], in0=pt[:], in1=gat[:, c, :], op=ALU.mult),
                         [bpt, bga], [btf])
                    S.op("pool", lambda e: e.tensor_tensor(out=mixT[:, j * 4 + c, :], in0=tf[:],
                                                           in1=yct[:, c, :], op=ALU.add), [btf, byc], [B_mix])
            for t in range(4):
                ob = og * 4 + t
                h0t, bh0 = h0_r.next()
                S.dma("sp", lambda e: e.dma_start(out=h0t[:], in_=h0_d[ob * 128:(ob + 1) * 128, :]),
                      B_h0, bh0, bh0)
                hp, bhp = hp_r.next()
                for j in range(4):
                    w, bw = load_w(w_r, wo_bf, B_wo, j * 512)
                    pt, bpt = P["f"].next()
                    for kc in range(KC):
                        S.op("pe", lambda e: e.matmul(
                            pt[:], lhsT=mixT[:, kc, t * 128:(t + 1) * 128], rhs=w[:, kc, :],
                            start=(kc == 0), stop=(kc == KC - 1)), [bw, B_mix], [bpt])
                    S.op("dve", lambda e: e.scalar_tensor_tensor(
                        out=hp[:, j * 512:(j + 1) * 512], in0=h0t[:, j * 512:(j + 1) * 512], scalar=DN_ALPHA,
                        in1=pt[:], op0=ALU.mult, op1=ALU.add), [bpt, bh0], [bhp])
                h1t, bh1 = h1_r.next()
                layer_norm_tile(hp, bhp, 128, l1g, l1b, [B_l1], h1t, bh1)
                S.dma("pool", lambda e: e.dma_start(out=h1_d[ob * 128:(ob + 1) * 128, :], in_=h1t[:]),
                      bh1, B_h1, bh1, append=True)
        S.barrier()

    if nph >= 6:
      with contextlib.ExitStack() as ph:
        skT = sb("skT", [128, 16, 128], BF16, ph)
        mkpsum(ph, 5, 0, 2)
        B_sk = Buf("skT")
        sk_r = Ring([sb("skl%d" % i, [128, 128], F32, ph) for i in range(2)], "skl")
        for hp_ in range(16):
            skl, bskl = sk_r.next()
            S.dma("sp", lambda e: e.dma_start(out=skl[:], in_=subk[hp_]), B_ext, bskl, bskl)
            pt, bpt = P["f"].next()
            S.op("pe", lambda e: e.transpose(out=pt[:, 0:128], in_=skl[:], identity=ident_f[:]),
                 [bskl] + CONST, [bpt])
            S.op("act", lambda e: e.activation(out=skT[:, hp_, :], in_=pt[:, 0:128], func=AF.Copy),
                 [bpt], [B_sk])
        h1_r = Ring([sb("h1l%d" % i, [128, D], F32, ph) for i in range(2)], "h1l")
        xb_r = Ring([sb("h1b%d" % i, [128, D], BF16, ph) for i in range(2)], "h1b")
        h1T = sb("h1T", [128, KC, 512], BF16, ph)
        B_h1T = Buf("h1T")
        w_r = Ring([sb("wstq%d" % i, [128, KC, 512], BF16, ph) for i in range(2)], "wstq")
        qpT = sb("qpT", [128, 16, 512], BF16, ph)
        B_qp = Buf("qpT")
        s_r = Ring([sb("st%d" % i, [128, D], F32, ph) for i in range(2)], "st")
        for og in range(NOG):
            for t in range(4):
                ob = og * 4 + t
                h1l, bh1l = h1_r.next()
                S.dma("sp", lambda e: e.dma_start(out=h1l[:], in_=h1_d[ob * 128:(ob + 1) * 128, :]),
                      B_h1, bh1l, bh1l)
                xb, bxb = xb_r.next()
                S.op("act", lambda e: e.activation(out=xb[:], in_=h1l[:], func=AF.Copy), [bh1l], [bxb])
                transpose_to(xb, bxb, 128, h1T, B_h1T, t * 128)
            for j in range(4):
                w, bw = load_w(w_r, wq_bf, B_wq, j * 512)
                for c in range(4):
                    pt, bpt = P["f"].next()
                    mm_acc(pt[:], bpt, w, bw, c, h1T, B_h1T, 0, 512)
                    S.op("act", lambda e: e.activation(out=qpT[:, j * 4 + c, :], in_=pt[:], func=AF.Copy),
                         [bpt], [B_qp])
            for t in range(4):
                ob = og * 4 + t
                stl, bst_ = s_r.next()
                for hq in range(4):
                    pt, bpt = P["f"].next()
                    for c in range(4):
                        hp_ = hq * 4 + c
                        S.op("pe", lambda e: e.matmul(
                            pt[:, c * 128:(c + 1) * 128], lhsT=qpT[:, hp_, t * 128:(t + 1) * 128],
                            rhs=skT[:, hp_, :], start=True, stop=True), [B_qp, B_sk], [bpt])
                    S.op("act", lambda e: e.activation(out=stl[:, hq * 512:(hq + 1) * 512], in_=pt[:],
                                                       func=AF.Copy), [bpt], [bst_])
                S.dma("pool", lambda e: e.dma_start(out=s_d[ob * 128:(ob + 1) * 128, :], in_=stl[:]),
                      bst_, B_s, bst_, append=True)
        S.barrier()

    if nph >= 7:
      with contextlib.ExitStack() as ph:
        l2g = sb("l2g", [128, D], F32, ph)
        l2b = sb("l2b", [128, D], F32, ph)
        B_l2 = Buf("l2")
        for dst_, src_ in ((l2g, ln2_g), (l2b, ln2_b)):
            S.dma("sp", lambda e: e.dma_start(out=dst_[:], in_=src_), B_ext, B_l2, B_l2)
        s_r = Ring([sb("sf%d" % i, [128, 16, 128], F32, ph) for i in range(1)], "sf")
        h1_r = Ring([sb("h1f%d" % i, [128, D], F32, ph) for i in range(2)], "h1f")
        tkbuf = sb("tkbuf", [128, 2048], F32, ph)
        s2 = tkbuf[:].rearrange("p (a b) -> p a b", b=128)
        m16 = sb("m16", [128, 16, 16], F32, ph)
        ix16 = sb("ix16", [128, 16, 16], U32, ph)
        ixf = sb("ixf", [128, 16, 16], F32, ph)
        cand = sb("cand", [128, 8, 256], F32, ph)
        cand2 = tkbuf[:].rearrange("p (a b) -> p a b", b=256)
        vals = sb("vals", [128, 8, 16], F32, ph)
        posu = sb("posu", [128, 8, 16], U32, ph)
        posf = sb("posf", [128, 128], F32, ph)
        rf = sb("rf", [128, 128], F32, ph)
        cf = sb("cf", [128, 128], F32, ph)
        oh = tkbuf[:].rearrange("p (a b) -> p a b", b=16)
        If = sb("If", [128, 128], F32, ph)
        Jf = sb("Jf", [128, 128], F32, ph)
        idf = sb("idf", [128, 128], F32, ph)
        ids_r = Ring([sb("ids%d" % i, [128, 128], I32, ph) for i in range(2)], "ids")
        ev8 = sb("ev8", [128, 8, 16], F32, ph)
        z8 = sb("z8", [128, 8], F32, ph)
        g_r = Ring([sb("gk%d" % i, [128, 128], F32, ph) for i in range(2)], "gk")
        dots = sb("dots", [128, 128], F32, ph)
        wk_r = Ring([sb("wk%d" % i, [128, 128], F32, ph) for i in range(2)], "wk")
        mkpsum(ph, 0, 4, 0)
        gbuf_r = Ring([sb("gb%d" % i, [128, 2 * D], BF16, ph) for i in range(8)], "gb")
        junk_r = Ring([sb("junk%d" % i, [128, D // 2], BF16, ph) for i in range(2)], "junk")
        pr_r = Ring([sb("prd%d" % i, [128, D // 2], BF16, ph) for i in range(3)], "prd")
        jk2_r = Ring([sb("jk2_%d" % i, [128, D // 2], BF16, ph) for i in range(2)], "jk2")
        dots2 = sb("dots2", [128, 128], F32, ph)
        dg_r = Ring([sb("dgf%d" % i, [128, 128], BF16, ph) for i in range(4)], "dgf")
        dg1_r = Ring([sb("dgg%d" % i, [128, 128], F32, ph) for i in range(4)], "dgg")
        ak = sb("ak", [128, 128], F32, ph)
        wkt = sb("wkt", [128, 128], F32, ph)
        pre_r = Ring([sb("pre%d" % i, [128, D], F32, ph) for i in range(1)], "pre")
        o_r = Ring([sb("of%d" % i, [128, D], F32, ph) for i in range(1)], "of")
        colb = [Buf("col%d" % i) for i in range(4)]
        h1b_r = Ring([sb("h1bf%d" % i, [128, D], BF16, ph) for i in range(1)], "h1bf")
        B_tk, B_junk, B_dots = Buf("topk"), Buf("junk"), Buf("dots")
        TK = [B_tk]
        RES = {}

        def topk_gen(ob):
                sf, bsf = s_r.next()
                S.dma("sp", lambda e: e.dma_start(
                    out=sf[:], in_=s_d[ob * 128:(ob + 1) * 128, :].rearrange("p (a b) -> p a b", b=128)),
                    B_s, bsf, bsf)
                yield
                for hp_ in range(16):
                    S.op("dve", lambda e: e.max(out=m16[:, hp_, 0:8], in_=sf[:, hp_, :]), [bsf], TK)
                    S.op("dve", lambda e: e.match_replace(out=s2[:, hp_, :], in_to_replace=m16[:, hp_, 0:8],
                                                          in_values=sf[:, hp_, :], imm_value=-1e30), [bsf] + TK, TK)
                    S.op("dve", lambda e: e.max(out=m16[:, hp_, 8:16], in_=s2[:, hp_, :]), TK, TK)
                    S.op("dve", lambda e: e.max_index(out=ix16[:, hp_, 0:8], in_max=m16[:, hp_, 0:8],
                                                      in_values=sf[:, hp_, :]), [bsf] + TK, TK)
                    S.op("dve", lambda e: e.max_index(out=ix16[:, hp_, 8:16], in_max=m16[:, hp_, 8:16],
                                                      in_values=sf[:, hp_, :]), [bsf] + TK, TK)
                    yield
                S.op("dve", lambda e: e.tensor_copy(out=ixf[:], in_=ix16[:]), TK, TK)
                yield
                m4 = m16[:].rearrange("p (h s) k -> p h s k", s=2)
                ix4 = ixf[:].rearrange("p (h s) k -> p h s k", s=2)
                for h in range(8):
                    S.op("dve", lambda e: e.tensor_tensor(
                        out=cand[:, h, :].rearrange("p (r c) -> p r c", c=16),
                        in0=m4[:, h, 0, :].unsqueeze(2).to_broadcast([128, 16, 16]),
                        in1=m4[:, h, 1, :].unsqueeze(1).to_broadcast([128, 16, 16]), op=ALU.add), TK, TK)
                    yield
                for h in range(8):
                    S.op("dve", lambda e: e.max(out=vals[:, h, 0:8], in_=cand[:, h, :]), TK, TK)
                    S.op("dve", lambda e: e.match_replace(out=cand2[:, h, :], in_to_replace=vals[:, h, 0:8],
                                                          in_values=cand[:, h, :], imm_value=-1e30), TK, TK)
                    S.op("dve", lambda e: e.max(out=vals[:, h, 8:16], in_=cand2[:, h, :]), TK, TK)
                    S.op("dve", lambda e: e.max_index(out=posu[:, h, 0:8], in_max=vals[:, h, 0:8],
                                                      in_values=cand[:, h, :]), TK, TK)
                    S.op("dve", lambda e: e.max_index(out=posu[:, h, 8:16], in_max=vals[:, h, 8:16],
                                                      in_values=cand[:, h, :]), TK, TK)
                    yield
                S.op("dve", lambda e: e.tensor_copy(out=posf[:], in_=posu[:].rearrange("p h k -> p (h k)")), TK, TK)
                yield
                S.op("dve", lambda e: e.tensor_tensor(
                    out=oh[:], in0=posf[:].unsqueeze(2).to_broadcast([128, 128, 16]),
                    in1=thr16[:].unsqueeze(1).to_broadcast([128, 128, 16]), op=ALU.is_ge), TK + CONST, TK)
                yield
                S.op("dve", lambda e: e.reduce_sum(out=rf[:], in_=oh[:], axis=AX.X), TK, TK)
                yield
                S.op("dve", lambda e: e.scalar_tensor_tensor(out=cf[:], in0=rf[:], scalar=-16.0, in1=posf[:],
                                                             op0=ALU.mult, op1=ALU.add), TK, TK)
                yield
                for (src_rc, side, dstI) in ((rf, 0, If), (cf, 1, Jf)):
                    S.op("dve", lambda e: e.tensor_tensor(
                        out=oh[:], in0=src_rc[:].unsqueeze(2).to_broadcast([128, 128, 16]),
                        in1=iota16[:].unsqueeze(1).to_broadcast([128, 128, 16]), op=ALU.is_equal), TK + CONST, TK)
                    for h in range(8):
                        S.op("dve", lambda e: e.tensor_tensor(
                            out=oh[:, h * 16:(h + 1) * 16, :], in0=oh[:, h * 16:(h + 1) * 16, :],
                            in1=ix4[:, h, side, :].unsqueeze(1).to_broadcast([128, 16, 16]), op=ALU.mult), TK, TK)
                    yield
                    S.op("dve", lambda e: e.reduce_sum(out=dstI[:], in_=oh[:], axis=AX.X), TK, TK)
                    yield
                S.op("dve", lambda e: e.scalar_tensor_tensor(out=idf[:], in0=If[:], scalar=128.0, in1=Jf[:],
                                                             op0=ALU.mult, op1=ALU.add), TK, TK)
                yield
                ids, bids = ids_r.next()
                S.op("dve", lambda e: e.tensor_copy(out=ids[:], in_=idf[:]), TK, [bids])
                yield
                S.op("dve", lambda e: e.tensor_tensor(
                    out=ev8[:], in0=vals[:], in1=vals[:, :, 0:1].to_broadcast([128, 8, 16]), op=ALU.subtract),
                    TK, TK)
                yield
                S.op("act", lambda e: e.activation(out=ev8[:], in_=ev8[:], func=AF.Exp), TK, TK)
                yield
                S.op("dve", lambda e: e.reduce_sum(out=z8[:], in_=ev8[:], axis=AX.X), TK, TK)
                yield
                S.op("dve", lambda e: e.reciprocal(out=z8[:], in_=z8[:]), TK, TK)
                yield
                gk, bgk = g_r.next()
                S.op("dve", lambda e: e.tensor_tensor(
                    out=gk[:].rearrange("p (h k) -> p h k", k=16), in0=ev8[:],
                    in1=z8[:].unsqueeze(2).to_broadcast([128, 8, 16]), op=ALU.mult), TK, [bgk])
                yield
                RES[ob] = (ids, bids, gk, bgk)
                yield

        for _ in topk_gen(0):
            pass
        for ob in range(NB):
            ids, bids, gk, bgk = RES.pop(ob)
            nxt = topk_gen(ob + 1) if ob + 1 < NB else None
            h1f, bh1f = h1_r.next()
            S.dma("sp", lambda e: e.dma_start(out=h1f[:], in_=h1_d[ob * 128:(ob + 1) * 128, :]),
                  B_h1, bh1f, bh1f)
            h1b, bh1b = h1b_r.next()
            S.op("act", lambda e: e.activation(out=h1b[:], in_=h1f[:], func=AF.Copy), [bh1f], [bh1b])
            accs = [P["o"].next() for _ in range(4)]
            for k in range(128):
                gb, bgb = gbuf_r.next()
                S.dma("pool", lambda e: e.indirect_dma_start(
                    out=gb[:], out_offset=None, in_=uv_bf,
                    in_offset=bass.IndirectOffsetOnAxis(ap=ids[:, k:k + 1], axis=0)),
                    B_uv, bgb, bgb, extra_reads=[bids])
                jk, bjk = junk_r.next()
                cb_ = colb[k % 4]
                H2 = D // 2
                S.op("dve", lambda e: e.scalar_tensor_tensor(
                    out=jk[:], in0=gb[:, 0:H2], scalar=1.0, in1=h1b[:, 0:H2], op0=ALU.mult, op1=ALU.mult,
                    accum_out=dots[:, k:k + 1]), [bgb, bh1b], [bjk, cb_])
                pr, bpr = pr_r.next()
                S.op("dve", lambda e: e.tensor_tensor(out=pr[:], in0=gb[:, H2:D], in1=h1b[:, H2:D], op=ALU.mult),
                     [bgb, bh1b], [bpr])
                jk2, bjk2 = jk2_r.next()
                S.op("act", lambda e: e.activation(out=jk2[:], in_=pr[:], func=AF.Identity,
                                                   accum_out=dots2[:, k:k + 1]), [bpr], [bjk2, cb_])
                S.op("act", lambda e: e.activation(out=ak[:, k:k + 1], in_=dots[:, k:k + 1], func=AF.Gelu,
                                                   bias=dots2[:, k:k + 1], scale=1.0), [cb_], [cb_])
                dg1, bdg1 = dg1_r.next()
                S.op("act", lambda e: e.activation(out=dg1[:], in_=ident_f[:], func=AF.Identity,
                                                   scale=ak[:, k:k + 1]), [cb_] + CONST, [bdg1])
                dg, bdg = dg_r.next()
                S.op("act", lambda e: e.activation(out=dg[:], in_=dg1[:], func=AF.Identity,
                                                   scale=gk[:, k:k + 1]), [bdg1, bgk], [bdg])
                for j in range(4):
                    po, bpo = accs[j]
                    S.op("pe", lambda e: e.matmul(po[:, 0:512], lhsT=dg[:],
                                                  rhs=gb[:, D + j * 512:D + (j + 1) * 512],
                                                  start=(k == 0), stop=(k == 127)), [bdg, bgb], [bpo])
                if nxt is not None:
                    next(nxt, None)
            if nxt is not None:
                for _ in nxt:
                    pass
            pre, bpre = pre_r.next()
            for j in range(4):
                po, bpo = accs[j]
                S.op("dve", lambda e: e.scalar_tensor_tensor(
                    out=pre[:, j * 512:(j + 1) * 512], in0=h1f[:, j * 512:(j + 1) * 512], scalar=DN_ALPHA,
                    in1=po[:, 0:512], op0=ALU.mult, op1=ALU.add), [bh1f, bpo], [bpre])
            of, bof = o_r.next()
            layer_norm_tile(pre, bpre, 128, l2g, l2b, [B_l2], of, bof)
            S.dma("pool", lambda e: e.dma_start(out=out[ob * 128:(ob + 1) * 128, :], in_=of[:]),
                  bof, B_out, bof, append=True)
        S.barrier()

    S.final_wait("sp", [B_out])
    print("kernel build: %d instructions, %d dma sems" % (S.ninstr, S.nsem))
    stack.close()
    return nc


def host_inputs(SEQ, inputs):
    AT = SEQ // 128
    NTOK = AT * 128
    f32 = np.float32
    x = np.asarray(inputs["x"], f32)
    meta = np.ascontiguousarray(np.asarray(inputs["meta_tokens"], f32))
    b_in = np.asarray(inputs["b_in"], f32)[0]
    b_fm = np.zeros((128, 113), f32)
    cols = np.concatenate([b_in[OFF_A:OFF_V], b_in[OFF_GC:N_IN]])
    b_fm[:, :96] = cols.reshape(96, 128).T
    bf2 = np.zeros((128, 113), f32)
    bf2[:, 0:64] = b_fm[:, 0:64]
    bf2[:, 80:112] = b_fm[:, 64:96]
    bf2[:16, 112] = b_in[OFF_F:OFF_F + 16]

    def bc(v):
        return np.ascontiguousarray(np.broadcast_to(np.asarray(v, f32).reshape(1, -1), (128, D)))

    def fm(v):
        return np.ascontiguousarray(np.asarray(v, f32).reshape(KC, 128).T)

    common = {
        "xmeta": meta,
        "lnin_g": bc(inputs["ln_in_g"]), "lnin_b": bc(inputs["ln_in_b"]),
        "w_in": np.ascontiguousarray(np.asarray(inputs["w_in"], f32)[0]),
        "b_fm": bf2, "bv_b": bc(b_in[OFF_V:OFF_F]),
        "dww": np.ascontiguousarray(np.asarray(inputs["conv_dw_w"], f32)[0].T.reshape(KC, 128, TAPS).transpose(1, 0, 2)),
        "dwb": fm(inputs["conv_dw_b"]), "cln_g": fm(inputs["conv_ln_g"]), "cln_b": fm(inputs["conv_ln_b"]),
        "w_co": np.ascontiguousarray(np.asarray(inputs["w_conv_out"], f32)[0]),
        "w_ao": np.ascontiguousarray(np.asarray(inputs["w_attn_out"], f32)[0]),
        "w_o": np.ascontiguousarray(np.asarray(inputs["w_out"], f32)[0]),
        "ln1_g": bc(inputs["ln1_g"]), "ln1_b": bc(inputs["ln1_b"]),
        "w_q": np.ascontiguousarray(np.asarray(inputs["peer_w_q"], f32)[0]),
        "subk": np.ascontiguousarray(np.asarray(inputs["peer_subkeys"], f32)[0].reshape(16, 128, 128)),
        "tab_u": np.ascontiguousarray(np.asarray(inputs["peer_u"], f32)[0]),
        "tab_v": np.ascontiguousarray(np.asarray(inputs["peer_v"], f32)[0]),
        "ln2_g": bc(inputs["ln2_g"]), "ln2_b": bc(inputs["ln2_b"]),
        "ident": np.eye(128, dtype=f32),
        "tri": np.triu(np.ones((128, 128), f32)),
        "iota16": np.ascontiguousarray(np.broadcast_to(np.arange(16, dtype=f32), (128, 16))),
    }
    maps = []
    for core in range(8):
        b, p = core // 2, core % 2
        xa = np.zeros((NTOK, D), f32)
        valid = np.ones((NTOK,), f32)
        kb0 = np.zeros((128, 1), f32)
        hmask = np.ones((128, 32), f32)
        if p == 0:
            xa[128:] = x[b, :NTOK - 128]
            xa[112:128] = meta
            valid[:128] = 0.0
            kb0[:] = NEG
            hmask[:, :16] = 0.0
        else:
            xa[:] = x[b, :NTOK]
        m = dict(common)
        m.update({"xa": xa, "valid16": np.ascontiguousarray(np.broadcast_to(valid, (16, NTOK))),
                  "kb0": kb0, "hmask": hmask})
        maps.append(m)
    return maps


def assemble(SEQ, results):
    AT = SEQ // 128
    NB = AT // 2
    out = np.zeros((4, SEQ, D), np.float32)
    for core in range(8):
        b, p = core // 2, core % 2
        o = results[core]["out"].reshape(NB, 128, D)
        for i in range(NB):
            rt = 2 * i + p
            out[b, rt * 128:(rt + 1) * 128] = o[i]
    return out


_NC_CACHE = {}


def kernel(**inputs):
    SEQ = int(np.asarray(inputs["x"]).shape[1])
    if SEQ not in _NC_CACHE:
        _NC_CACHE[SEQ] = build(SEQ)
    nc = _NC_CACHE[SEQ]
    maps = host_inputs(SEQ, inputs)
    res = run_bass_kernel_spmd(nc, maps, core_ids=list(range(8)))
    return assemble(SEQ, res.results)
```

```python
import contextlib
import numpy as np
import concourse.bass as bass
import concourse.mybir as mybir
from concourse.bass_utils import run_bass_kernel_spmd

F32 = mybir.dt.float32
BF16 = mybir.dt.bfloat16
U32 = mybir.dt.uint32
I32 = mybir.dt.int32
ALU = mybir.AluOpType
AF = mybir.ActivationFunctionType
AX = mybir.AxisListType

D = 2048
KC = 16
NH = 16
HD = 128
N_META = 16
TAPS = 31
N_IN = 14352
OFF_A, OFF_G, OFF_Q, OFF_K, OFF_V, OFF_F, OFF_GC, OFF_GA = 0, 2048, 4096, 6144, 8192, 10240, 10256, 12304
LN_EPS = 1e-5
DN_ALPHA = 2.0 ** 0.25
NEXP = 16384
NEG = -30000.0


class Buf:
    __slots__ = ("name", "w", "r", "sem")

    def __init__(self, name):
        self.name = name
        self.w = {}
        self.r = {}
        self.sem = None


class Sched:
    def __init__(self, nc, stack):
        self.nc = nc
        self.stack = stack
        self.names = ["pe", "act", "dve", "pool", "sp"]
        self.eng = {"pe": nc.tensor, "act": nc.scalar, "dve": nc.vector, "pool": nc.gpsimd, "sp": nc.sync}
        self.esem = {e: stack.enter_context(nc.semaphore("s_" + e)) for e in self.names}
        self.ecount = {e: 0 for e in self.names}
        self.waited = {e: {} for e in self.names}
        self.semcount = {}
        self.nsem = 0
        self.ninstr = 0

    def _wait(self, eng, deps):
        wd = self.waited[eng]
        for sem, val in deps.items():
            if eng == "pe" and sem is self.esem["pe"]:
                continue
            if wd.get(sem, 0) >= val:
                continue
            wd[sem] = val
            self.eng[eng].wait_ge(sem, val)

    @staticmethod
    def _merge(dst, src):
        for s, v in src.items():
            if dst.get(s, 0) < v:
                dst[s] = v

    def op(self, eng, fn, reads=(), writes=()):
        deps = {}
        for b in reads:
            self._merge(deps, b.w)
        for b in writes:
            self._merge(deps, b.w)
            self._merge(deps, b.r)
        self._wait(eng, deps)
        self.ecount[eng] += 1
        self.ninstr += 1
        sem = self.esem[eng]
        tok = {sem: self.ecount[eng]}
        fn(self.eng[eng]).then_inc(sem, 1)
        for b in reads:
            self._merge(b.r, tok)
        for b in writes:
            b.w = dict(tok)
            b.r = {}

    def _slot_sem(self, b):
        if b.sem is None:
            b.sem = self.stack.enter_context(self.nc.semaphore("d%d" % self.nsem))
            self.nsem += 1
            self.semcount[b.sem] = 0
        return b.sem

    def dma(self, q, fn, src, dst, slot, extra_reads=(), append=False):
        deps = {}
        self._merge(deps, src.w)
        for b in extra_reads:
            self._merge(deps, b.w)
        if not append:
            self._merge(deps, dst.w)
            self._merge(deps, dst.r)
        self._wait(q, deps)
        sem = self._slot_sem(slot)
        self.semcount[sem] += 16
        self.ninstr += 1
        tok = {sem: self.semcount[sem]}
        fn(self.eng[q]).then_inc(sem, 16)
        self._merge(src.r, tok)
        for b in extra_reads:
            self._merge(b.r, tok)
        if append:
            self._merge(dst.w, tok)
        else:
            dst.w = dict(tok)
            dst.r = {}

    def final_wait(self, eng, bufs):
        deps = {}
        for b in bufs:
            self._merge(deps, b.w)
        self._wait(eng, deps)

    def barrier(self):
        deps = {self.esem[e]: self.ecount[e] for e in self.names if self.ecount[e] > 0}
        for sem, v in self.semcount.items():
            if v > 0:
                deps[sem] = v
        for e in self.names:
            self._wait(e, dict(deps))


class Ring:
    def __init__(self, tiles, name):
        self.tiles = tiles
        self.bufs = [Buf("%s%d" % (name, i)) for i in range(len(tiles))]
        self.i = -1

    def next(self):
        self.i = (self.i + 1) % len(self.tiles)
        return self.tiles[self.i], self.bufs[self.i]


def build(SEQ, dbg=False, stop_after="F"):
    AT = SEQ // 128
    NB = AT // 2
    NG = AT // 4
    NTOK = AT * 128
    NOWN = NB * 128
    LK = N_META + NTOK
    NOG = NOWN // 512
    assert AT % 8 == 0
    PH = "0 AB B2 C D E1 E2 F".split()
    nph = PH.index(stop_after)

    nc = bass.Bass("TRN2", target_bir_lowering=False)
    stack = contextlib.ExitStack()

    def din(name, shape, dt=F32):
        return nc.dram_tensor(name, list(shape), dt, kind="ExternalInput").ap()

    def dscr(name, shape, dt):
        return nc.dram_tensor(name, list(shape), dt,
                              kind="ExternalOutput" if dbg else "Internal").ap()

    def dint(name, shape, dt):
        return nc.dram_tensor(name, list(shape), dt, kind="Internal").ap()

    xa = din("xa", [NTOK, D])
    xmeta = din("xmeta", [N_META, D])
    valid16 = din("valid16", [16, NTOK])
    kb0 = din("kb0", [128, 1])
    hmask = din("hmask", [128, 32])
    lnin_g = din("lnin_g", [128, D])
    lnin_b = din("lnin_b", [128, D])
    w_in = din("w_in", [D, N_IN])
    b_fm = din("b_fm", [128, 113])
    bv_b = din("bv_b", [128, D])
    dww = din("dww", [128, KC, TAPS])
    dwb = din("dwb", [128, KC])
    cln_g = din("cln_g", [128, KC])
    cln_b = din("cln_b", [128, KC])
    w_co = din("w_co", [D, D])
    w_ao = din("w_ao", [D, D])
    w_o = din("w_o", [D, D])
    ln1_g = din("ln1_g", [128, D])
    ln1_b = din("ln1_b", [128, D])
    w_q = din("w_q", [D, D])
    subk = din("subk", [16, 128, 128])
    tab_u = din("tab_u", [NEXP, D])
    tab_v = din("tab_v", [NEXP, D])
    ln2_g = din("ln2_g", [128, D])
    ln2_b = din("ln2_b", [128, D])
    ident_in = din("ident", [128, 128])
    tri_in = din("tri", [128, 128])
    iota16_in = din("iota16", [128, 16])
    out = nc.dram_tensor("out", [NOWN, D], F32, kind="ExternalOutput").ap()

    win_bf = dint("win_bf", [KC, 128, N_IN], BF16)
    wco_bf = dint("wco_bf", [KC, 128, D], BF16)
    wao_bf = dint("wao_bf", [KC, 128, D], BF16)
    wo_bf = dint("wo_bf", [KC, 128, D], BF16)
    wq_bf = dint("wq_bf", [KC, 128, D], BF16)
    kT_d = dscr("kT_d", [NH, 128, LK], BF16)
    v_d = dscr("v_d", [LK, D], BF16)
    qT_d = dscr("qT_d", [NH, 128, NOWN], BF16)
    uT_d = dscr("uT_d", [KC, 128, NB, 160], BF16)
    gcT_d = dscr("gcT_d", [KC, 128, NOWN], BF16)
    gaT_d = dscr("gaT_d", [KC, 128, NOWN], BF16)
    logf_d = dscr("logf_d", [16, LK], F32)
    cT_d = dscr("cT_d", [16, LK], F32)
    ycg_d = dscr("ycg_d", [KC, 128, NOWN], BF16)
    o_d = dscr("o_d", [NOWN, D], BF16)
    h0_d = dscr("h0_d", [NOWN, D], F32)
    h1_d = dscr("h1_d", [NOWN, D], F32)
    s_d = dscr("s_d", [NOWN, D], F32)
    uv_bf = dint("uv_bf", [NEXP, 2 * D], BF16)

    S = Sched(nc, stack)
    B_ext = Buf("ext")

    def sb(name, shape, dt, st=None):
        return (st or stack).enter_context(nc.sbuf_tensor("t_" + name, list(shape), dt))

    def ps(name, shape, dt=F32):
        return stack.enter_context(nc.psum_tensor(name, list(shape), dt))

    P = {}
    pscount = [0]

    def mkpsum(st, nf, no, nb):
        assert nf + no + nb <= 8
        pscount[0] += 1
        tag = "p%d_" % pscount[0]
        P["f"] = Ring([st.enter_context(nc.psum_tensor(tag + "f%d" % i, [128, 512], F32)) for i in range(nf)], "psf")
        P["o"] = Ring([st.enter_context(nc.psum_tensor(tag + "o%d" % i, [128, 512], F32)) for i in range(no)], "pso")
        P["b"] = Ring([st.enter_context(nc.psum_tensor(tag + "b%d" % i, [128, 1024], BF16)) for i in range(nb)], "psb")

    B_c = Buf("consts")
    ident_f = sb("ident_f", [128, 128], F32)
    ident_b = sb("ident_b", [128, 128], BF16)
    tri_b = sb("tri_b", [128, 128], BF16)
    tri_f = sb("tri_f", [128, 128], F32)
    trineg = sb("trineg", [128, 128], F32)
    iota16 = sb("iota16", [128, 16], F32)
    thr16 = sb("thr16", [128, 16], F32)
    bfm = sb("bfm", [128, 113], F32)
    nbf = sb("nbf", [16, 1], F32)
    kb0_t = sb("kb0_t", [128, 1], F32)
    hmask_t = sb("hmask_t", [128, 32], F32)
    eps_t = sb("eps_t", [128, 1], F32)
    one_t = sb("one_t", [128, 1], F32)
    onesm = sb("onesm", [128, 128], BF16)
    dww_t = sb("dww_t", [128, KC, TAPS], F32)
    dwb_t = sb("dwb_t", [128, KC], F32)
    clng_t = sb("clng_t", [128, KC], F32)
    clnb_t = sb("clnb_t", [128, KC], F32)
    negck = sb("negck", [128, AT, NH], F32)
    negckm = sb("negckm", [16, NH], F32)
    st_r = Ring([sb("lnst%d" % i, [128, 4, 6], F32) for i in range(2)], "lnst")
    mv_r = Ring([sb("lnmv%d" % i, [128, 4], F32) for i in range(2)], "lnmv")

    def cload(dst, src):
        S.dma("sp", lambda e: e.dma_start(out=dst, in_=src), B_ext, B_c, B_c)

    cload(ident_f[:], ident_in)
    cload(tri_f[:], tri_in)
    cload(iota16[:], iota16_in)
    cload(bfm[:], b_fm)
    cload(kb0_t[:], kb0)
    cload(hmask_t[:], hmask)
    cload(dww_t[:], dww)
    cload(dwb_t[:], dwb)
    cload(clng_t[:], cln_g)
    cload(clnb_t[:], cln_b)
    B_c2 = Buf("consts2")
    S.op("dve", lambda e: e.tensor_copy(out=ident_b[:], in_=ident_f[:]), [B_c], [B_c2])
    S.op("dve", lambda e: e.tensor_copy(out=tri_b[:], in_=tri_f[:]), [B_c], [B_c2])
    S.op("dve", lambda e: e.memset(eps_t[:], LN_EPS), [], [B_c2])
    S.op("dve", lambda e: e.memset(one_t[:], 1.0), [], [B_c2])
    S.op("dve", lambda e: e.memset(onesm[:], 1.0 / D), [], [B_c2])
    S.op("dve", lambda e: e.tensor_scalar(out=nbf[:], in0=bfm[0:16, 112:113], scalar1=-1.0, scalar2=None,
                                          op0=ALU.mult), [B_c], [B_c2])
    S.op("dve", lambda e: e.tensor_scalar(out=thr16[:], in0=iota16[:], scalar1=16.0, scalar2=16.0,
                                          op0=ALU.mult, op1=ALU.add), [B_c], [B_c2])
    S.op("dve", lambda e: e.tensor_scalar(out=trineg[:], in0=tri_f[:], scalar1=-1.0, scalar2=-NEG,
                                          op0=ALU.add, op1=ALU.mult), [B_c], [B_c2])
    CONST = [B_c, B_c2]

    (B_win, B_wco, B_wao, B_wo, B_wq, B_kT, B_v, B_qT, B_uT, B_gc, B_ga, B_lf, B_cT, B_h0,
     B_ycg, B_oT, B_h1, B_s, B_out, B_nck, B_uv) = (Buf(n) for n in (
         "win wco wao wo wq kT v qT uT gc ga lf cT h0 ycg oT h1 s out nck uv").split())

    def layer_norm_tile(xt, bx, rows, g_t, b_t, gb_bufs, out_f32, b_out):
        st, bst = st_r.next()
        mv, bmv = mv_r.next()
        for j in range(4):
            S.op("dve", lambda e, j=j: e.bn_stats(out=st[:rows, j, :], in_=xt[:rows, j * 512:(j + 1) * 512]),
                 [bx], [bst])
        S.op("dve", lambda e: e.bn_aggr(out=mv[:rows, 0:2], in_=st[:rows].rearrange("p a b -> p (a b)")),
             [bst], [bmv])
        S.op("act", lambda e: e.activation(out=mv[:rows, 2:3], in_=mv[:rows, 1:2], func=AF.Sqrt,
                                           bias=eps_t[:rows, :], scale=1.0), [bmv] + CONST, [bmv])
        S.op("dve", lambda e: e.reciprocal(out=mv[:rows, 3:4], in_=mv[:rows, 2:3]), [bmv], [bmv])
        S.op("dve", lambda e: e.tensor_scalar(out=out_f32[:rows], in0=xt[:rows], scalar1=mv[:rows, 0:1],
                                              scalar2=mv[:rows, 3:4], op0=ALU.subtract, op1=ALU.mult),
             [bx, bmv], [b_out])
        S.op("pool", lambda e: e.tensor_tensor(out=out_f32[:rows], in0=out_f32[:rows], in1=g_t[:rows],
                                               op=ALU.mult), [b_out] + gb_bufs, [b_out])
        S.op("dve", lambda e: e.tensor_tensor(out=out_f32[:rows], in0=out_f32[:rows], in1=b_t[:rows],
                                              op=ALU.add), [b_out] + gb_bufs, [b_out])

    def transpose_to(xb, bxb, rows, dstT, bdst, col0):
        for half in range(2):
            pt, bpt = P["b"].next()
            for j in range(8):
                kc = half * 8 + j
                S.op("pe", lambda e, kc=kc, j=j, pt=pt: e.transpose(
                    out=pt[:, j * 128:j * 128 + rows], in_=xb[:rows, kc * 128:(kc + 1) * 128],
                    identity=ident_b[:rows, :rows]), [bxb] + CONST, [bpt])
            S.op("act", lambda e, half=half, pt=pt: e.activation(
                out=dstT[:, half * 8:(half + 1) * 8, col0:col0 + rows],
                in_=pt[:].rearrange("p (j t) -> p j t", t=128)[:, :, 0:rows], func=AF.Copy),
                [bpt], [bdst])

    def mm_acc(pt_ap, bpt, w, bw, wc, rhsT, brhs, c0, n):
        for kc in range(KC):
            S.op("pe", lambda e, kc=kc: e.matmul(
                pt_ap, lhsT=w[:, kc, wc * 128:(wc + 1) * 128], rhs=rhsT[:, kc, c0:c0 + n],
                start=(kc == 0), stop=(kc == KC - 1)), [bw, brhs], [bpt])

    def mm_own(pt, bpt, w, bw, wc, rhsT, brhs, c0, n):
        for kc in range(KC):
            S.op("pe", lambda e: e.matmul(
                pt[:, 0:2 * n].rearrange("p (a b) -> p a b", b=n), lhsT=w[:, kc, wc * 128:(wc + 1) * 128],
                rhs=rhsT[:, kc, :].rearrange("p (a b) -> p a b", b=256)[:, :, c0:c0 + n],
                start=(kc == 0), stop=(kc == KC - 1)), [bw, brhs], [bpt])

    def load_w(w_r, src_bf, bsrc, col0, ncols=512):
        w, bw = w_r.next()
        S.dma("sp", lambda e: e.dma_start(
            out=w[:, :, 0:ncols], in_=src_bf[:, :, col0:col0 + ncols].rearrange("k p c -> p k c")),
            bsrc, bw, bw)
        return w, bw

    with contextlib.ExitStack() as ph:
        stage_r = Ring([sb("cst%d" % i, [128, 2048], BF16, ph) for i in range(3)], "cst")

        def precast(src, dst, ncols, dbuf):
            for kc in range(KC):
                for c0 in range(0, ncols, 2048):
                    cw = min(2048, ncols - c0)
                    t, b = stage_r.next()
                    S.dma("pool", lambda e: e.dma_start(
                        out=t[:, 0:cw], in_=src[kc * 128:(kc + 1) * 128, c0:c0 + cw]), B_ext, b, b)
                    S.dma("sp", lambda e: e.dma_start(
                        out=dst[kc, :, c0:c0 + cw], in_=t[:, 0:cw]), b, dbuf, b, append=True)

        precast(w_in, win_bf, N_IN, B_win)
        precast(w_co, wco_bf, D, B_wco)
        if nph < 4:
            precast(w_ao, wao_bf, D, B_wao)
            precast(w_o, wo_bf, D, B_wo)
            precast(w_q, wq_bf, D, B_wq)
        S.barrier()

    if nph >= 1:
      with contextlib.ExitStack() as ph:
        lng_t = sb("lng_t", [128, D], F32, ph)
        mkpsum(ph, 6, 0, 2)
        lnb_t = sb("lnb_t", [128, D], F32, ph)
        bvb_t = sb("bvb_t", [128, D], F32, ph)
        B_ln = Buf("lnconst")
        for dst_, src_ in ((lng_t, lnin_g), (lnb_t, lnin_b), (bvb_t, bv_b)):
            S.dma("sp", lambda e: e.dma_start(out=dst_[:], in_=src_), B_ext, B_ln, B_ln)
        x_r = Ring([sb("xt%d" % i, [128, D], F32, ph) for i in range(2)], "xt")
        xn_r = Ring([sb("xn%d" % i, [128, D], F32, ph) for i in range(2)], "xn")
        xb_r = Ring([sb("xb%d" % i, [128, D], BF16, ph) for i in range(2)], "xb")
        h0T_r = Ring([sb("h0T%d" % i, [128, KC, 512], BF16, ph) for i in range(2)], "h0T")
        w_r = Ring([sb("wst%d" % i, [128, KC, 512], BF16, ph) for i in range(2)], "wst")
        wf_t = sb("wf_t", [128, KC, 16], BF16, ph)
        B_wf = Buf("wf")
        S.dma("sp", lambda e: e.dma_start(out=wf_t[:], in_=win_bf[:, :, OFF_F:OFF_F + 16].rearrange("k p c -> p k c")),
              B_win, B_wf, B_wf)
        sig_r = Ring([sb("sig%d" % i, [128, 4, 320], F32, ph) for i in range(2)], "sig")
        ev_r = Ring([sb("ev%d" % i, [128, 4, 512], BF16, ph) for i in range(3)], "ev")
        lf_r = Ring([sb("lft%d" % i, [16, 512], F32, ph) for i in range(2)], "lft")

        def group_proj(h0T, bh, gi, ntok, meta):
            ktok0 = 0 if meta else N_META + gi * 512
            own = [] if meta else [(2 * gi, 1), (2 * gi + 1, 3)]
            if not meta:
                for j in range(4):
                    wg, bwg = load_w(w_r, win_bf, B_win, OFF_G + j * 512)
                    sg, bsg = sig_r.next()
                    for c in range(4):
                        pt, bpt = P["f"].next()
                        mm_own(pt, bpt, wg, bwg, c, h0T, bh, 96, 160)
                        bc = 16 + j * 4 + c
                        S.op("act", lambda e: e.activation(
                            out=sg[:, c, :], in_=pt[:, 0:320], func=AF.Sigmoid,
                            bias=bfm[:, bc:bc + 1], scale=1.0), [bpt] + CONST, [bsg])
                    wa, bwa = load_w(w_r, win_bf, B_win, OFF_A + j * 512)
                    ev, bev = ev_r.next()
                    for c in range(4):
                        pt, bpt = P["f"].next()
                        mm_own(pt, bpt, wa, bwa, c, h0T, bh, 96, 160)
                        bc = j * 4 + c
                        S.op("dve", lambda e: e.scalar_tensor_tensor(
                            out=ev[:, c, 0:320], in0=pt[:, 0:320], scalar=bfm[:, bc:bc + 1],
                            in1=sg[:, c, :], op0=ALU.add, op1=ALU.mult), [bpt, bsg] + CONST, [bev])
                    if gi == 0:
                        S.op("pool", lambda e: e.tensor_tensor(
                            out=ev[:, :, 0:32], in0=ev[:, :, 0:32],
                            in1=hmask_t[:].unsqueeze(1).to_broadcast([128, 4, 32]), op=ALU.mult),
                            [bev] + CONST, [bev])
                    for (ob, _), o in zip(own, (0, 160)):
                        S.dma("pool", lambda e: e.dma_start(
                            out=uT_d[j * 4:(j + 1) * 4, :, ob, :].rearrange("c p t -> p c t"),
                            in_=ev[:, :, o:o + 160]), bev, B_uT, bev, append=True)
                for (off, dst, bdst, func, bcol0) in ((OFF_Q, qT_d, B_qT, AF.Identity, 32),
                                                       (OFF_GC, gcT_d, B_gc, AF.Sigmoid, 80),
                                                       (OFF_GA, gaT_d, B_ga, AF.Sigmoid, 96)):
                    for j in range(4):
                        w, bw = load_w(w_r, win_bf, B_win, off + j * 512)
                        ev, bev = ev_r.next()
                        for c in range(4):
                            pt, bpt = P["f"].next()
                            mm_own(pt, bpt, w, bw, c, h0T, bh, 128, 128)
                            bc = bcol0 + j * 4 + c
                            S.op("act", lambda e: e.activation(
                                out=ev[:, c, 0:256], in_=pt[:, 0:256], func=func,
                                bias=bfm[:, bc:bc + 1], scale=1.0), [bpt] + CONST, [bev])
                        S.dma("pool", lambda e: e.dma_start(
                            out=dst[j * 4:(j + 1) * 4, :, gi * 256:(gi + 1) * 256].rearrange("c p t -> p c t"),
                            in_=ev[:, :, 0:256]), bev, bdst, bev, append=True)
            for j in range(4):
                w, bw = load_w(w_r, win_bf, B_win, OFF_K + j * 512)
                ev, bev = ev_r.next()
                for c in range(4):
                    pt, bpt = P["f"].next()
                    mm_acc(pt[:, 0:ntok], bpt, w, bw, c, h0T, bh, 0, ntok)
                    bc = 48 + j * 4 + c
                    S.op("dve", lambda e: e.tensor_scalar(
                        out=ev[:, c, 0:ntok], in0=pt[:, 0:ntok], scalar1=bfm[:, bc:bc + 1], scalar2=None,
                        op0=ALU.add), [bpt] + CONST, [bev])
                S.dma("pool", lambda e: e.dma_start(
                    out=kT_d[j * 4:(j + 1) * 4, :, ktok0:ktok0 + ntok].rearrange("c p t -> p c t"),
                    in_=ev[:, :, 0:ntok]), bev, B_kT, bev, append=True)
            ntile = 1 if meta else 4
            rows = ntok if meta else 128
            for j in range(4):
                w, bw = load_w(w_r, win_bf, B_win, OFF_V + j * 512)
                ev, bev = ev_r.next()
                for t in range(ntile):
                    pt, bpt = P["f"].next()
                    for kc in range(KC):
                        S.op("pe", lambda e: e.matmul(
                            pt[:rows, :], lhsT=h0T[:, kc, t * 128:t * 128 + rows], rhs=w[:, kc, :],
                            start=(kc == 0), stop=(kc == KC - 1)), [bw, bh], [bpt])
                    S.op("dve", lambda e: e.tensor_tensor(
                        out=ev[:rows, t, :], in0=pt[:rows, :], in1=bvb_t[:rows, j * 512:(j + 1) * 512],
                        op=ALU.add), [bpt, B_ln], [bev])
                if meta:
                    S.dma("pool", lambda e: e.dma_start(
                        out=v_d[0:rows, j * 512:(j + 1) * 512], in_=ev[:rows, 0, :]), bev, B_v, bev, append=True)
                else:
                    S.dma("pool", lambda e: e.dma_start(
                        out=v_d[ktok0:ktok0 + 512, j * 512:(j + 1) * 512].rearrange("(t p) c -> p t c", p=128),
                        in_=ev[:, :, :]), bev, B_v, bev, append=True)
            pt, bpt = P["f"].next()
            for kc in range(KC):
                S.op("pe", lambda e: e.matmul(
                    pt[:16, 0:ntok], lhsT=wf_t[:, kc, :], rhs=h0T[:, kc, 0:ntok],
                    start=(kc == 0), stop=(kc == KC - 1)), [B_wf, bh], [bpt])
            lt, blt = lf_r.next()
            S.op("act", lambda e: e.activation(out=lt[:, 0:ntok], in_=pt[:16, 0:ntok], func=AF.Exp,
                                               bias=nbf[:, :], scale=-1.0), [bpt] + CONST, [blt])
            S.op("act", lambda e: e.activation(out=lt[:, 0:ntok], in_=lt[:, 0:ntok], func=AF.Ln,
                                               bias=one_t[:16, :], scale=1.0), [blt] + CONST, [blt])
            S.op("dve", lambda e: e.tensor_scalar(out=lt[:, 0:ntok], in0=lt[:, 0:ntok],
                                                  scalar1=-1.0, scalar2=None, op0=ALU.mult), [blt], [blt])
            S.dma("pool", lambda e: e.dma_start(out=logf_d[:, ktok0:ktok0 + ntok], in_=lt[:, 0:ntok]),
                  blt, B_lf, blt, append=True)

        def ln_in_tile(src_ap, rows, h0T, bh, col0, own_block):
            xt, bx = x_r.next()
            S.dma("sp", lambda e: e.dma_start(out=xt[:rows], in_=src_ap), B_ext, bx, bx)
            xn, bxn = xn_r.next()
            layer_norm_tile(xt, bx, rows, lng_t, lnb_t, [B_ln], xn, bxn)
            if own_block is not None:
                S.dma("pool", lambda e: e.dma_start(out=h0_d[own_block * 128:(own_block + 1) * 128, :],
                                                    in_=xn[:]), bxn, B_h0, bxn, append=True)
            xb, bxb = xb_r.next()
            S.op("act", lambda e: e.activation(out=xb[:rows], in_=xn[:rows], func=AF.Copy), [bxn], [bxb])
            transpose_to(xb, bxb, rows, h0T, bh, col0)

        h0T, bh = h0T_r.next()
        ln_in_tile(xmeta, N_META, h0T, bh, 0, None)
        group_proj(h0T, bh, 0, N_META, True)
        for gi in range(NG):
            h0T, bh = h0T_r.next()
            for t in range(4):
                at = gi * 4 + t
                ln_in_tile(xa[at * 128:(at + 1) * 128, :], 128, h0T, bh, t * 128,
                           (at // 2) if (at % 2 == 1) else None)
            group_proj(h0T, bh, gi, 512, False)
        S.barrier()

    if nph >= 2:
      with contextlib.ExitStack() as ph:
        CH = 2048
        mkpsum(ph, 4, 0, 0)
        zeros16 = sb("zeros16", [16, CH], F32, ph)
        B_z16 = Buf("z16")
        S.op("pool", lambda e: e.memset(zeros16[:], 0.0), [], [B_z16])
        lf_r2 = Ring([sb("lfc%d" % i, [16, CH], F32, ph) for i in range(2)], "lfc")
        val_r = Ring([sb("val%d" % i, [16, CH], F32, ph) for i in range(2)], "val")
        ct_r = Ring([sb("ctc%d" % i, [16, CH], F32, ph) for i in range(2)], "ctc")
        lfm, blfm = lf_r2.next()
        S.dma("sp", lambda e: e.dma_start(out=lfm[:, 0:N_META], in_=logf_d[:, 0:N_META]), B_lf, blfm, blfm)
        ctp, bctp = ct_r.next()
        S.op("dve", lambda e: e.tensor_tensor_scan(out=ctp[:, 0:N_META], data0=lfm[:, 0:N_META],
                                                   data1=zeros16[:, 0:N_META], initial=0.0,
                                                   op0=ALU.add, op1=ALU.add), [blfm, B_z16], [bctp])
        S.dma("pool", lambda e: e.dma_start(out=cT_d[:, 0:N_META], in_=ctp[:, 0:N_META]), bctp, B_cT, bctp,
              append=True)
        pt, bpt = P["f"].next()
        S.op("pe", lambda e: e.matmul(pt[:16, 0:16], lhsT=ctp[:, 0:N_META], rhs=ident_f[0:16, 0:16],
                                      start=True, stop=True), [bctp] + CONST, [bpt])
        S.op("dve", lambda e: e.tensor_scalar(out=negckm[:, :], in0=pt[:16, 0:16], scalar1=-1.0,
                                              scalar2=None, op0=ALU.mult), [bpt], [B_nck])
        prev_last = ctp[:, N_META - 1:N_META]
        bprev = bctp
        for c0 in range(0, NTOK, CH):
            cw = min(CH, NTOK - c0)
            a0 = N_META + c0
            lf, blf = lf_r2.next()
            S.dma("sp", lambda e: e.dma_start(out=lf[:, 0:cw], in_=logf_d[:, a0:a0 + cw]), B_lf, blf, blf)
            vt, bvt = val_r.next()
            S.dma("sp", lambda e: e.dma_start(out=vt[:, 0:cw], in_=valid16[:, c0:c0 + cw]), B_ext, bvt, bvt)
            S.op("dve", lambda e: e.tensor_tensor(out=lf[:, 0:cw], in0=lf[:, 0:cw], in1=vt[:, 0:cw],
                                                  op=ALU.mult), [bvt, blf], [blf])
            ct, bct = ct_r.next()
            S.op("dve", lambda e: e.tensor_tensor_scan(
                out=ct[:, 0:cw], data0=lf[:, 0:cw], data1=zeros16[:, 0:cw], initial=prev_last,
                op0=ALU.add, op1=ALU.add), [blf, B_z16, bprev], [bct])
            S.dma("pool", lambda e: e.dma_start(out=cT_d[:, a0:a0 + cw], in_=ct[:, 0:cw]), bct, B_cT, bct,
                  append=True)
            nt = cw // 128
            pt, bpt = P["f"].next()
            for t in range(nt):
                S.op("pe", lambda e: e.matmul(
                    pt[:, t * 16:(t + 1) * 16], lhsT=ct[:, t * 128:(t + 1) * 128],
                    rhs=ident_f[0:16, 0:16], start=True, stop=True), [bct] + CONST, [bpt])
            t0 = c0 // 128
            S.op("dve", lambda e: e.tensor_scalar(
                out=negck[:, t0:t0 + nt, :].rearrange("p t h -> p (t h)"), in0=pt[:, 0:nt * 16], scalar1=-1.0,
                scalar2=None, op0=ALU.mult), [bpt], [B_nck])
            prev_last = ct[:, cw - 1:cw]
            bprev = bct
        S.op("dve", lambda e: e.tensor_scalar(out=negck[:, 0, :], in0=negck[:, 0, :], scalar1=kb0_t[:, 0:1],
                                              scalar2=None, op0=ALU.add), [B_nck] + CONST, [B_nck])
        S.barrier()

    if nph >= 3:
      with contextlib.ExitStack() as ph:
        convT = sb("convT", [128, KC, 512], F32, ph)
        mkpsum(ph, 6, 2, 0)
        cb = sb("cb", [128, KC, 512], BF16, ph)
        sq = sb("sq", [128, KC, 512], BF16, ph)
        aT = sb("aT", [128, KC, 512], BF16, ph)
        B_conv, B_cb, B_sq, B_aT = Buf("convT"), Buf("cb"), Buf("sq"), Buf("aT")
        ut_r = Ring([sb("ut%d" % i, [128, 4, 160], BF16, ph) for i in range(2)], "ut")
        dg_r = Ring([sb("dg%d" % i, [128, TAPS, 128], BF16, ph) for i in range(2)], "dg")
        w_r = Ring([sb("wstc%d" % i, [128, KC, 512], BF16, ph) for i in range(2)], "wstc")
        gc_r = Ring([sb("gct%d" % i, [128, 4, 512], BF16, ph) for i in range(2)], "gct")
        ev_r = Ring([sb("evc%d" % i, [128, 4, 512], BF16, ph) for i in range(2)], "evc")
        mean_sb = sb("mean_sb", [128, 512], F32, ph)
        rstd_sb = sb("rstd_sb", [128, 512], F32, ph)
        m2_sb = sb("m2_sb", [128, 512], F32, ph)
        B_mean, B_rstd, B_m2 = Buf("mean"), Buf("rstd"), Buf("m2")
        xc_r = Ring([sb("xc%d" % i, [128, 512], F32, ph) for i in range(3)], "xc")
        for og in range(NOG):
            pmean, bpmean = P["o"].next()
            pex2, bpex2 = P["o"].next()
            for cc in range(KC):
                ut, but = ut_r.next()
                S.dma("sp", lambda e: e.dma_start(out=ut[:], in_=uT_d[cc, :, og * 4:(og + 1) * 4, :]),
                      B_uT, but, but)
                dg, bdg = dg_r.next()
                S.op("pool", lambda e: e.tensor_tensor(
                    out=dg[:], in0=ident_b[:].unsqueeze(1).to_broadcast([128, TAPS, 128]),
                    in1=dww_t[:, cc, :].unsqueeze(2).to_broadcast([128, TAPS, 128]), op=ALU.mult),
                    CONST, [bdg])
                pt, bpt = P["f"].next()
                for tap in range(TAPS):
                    S.op("pe", lambda e: e.matmul(
                        pt[:].rearrange("p (a b) -> p a b", b=128), lhsT=dg[:, tap, :],
                        rhs=ut[:, :, 2 + tap:2 + tap + 128], start=(tap == 0), stop=(tap == TAPS - 1)),
                        [bdg, but], [bpt])
                S.op("act", lambda e: e.activation(out=convT[:, cc, :], in_=pt[:], func=AF.Identity,
                                                   bias=dwb_t[:, cc:cc + 1], scale=1.0),
                     [bpt] + CONST, [B_conv])
                S.op("dve", lambda e: e.tensor_copy(out=cb[:, cc, :], in_=convT[:, cc, :]), [B_conv], [B_cb])
                S.op("act", lambda e: e.activation(out=sq[:, cc, :], in_=pt[:], func=AF.Square,
                                                   bias=dwb_t[:, cc:cc + 1], scale=1.0),
                     [bpt] + CONST, [B_sq])
                S.op("pe", lambda e: e.matmul(pmean[:], lhsT=onesm[:], rhs=cb[:, cc, :],
                                              start=(cc == 0), stop=(cc == KC - 1)), [B_cb] + CONST, [bpmean])
                S.op("pe", lambda e: e.matmul(pex2[:], lhsT=onesm[:], rhs=sq[:, cc, :],
                                              start=(cc == 0), stop=(cc == KC - 1)), [B_sq] + CONST, [bpex2])
            S.op("act", lambda e: e.activation(out=mean_sb[:], in_=pmean[:], func=AF.Copy), [bpmean], [B_mean])
            S.op("dve", lambda e: e.tensor_tensor(out=m2_sb[:], in0=mean_sb[:], in1=mean_sb[:], op=ALU.mult),
                 [B_mean], [B_m2])
            S.op("dve", lambda e: e.tensor_tensor(out=m2_sb[:], in0=pex2[:], in1=m2_sb[:], op=ALU.subtract),
                 [bpex2, B_m2], [B_m2])
            S.op("act", lambda e: e.activation(out=m2_sb[:], in_=m2_sb[:], func=AF.Sqrt, bias=eps_t[:, :],
                                               scale=1.0), [B_m2] + CONST, [B_m2])
            S.op("dve", lambda e: e.reciprocal(out=rstd_sb[:], in_=m2_sb[:]), [B_m2], [B_rstd])
            for cc in range(KC):
                xc, bxc = xc_r.next()
                S.op("dve", lambda e: e.tensor_tensor(out=xc[:], in0=convT[:, cc, :], in1=mean_sb[:],
                                                      op=ALU.subtract), [B_conv, B_mean], [bxc])
                S.op("dve", lambda e: e.tensor_tensor(out=xc[:], in0=xc[:], in1=rstd_sb[:], op=ALU.mult),
                     [bxc, B_rstd], [bxc])
                S.op("act", lambda e: e.activation(out=aT[:, cc, :], in_=xc[:], func=AF.Silu,
                                                   bias=clnb_t[:, cc:cc + 1], scale=clng_t[:, cc:cc + 1]),
                     [bxc] + CONST, [B_aT])
            for j in range(4):
                w, bw = load_w(w_r, wco_bf, B_wco, j * 512)
                gct, bgc = gc_r.next()
                S.dma("sp", lambda e: e.dma_start(
                    out=gct[:], in_=gcT_d[j * 4:(j + 1) * 4, :, og * 512:(og + 1) * 512].rearrange("c p t -> p c t")),
                    B_gc, bgc, bgc)
                ev, bev = ev_r.next()
                for c in range(4):
                    pt, bpt = P["f"].next()
                    mm_acc(pt[:], bpt, w, bw, c, aT, B_aT, 0, 512)
                    S.op("dve", lambda e: e.tensor_tensor(out=ev[:, c, :], in0=pt[:], in1=gct[:, c, :],
                                                          op=ALU.mult), [bpt, bgc], [bev])
                S.dma("pool", lambda e: e.dma_start(
                    out=ycg_d[j * 4:(j + 1) * 4, :, og * 512:(og + 1) * 512].rearrange("c p t -> p c t"),
                    in_=ev[:]), bev, B_ycg, bev, append=True)
        S.barrier()

    if nph >= 4:
      with contextlib.ExitStack() as ph:
        LA = 3
        mkpsum(ph, LA + 1, 4, 0)
        kT_r = Ring([sb("kTh%d" % i, [128, LK], BF16, ph) for i in range(2)], "kTh")
        v_r = Ring([sb("vh%d" % i, [128, AT, 129], BF16, ph) for i in range(2)], "vh")
        vm_r = Ring([sb("vm%d" % i, [16, 129], BF16, ph) for i in range(2)], "vm")
        q_r = Ring([sb("qTh%d" % i, [128, NOWN], BF16, ph) for i in range(2)], "qTh")
        cq_r = Ring([sb("cqb%d" % i, [128, NB, 128], F32, ph) for i in range(2)], "cqb")
        o_r = Ring([sb("oTh%d" % i, [128, NB, 128], BF16, ph) for i in range(2)], "oTh")
        tmp_r = Ring([sb("atmp%d" % i, [128, 512], F32, ph) for i in range(6)], "atmp")
        pT_r = Ring([sb("pT%d" % i, [128, 512], BF16, ph) for i in range(7)], "pT")
        rc_r = Ring([sb("rc%d" % i, [128, 1], F32, ph) for i in range(3)], "rc")
        for i in range(2):
            S.op("pool", lambda e: e.memset(v_r.tiles[i][:, :, 128:129], 1.0), [], [v_r.bufs[i]])
            S.op("pool", lambda e: e.memset(vm_r.tiles[i][:, 128:129], 1.0), [], [vm_r.bufs[i]])
        scale = float(HD) ** -0.5
        NGQ = NB // 4
        dst_r = Ring([sb("dcst%d" % i, [128, 2 * D], BF16, ph) for i in range(2)], "dcst")
        tasks = []

        def mk_w(src, dst, dbuf, kc):
            def f():
                t, b = dst_r.next()
                S.dma("pool", lambda e: e.dma_start(out=t[:, 0:D], in_=src[kc * 128:(kc + 1) * 128, :]), B_ext, b, b)
                S.dma("sp", lambda e: e.dma_start(out=dst[kc, :, :], in_=t[:, 0:D]), b, dbuf, b, append=True)
            return f

        def mk_uv(r0):
            def f():
                t, b = dst_r.next()
                S.dma("pool", lambda e: e.dma_start(out=t[:, 0:D], in_=tab_u[r0:r0 + 128, :]), B_ext, b, b)
                S.dma("pool", lambda e: e.dma_start(out=t[:, D:2 * D], in_=tab_v[r0:r0 + 128, :]), B_ext, b, b)
                S.dma("sp", lambda e: e.dma_start(out=uv_bf[r0:r0 + 128, :], in_=t[:]), b, B_uv, b, append=True)
            return f

        for (src, dst, dbuf) in ((w_ao, wao_bf, B_wao), (w_o, wo_bf, B_wo), (w_q, wq_bf, B_wq)):
            for kc in range(KC):
                tasks.append(mk_w(src, dst, dbuf, kc))
        if nph >= 7:
            for r0 in range(0, NEXP, 128):
                tasks.append(mk_uv(r0))
        items = []
        for h in range(NH):
            for g in range(NGQ):
                items.append((h, g, -1))
                for kt in range(8 * g + 8):
                    items.append((h, g, kt))
        HS = {}
        SP_ = {}
        ACC = {}

        def head_load(h):
            kT, bk = kT_r.next()
            S.dma("sp", lambda e: e.dma_start(out=kT[:], in_=kT_d[h]), B_kT, bk, bk)
            vh, bvh = v_r.next()
            for t0 in range(0, AT, 8):
                S.dma("sp", lambda e: e.dma_start(
                    out=vh[:, t0:t0 + 8, 0:128],
                    in_=v_d[N_META + t0 * 128:N_META + (t0 + 8) * 128, h * 128:(h + 1) * 128]
                    .rearrange("(t p) c -> p t c", p=128)), B_v, bvh, bvh)
            vm, bvm = vm_r.next()
            S.dma("sp", lambda e: e.dma_start(out=vm[:, 0:128], in_=v_d[0:N_META, h * 128:(h + 1) * 128]),
                  B_v, bvm, bvm)
            qT, bq = q_r.next()
            S.dma("sp", lambda e: e.dma_start(out=qT[:], in_=qT_d[h]), B_qT, bq, bq)
            cq, bcq = cq_r.next()
            for b0 in range(0, NB, 16):
                b1 = min(NB, b0 + 16)
                S.dma("sp", lambda e: e.dma_start(
                    out=cq[:, b0:b1, :],
                    in_=cT_d[h, N_META:LK].rearrange("(b two q) -> b two q", two=2, q=128)[b0:b1, 1, :]
                    .partition_broadcast(128)), B_cT, bcq, bcq)
            oT, bo = o_r.next()
            HS[h] = (kT, bk, vh, bvh, vm, bvm, qT, bq, cq, bcq, oT, bo)

        def jmin_of(g, kt):
            return 0 if kt < 0 else max(0, (kt - 8 * g) // 2)

        def emit_S(n):
            h, g, kt = items[n]
            if h not in HS:
                head_load(h)
            kT, bk, vh, bvh, vm, bvm, qT, bq, cq, bcq, oT, bo = HS[h]
            pt, bpt = P["f"].next()
            SP_[n] = (pt, bpt)
            c0 = jmin_of(g, kt) * 128
            if kt < 0:
                S.op("pe", lambda e: e.matmul(pt[:16, 0:512], lhsT=kT[:, 0:N_META],
                                              rhs=qT[:, g * 512:(g + 1) * 512], start=True, stop=True),
                     [bk, bq], [bpt])
            else:
                S.op("pe", lambda e: e.matmul(
                    pt[:, c0:512], lhsT=kT[:, N_META + kt * 128:N_META + (kt + 1) * 128],
                    rhs=qT[:, g * 512 + c0:(g + 1) * 512], start=True, stop=True), [bk, bq], [bpt])

        def emit_post(n):
            h, g, kt = items[n]
            kT, bk, vh, bvh, vm, bvm, qT, bq, cq, bcq, oT, bo = HS[h]
            pt, bpt = SP_.pop(n)
            rows = 16 if kt < 0 else 128
            jm = jmin_of(g, kt)
            c0 = jm * 128
            cqg = cq[:, 4 * g:4 * g + 4, :].rearrange("p a b -> p (a b)")
            tmp, btmp = tmp_r.next()
            S.op("dve", lambda e: e.scalar_tensor_tensor(
                out=tmp[:rows, c0:512], in0=pt[:rows, c0:512], scalar=scale, in1=cqg[:rows, c0:512],
                op0=ALU.mult, op1=ALU.add), [bpt, bcq], [btmp])
            if kt >= 8 * g and (kt - 8 * g) % 2 == 1:
                S.op("dve", lambda e: e.tensor_tensor(out=tmp[:, c0:c0 + 128], in0=tmp[:, c0:c0 + 128],
                                                      in1=trineg[:], op=ALU.add), [btmp] + CONST, [btmp])
            pT, bpT = pT_r.next()
            bias = negckm[:, h:h + 1] if kt < 0 else negck[:, kt, h:h + 1]
            S.op("act", lambda e: e.activation(out=pT[:rows, c0:512], in_=tmp[:rows, c0:512], func=AF.Exp,
                                               bias=bias, scale=1.0), [btmp, B_nck], [bpT])
            if kt < 0:
                ACC[(h, g)] = [P["o"].next() for _ in range(4)]
            accs = ACC[(h, g)]
            for j in range(jm, 4):
                i = 4 * g + j
                po, bpo = accs[j]
                last = (kt == 2 * i + 1)
                if kt < 0:
                    S.op("pe", lambda e: e.matmul(po[:, 0:129], lhsT=pT[:16, j * 128:(j + 1) * 128],
                                                  rhs=vm[:, :], start=True, stop=False), [bpT, bvm], [bpo])
                else:
                    S.op("pe", lambda e: e.matmul(po[:, 0:129], lhsT=pT[:, j * 128:(j + 1) * 128],
                                                  rhs=vh[:, kt, :], start=False, stop=last), [bpT, bvh], [bpo])
                if last:
                    rc, brc = rc_r.next()
                    S.op("dve", lambda e: e.reciprocal(out=rc[:], in_=po[:, 128:129]), [bpo], [brc])
                    S.op("dve", lambda e: e.tensor_scalar(out=oT[:, i, :], in0=po[:, 0:128], scalar1=rc[:, 0:1],
                                                          scalar2=None, op0=ALU.mult), [bpo, brc], [bo])
            if g == NGQ - 1 and kt == 8 * g + 7:
                for b0 in range(0, NB, 8):
                    b1 = min(NB, b0 + 8)
                    S.dma("pool", lambda e: e.dma_start(
                        out=o_d[b0 * 128:b1 * 128, h * 128:(h + 1) * 128].rearrange("(b p) c -> p b c", p=128),
                        in_=oT[:, b0:b1, :]), bo, B_oT, bo, append=True)
                del HS[h]

        NI = len(items)
        every = max(1, (NI * 3 // 4) // max(1, len(tasks)))
        for n in range(NI + LA):
            if n < NI:
                emit_S(n)
            if n >= LA:
                emit_post(n - LA)
            if tasks and n % every == 0:
                tasks.pop(0)()
        while tasks:
            tasks.pop(0)()
        S.barrier()

    if nph >= 5:
      with contextlib.ExitStack() as ph:
        l1g = sb("l1g", [128, D], F32, ph)
        mkpsum(ph, 6, 0, 2)
        ot_r = Ring([sb("otk%d" % i, [128, D], BF16, ph) for i in range(2)], "otk")
        l1b = sb("l1b", [128, D], F32, ph)
        B_l1 = Buf("l1")
        for dst_, src_ in ((l1g, ln1_g), (l1b, ln1_b)):
            S.dma("sp", lambda e: e.dma_start(out=dst_[:], in_=src_), B_ext, B_l1, B_l1)
        oT_r = Ring([sb("oTg%d" % i, [128, KC, 512], BF16, ph) for i in range(1)], "oTg")
        mixT = sb("mixT", [128, KC, 512], BF16, ph)
        B_mix = Buf("mixT")
        w_r = Ring([sb("wste%d" % i, [128, KC, 512], BF16, ph) for i in range(2)], "wste")
        ga_r = Ring([sb("gat%d" % i, [128, 4, 512], BF16, ph) for i in range(2)], "gat")
        yc_r = Ring([sb("yct%d" % i, [128, 4, 512], BF16, ph) for i in range(2)], "yct")
        tf_r = Ring([sb("tfe%d" % i, [128, 512], F32, ph) for i in range(2)], "tfe")
        h0_r = Ring([sb("h0t%d" % i, [128, D], F32, ph) for i in range(2)], "h0t")
        hp_r = Ring([sb("h1p%d" % i, [128, D], F32, ph) for i in range(2)], "h1p")
        h1_r = Ring([sb("h1t%d" % i, [128, D], F32, ph) for i in range(2)], "h1t")
        for og in range(NOG):
            oTg, boT = oT_r.next()
            for t in range(4):
                ob = og * 4 + t
                otk, botk = ot_r.next()
                S.dma("sp", lambda e: e.dma_start(out=otk[:], in_=o_d[ob * 128:(ob + 1) * 128, :]),
                      B_oT, botk, botk)
                transpose_to(otk, botk, 128, oTg, boT, t * 128)
            for j in range(4):
                w, bw = load_w(w_r, wao_bf, B_wao, j * 512)
                gat, bga = ga_r.next()
                S.dma("sp", lambda e: e.dma_start(
                    out=gat[:], in_=gaT_d[j * 4:(j + 1) * 4, :, og * 512:(og + 1) * 512].rearrange("c p t -> p c t")),
                    B_ga, bga, bga)
                yct, byc = yc_r.next()
                S.dma("sp", lambda e: e.dma_start(
                    out=yct[:], in_=ycg_d[j * 4:(j + 1) * 4, :, og * 512:(og + 1) * 512].rearrange("c p t -> p c t")),
                    B_ycg, byc, byc)
                for c in range(4):
                    pt, bpt = P["f"].next()
                    mm_acc(pt[:], bpt, w, bw, c, oTg, boT, 0, 512)
                    tf, btf = tf_r.next()
                    S.op("dve", lambda e: e.tensor_tensor(out=tf[:], in0=pt[:], in1=gat[:, c, :], op=ALU.mult),
                         [bpt, bga], [btf])
                    S.op("pool", lambda e: e.tensor_tensor(out=mixT[:, j * 4 + c, :], in0=tf[:],
                                                           in1=yct[:, c, :], op=ALU.add), [btf, byc], [B_mix])
            for t in range(4):
                ob = og * 4 + t
                h0t, bh0 = h0_r.next()
                S.dma("sp", lambda e: e.dma_start(out=h0t[:], in_=h0_d[ob * 128:(ob + 1) * 128, :]),
                      B_h0, bh0, bh0)
                hp, bhp = hp_r.next()
                for j in range(4):
                    w, bw = load_w(w_r, wo_bf, B_wo, j * 512)
                    pt, bpt = P["f"].next()
                    for kc in range(KC):
                        S.op("pe", lambda e: e.matmul(
                            pt[:], lhsT=mixT[:, kc, t * 128:(t + 1) * 128], rhs=w[:, kc, :],
                            start=(kc == 0), stop=(kc == KC - 1)), [bw, B_mix], [bpt])
                    S.op("dve", lambda e: e.scalar_tensor_tensor(
                        out=hp[:, j * 512:(j + 1) * 512], in0=h0t[:, j * 512:(j + 1) * 512], scalar=DN_ALPHA,
                        in1=pt[:], op0=ALU.mult, op1=ALU.add), [bpt, bh0], [bhp])
                h1t, bh1 = h1_r.next()
                layer_norm_tile(hp, bhp, 128, l1g, l1b, [B_l1], h1t, bh1)
                S.dma("pool", lambda e: e.dma_start(out=h1_d[ob * 128:(ob + 1) * 128, :], in_=h1t[:]),
                      bh1, B_h1, bh1, append=True)
        S.barrier()

    if nph >= 6:
      with contextlib.ExitStack() as ph:
        skT = sb("skT", [128, 16, 128], BF16, ph)
        mkpsum(ph, 6, 0, 2)
        B_sk = Buf("skT")
        sk_r = Ring([sb("skl%d" % i, [128, 128], F32, ph) for i in range(2)], "skl")
        for hp_ in range(16):
            skl, bskl = sk_r.next()
            S.dma("sp", lambda e: e.dma_start(out=skl[:], in_=subk[hp_]), B_ext, bskl, bskl)
            pt, bpt = P["f"].next()
            S.op("pe", lambda e: e.transpose(out=pt[:, 0:128], in_=skl[:], identity=ident_f[:]),
                 [bskl] + CONST, [bpt])
            S.op("act", lambda e: e.activation(out=skT[:, hp_, :], in_=pt[:, 0:128], func=AF.Copy),
                 [bpt], [B_sk])
        h1_r = Ring([sb("h1l%d" % i, [128, D], F32, ph) for i in range(2)], "h1l")
        xb_r = Ring([sb("h1b%d" % i, [128, D], BF16, ph) for i in range(2)], "h1b")
        h1T = sb("h1T", [128, KC, 512], BF16, ph)
        B_h1T = Buf("h1T")
        w_r = Ring([sb("wstq%d" % i, [128, KC, 512], BF16, ph) for i in range(2)], "wstq")
        qpT = sb("qpT", [128, 16, 512], BF16, ph)
        B_qp = Buf("qpT")
        s_r = Ring([sb("st%d" % i, [128, D], F32, ph) for i in range(2)], "st")
        for og in range(NOG):
            for t in range(4):
                ob = og * 4 + t
                h1l, bh1l = h1_r.next()
                S.dma("sp", lambda e: e.dma_start(out=h1l[:], in_=h1_d[ob * 128:(ob + 1) * 128, :]),
                      B_h1, bh1l, bh1l)
                xb, bxb = xb_r.next()
                S.op("act", lambda e: e.activation(out=xb[:], in_=h1l[:], func=AF.Copy), [bh1l], [bxb])
                transpose_to(xb, bxb, 128, h1T, B_h1T, t * 128)
            for j in range(4):
                w, bw = load_w(w_r, wq_bf, B_wq, j * 512)
                for c in range(4):
                    pt, bpt = P["f"].next()
                    mm_acc(pt[:], bpt, w, bw, c, h1T, B_h1T, 0, 512)
                    S.op("act", lambda e: e.activation(out=qpT[:, j * 4 + c, :], in_=pt[:], func=AF.Copy),
                         [bpt], [B_qp])
            for t in range(4):
                ob = og * 4 + t
                stl, bst_ = s_r.next()
                for hq in range(4):
                    pt, bpt = P["f"].next()
                    for c in range(4):
                        hp_ = hq * 4 + c
                        S.op("pe", lambda e: e.matmul(
                            pt[:, c * 128:(c + 1) * 128], lhsT=qpT[:, hp_, t * 128:(t + 1) * 128],
                            rhs=skT[:, hp_, :], start=True, stop=True), [B_qp, B_sk], [bpt])
                    S.op("act", lambda e: e.activation(out=stl[:, hq * 512:(hq + 1) * 512], in_=pt[:],
                                                       func=AF.Copy), [bpt], [bst_])
                S.dma("pool", lambda e: e.dma_start(out=s_d[ob * 128:(ob + 1) * 128, :], in_=stl[:]),
                      bst_, B_s, bst_, append=True)
        S.barrier()

    if nph >= 7:
      with contextlib.ExitStack() as ph:
        l2g = sb("l2g", [128, D], F32, ph)
        l2b = sb("l2b", [128, D], F32, ph)
        B_l2 = Buf("l2")
        for dst_, src_ in ((l2g, ln2_g), (l2b, ln2_b)):
            S.dma("sp", lambda e: e.dma_start(out=dst_[:], in_=src_), B_ext, B_l2, B_l2)
        s_r = Ring([sb("sf%d" % i, [128, 16, 128], F32, ph) for i in range(1)], "sf")
        h1_r = Ring([sb("h1f%d" % i, [128, D], F32, ph) for i in range(2)], "h1f")
        tkbuf = sb("tkbuf", [128, 2048], F32, ph)
        s2 = tkbuf[:].rearrange("p (a b) -> p a b", b=128)
        m16 = sb("m16", [128, 16, 16], F32, ph)
        ix16 = sb("ix16", [128, 16, 16], U32, ph)
        ixf = sb("ixf", [128, 16, 16], F32, ph)
        cand = sb("cand", [128, 8, 256], F32, ph)
        cand2 = tkbuf[:].rearrange("p (a b) -> p a b", b=256)
        vals = sb("vals", [128, 8, 16], F32, ph)
        posu = sb("posu", [128, 8, 16], U32, ph)
        posf = sb("posf", [128, 128], F32, ph)
        rf = sb("rf", [128, 128], F32, ph)
        cf = sb("cf", [128, 128], F32, ph)
        oh = tkbuf[:].rearrange("p (a b) -> p a b", b=16)
        If = sb("If", [128, 128], F32, ph)
        Jf = sb("Jf", [128, 128], F32, ph)
        idf = sb("idf", [128, 128], F32, ph)
        ids_r = Ring([sb("ids%d" % i, [128, 128], I32, ph) for i in range(2)], "ids")
        ev8 = sb("ev8", [128, 8, 16], F32, ph)
        z8 = sb("z8", [128, 8], F32, ph)
        g_r = Ring([sb("gk%d" % i, [128, 128], F32, ph) for i in range(2)], "gk")
        dots = sb("dots", [128, 128], F32, ph)
        wk_r = Ring([sb("wk%d" % i, [128, 128], F32, ph) for i in range(2)], "wk")
        mkpsum(ph, 0, 4, 0)
        gbuf_r = Ring([sb("gb%d" % i, [128, 2 * D], BF16, ph) for i in range(8)], "gb")
        junk_r = Ring([sb("junk%d" % i, [128, D // 2], BF16, ph) for i in range(2)], "junk")
        pr_r = Ring([sb("prd%d" % i, [128, D // 2], BF16, ph) for i in range(3)], "prd")
        jk2_r = Ring([sb("jk2_%d" % i, [128, D // 2], BF16, ph) for i in range(2)], "jk2")
        dots2 = sb("dots2", [128, 128], F32, ph)
        dg_r = Ring([sb("dgf%d" % i, [128, 128], BF16, ph) for i in range(4)], "dgf")
        dg1_r = Ring([sb("dgg%d" % i, [128, 128], F32, ph) for i in range(4)], "dgg")
        ak = sb("ak", [128, 128], F32, ph)
        wkt = sb("wkt", [128, 128], F32, ph)
        pre_r = Ring([sb("pre%d" % i, [128, D], F32, ph) for i in range(1)], "pre")
        o_r = Ring([sb("of%d" % i, [128, D], F32, ph) for i in range(1)], "of")
        colb = [Buf("col%d" % i) for i in range(4)]
        h1b_r = Ring([sb("h1bf%d" % i, [128, D], BF16, ph) for i in range(1)], "h1bf")
        B_tk, B_junk, B_dots = Buf("topk"), Buf("junk"), Buf("dots")
        TK = [B_tk]
        RES = {}

        def topk_gen(ob):
                sf, bsf = s_r.next()
                S.dma("sp", lambda e: e.dma_start(
                    out=sf[:], in_=s_d[ob * 128:(ob + 1) * 128, :].rearrange("p (a b) -> p a b", b=128)),
                    B_s, bsf, bsf)
                yield
                for hp_ in range(16):
                    S.op("dve", lambda e: e.max(out=m16[:, hp_, 0:8], in_=sf[:, hp_, :]), [bsf], TK)
                    S.op("dve", lambda e: e.match_replace(out=s2[:, hp_, :], in_to_replace=m16[:, hp_, 0:8],
                                                          in_values=sf[:, hp_, :], imm_value=-1e30), [bsf] + TK, TK)
                    S.op("dve", lambda e: e.max(out=m16[:, hp_, 8:16], in_=s2[:, hp_, :]), TK, TK)
                    S.op("dve", lambda e: e.max_index(out=ix16[:, hp_, 0:8], in_max=m16[:, hp_, 0:8],
                                                      in_values=sf[:, hp_, :]), [bsf] + TK, TK)
                    S.op("dve", lambda e: e.max_index(out=ix16[:, hp_, 8:16], in_max=m16[:, hp_, 8:16],
                                                      in_values=sf[:, hp_, :]), [bsf] + TK, TK)
                    yield
                S.op("dve", lambda e: e.tensor_copy(out=ixf[:], in_=ix16[:]), TK, TK)
                yield
                m4 = m16[:].rearrange("p (h s) k -> p h s k", s=2)
                ix4 = ixf[:].rearrange("p (h s) k -> p h s k", s=2)
                for h in range(8):
                    S.op("dve", lambda e: e.tensor_tensor(
                        out=cand[:, h, :].rearrange("p (r c) -> p r c", c=16),
                        in0=m4[:, h, 0, :].unsqueeze(2).to_broadcast([128, 16, 16]),
                        in1=m4[:, h, 1, :].unsqueeze(1).to_broadcast([128, 16, 16]), op=ALU.add), TK, TK)
                    yield
                for h in range(8):
                    S.op("dve", lambda e: e.max(out=vals[:, h, 0:8], in_=cand[:, h, :]), TK, TK)
                    S.op("dve", lambda e: e.match_replace(out=cand2[:, h, :], in_to_replace=vals[:, h, 0:8],
                                                          in_values=cand[:, h, :], imm_value=-1e30), TK, TK)
                    S.op("dve", lambda e: e.max(out=vals[:, h, 8:16], in_=cand2[:, h, :]), TK, TK)
                    S.op("dve", lambda e: e.max_index(out=posu[:, h, 0:8], in_max=vals[:, h, 0:8],
                                                      in_values=cand[:, h, :]), TK, TK)
                    S.op("dve", lambda e: e.max_index(out=posu[:, h, 8:16], in_max=vals[:, h, 8:16],
                                                      in_values=cand[:, h, :]), TK, TK)
                    yield
                S.op("dve", lambda e: e.tensor_copy(out=posf[:], in_=posu[:].rearrange("p h k -> p (h k)")), TK, TK)
                yield
                S.op("dve", lambda e: e.tensor_tensor(
                    out=oh[:], in0=posf[:].unsqueeze(2).to_broadcast([128, 128, 16]),
                    in1=thr16[:].unsqueeze(1).to_broadcast([128, 128, 16]), op=ALU.is_ge), TK + CONST, TK)
                yield
                S.op("dve", lambda e: e.reduce_sum(out=rf[:], in_=oh[:], axis=AX.X), TK, TK)
                yield
                S.op("dve", lambda e: e.scalar_tensor_tensor(out=cf[:], in0=rf[:], scalar=-16.0, in1=posf[:],
                                                             op0=ALU.mult, op1=ALU.add), TK, TK)
                yield
                for (src_rc, side, dstI) in ((rf, 0, If), (cf, 1, Jf)):
                    S.op("dve", lambda e: e.tensor_tensor(
                        out=oh[:], in0=src_rc[:].unsqueeze(2).to_broadcast([128, 128, 16]),
                        in1=iota16[:].unsqueeze(1).to_broadcast([128, 128, 16]), op=ALU.is_equal), TK + CONST, TK)
                    for h in range(8):
                        S.op("dve", lambda e: e.tensor_tensor(
                            out=oh[:, h * 16:(h + 1) * 16, :], in0=oh[:, h * 16:(h + 1) * 16, :],
                            in1=ix4[:, h, side, :].unsqueeze(1).to_broadcast([128, 16, 16]), op=ALU.mult), TK, TK)
                    yield
                    S.op("dve", lambda e: e.reduce_sum(out=dstI[:], in_=oh[:], axis=AX.X), TK, TK)
                    yield
                S.op("dve", lambda e: e.scalar_tensor_tensor(out=idf[:], in0=If[:], scalar=128.0, in1=Jf[:],
                                                             op0=ALU.mult, op1=ALU.add), TK, TK)
                yield
                ids, bids = ids_r.next()
                S.op("dve", lambda e: e.tensor_copy(out=ids[:], in_=idf[:]), TK, [bids])
                yield
                S.op("dve", lambda e: e.tensor_tensor(
                    out=ev8[:], in0=vals[:], in1=vals[:, :, 0:1].to_broadcast([128, 8, 16]), op=ALU.subtract),
                    TK, TK)
                yield
                S.op("act", lambda e: e.activation(out=ev8[:], in_=ev8[:], func=AF.Exp), TK, TK)
                yield
                S.op("dve", lambda e: e.reduce_sum(out=z8[:], in_=ev8[:], axis=AX.X), TK, TK)
                yield
                S.op("dve", lambda e: e.reciprocal(out=z8[:], in_=z8[:]), TK, TK)
                yield
                gk, bgk = g_r.next()
                S.op("dve", lambda e: e.tensor_tensor(
                    out=gk[:].rearrange("p (h k) -> p h k", k=16), in0=ev8[:],
                    in1=z8[:].unsqueeze(2).to_broadcast([128, 8, 16]), op=ALU.mult), TK, [bgk])
                yield
                RES[ob] = (ids, bids, gk, bgk)
                yield

        for _ in topk_gen(0):
            pass
        for ob in range(NB):
            ids, bids, gk, bgk = RES.pop(ob)
            nxt = topk_gen(ob + 1) if ob + 1 < NB else None
            h1f, bh1f = h1_r.next()
            S.dma("sp", lambda e: e.dma_start(out=h1f[:], in_=h1_d[ob * 128:(ob + 1) * 128, :]),
                  B_h1, bh1f, bh1f)
            h1b, bh1b = h1b_r.next()
            S.op("act", lambda e: e.activation(out=h1b[:], in_=h1f[:], func=AF.Copy), [bh1f], [bh1b])
            accs = [P["o"].next() for _ in range(4)]
            for k in range(128):
                gb, bgb = gbuf_r.next()
                S.dma("pool", lambda e: e.indirect_dma_start(
                    out=gb[:], out_offset=None, in_=uv_bf,
                    in_offset=bass.IndirectOffsetOnAxis(ap=ids[:, k:k + 1], axis=0)),
                    B_uv, bgb, bgb, extra_reads=[bids])
                jk, bjk = junk_r.next()
                cb_ = colb[k % 4]
                H2 = D // 2
                S.op("dve", lambda e: e.scalar_tensor_tensor(
                    out=jk[:], in0=gb[:, 0:H2], scalar=1.0, in1=h1b[:, 0:H2], op0=ALU.mult, op1=ALU.mult,
                    accum_out=dots[:, k:k + 1]), [bgb, bh1b], [bjk, cb_])
                pr, bpr = pr_r.next()
                S.op("dve", lambda e: e.tensor_tensor(out=pr[:], in0=gb[:, H2:D], in1=h1b[:, H2:D], op=ALU.mult),
                     [bgb, bh1b], [bpr])
                jk2, bjk2 = jk2_r.next()
                S.op("act", lambda e: e.activation(out=jk2[:], in_=pr[:], func=AF.Identity,
                                                   accum_out=dots2[:, k:k + 1]), [bpr], [bjk2, cb_])
                S.op("act", lambda e: e.activation(out=ak[:, k:k + 1], in_=dots[:, k:k + 1], func=AF.Gelu,
                                                   bias=dots2[:, k:k + 1], scale=1.0), [cb_], [cb_])
                S.op("act", lambda e: e.activation(out=wkt[:, k:k + 1], in_=ak[:, k:k + 1], func=AF.Identity,
                                                   scale=gk[:, k:k + 1]), [cb_, bgk], [cb_])
                dg, bdg = dg_r.next()
                S.op("act", lambda e: e.activation(out=dg[:], in_=ident_b[:], func=AF.Identity,
                                                   scale=wkt[:, k:k + 1]), [cb_] + CONST, [bdg])
                for j in range(4):
                    po, bpo = accs[j]
                    S.op("pe", lambda e: e.matmul(po[:, 0:512], lhsT=dg[:],
                                                  rhs=gb[:, D + j * 512:D + (j + 1) * 512],
                                                  start=(k == 0), stop=(k == 127)), [bdg, bgb], [bpo])
                if nxt is not None:
                    next(nxt, None)
            if nxt is not None:
                for _ in nxt:
                    pass
            pre, bpre = pre_r.next()
            for j in range(4):
                po, bpo = accs[j]
                S.op("dve", lambda e: e.scalar_tensor_tensor(
                    out=pre[:, j * 512:(j + 1) * 512], in0=h1f[:, j * 512:(j + 1) * 512], scalar=DN_ALPHA,
                    in1=po[:, 0:512], op0=ALU.mult, op1=ALU.add), [bh1f, bpo], [bpre])
            of, bof = o_r.next()
            layer_norm_tile(pre, bpre, 128, l2g, l2b, [B_l2], of, bof)
            S.dma("pool", lambda e: e.dma_start(out=out[ob * 128:(ob + 1) * 128, :], in_=of[:]),
                  bof, B_out, bof, append=True)
        S.barrier()

    S.final_wait("sp", [B_out])
    print("kernel build: %d instructions, %d dma sems" % (S.ninstr, S.nsem))
    stack.close()
    return nc


def host_inputs(SEQ, inputs):
    AT = SEQ // 128
    NTOK = AT * 128
    f32 = np.float32
    x = np.asarray(inputs["x"], f32)
    meta = np.ascontiguousarray(np.asarray(inputs["meta_tokens"], f32))
    b_in = np.asarray(inputs["b_in"], f32)[0]
    b_fm = np.zeros((128, 113), f32)
    cols = np.concatenate([b_in[OFF_A:OFF_V], b_in[OFF_GC:N_IN]])
    b_fm[:, :96] = cols.reshape(96, 128).T
    bf2 = np.zeros((128, 113), f32)
    bf2[:, 0:64] = b_fm[:, 0:64]
    bf2[:, 80:112] = b_fm[:, 64:96]
    bf2[:16, 112] = b_in[OFF_F:OFF_F + 16]

    def bc(v):
        return np.ascontiguousarray(np.broadcast_to(np.asarray(v, f32).reshape(1, -1), (128, D)))

    def fm(v):
        return np.ascontiguousarray(np.asarray(v, f32).reshape(KC, 128).T)

    common = {
        "xmeta": meta,
        "lnin_g": bc(inputs["ln_in_g"]), "lnin_b": bc(inputs["ln_in_b"]),
        "w_in": np.ascontiguousarray(np.asarray(inputs["w_in"], f32)[0]),
        "b_fm": bf2, "bv_b": bc(b_in[OFF_V:OFF_F]),
        "dww": np.ascontiguousarray(np.asarray(inputs["conv_dw_w"], f32)[0].T.reshape(KC, 128, TAPS).transpose(1, 0, 2)),
        "dwb": fm(inputs["conv_dw_b"]), "cln_g": fm(inputs["conv_ln_g"]), "cln_b": fm(inputs["conv_ln_b"]),
        "w_co": np.ascontiguousarray(np.asarray(inputs["w_conv_out"], f32)[0]),
        "w_ao": np.ascontiguousarray(np.asarray(inputs["w_attn_out"], f32)[0]),
        "w_o": np.ascontiguousarray(np.asarray(inputs["w_out"], f32)[0]),
        "ln1_g": bc(inputs["ln1_g"]), "ln1_b": bc(inputs["ln1_b"]),
        "w_q": np.ascontiguousarray(np.asarray(inputs["peer_w_q"], f32)[0]),
        "subk": np.ascontiguousarray(np.asarray(inputs["peer_subkeys"], f32)[0].reshape(16, 128, 128)),
        "tab_u": np.ascontiguousarray(np.asarray(inputs["peer_u"], f32)[0]),
        "tab_v": np.ascontiguousarray(np.asarray(inputs["peer_v"], f32)[0]),
        "ln2_g": bc(inputs["ln2_g"]), "ln2_b": bc(inputs["ln2_b"]),
        "ident": np.eye(128, dtype=f32),
        "tri": np.triu(np.ones((128, 128), f32)),
        "iota16": np.ascontiguousarray(np.broadcast_to(np.arange(16, dtype=f32), (128, 16))),
    }
    maps = []
    for core in range(8):
        b, p = core // 2, core % 2
        xa = np.zeros((NTOK, D), f32)
        valid = np.ones((NTOK,), f32)
        kb0 = np.zeros((128, 1), f32)
        hmask = np.ones((128, 32), f32)
        if p == 0:
            xa[128:] = x[b, :NTOK - 128]
            xa[112:128] = meta
            valid[:128] = 0.0
            kb0[:] = NEG
            hmask[:, :16] = 0.0
        else:
            xa[:] = x[b, :NTOK]
        m = dict(common)
        m.update({"xa": xa, "valid16": np.ascontiguousarray(np.broadcast_to(valid, (16, NTOK))),
                  "kb0": kb0, "hmask": hmask})
        maps.append(m)
    return maps


def assemble(SEQ, results):
    AT = SEQ // 128
    NB = AT // 2
    out = np.zeros((4, SEQ, D), np.float32)
    for core in range(8):
        b, p = core // 2, core % 2
        o = results[core]["out"].reshape(NB, 128, D)
        for i in range(NB):
            rt = 2 * i + p
            out[b, rt * 128:(rt + 1) * 128] = o[i]
    return out


_NC_CACHE = {}


def kernel(**inputs):
    SEQ = int(np.asarray(inputs["x"]).shape[1])
    if SEQ not in _NC_CACHE:
        _NC_CACHE[SEQ] = build(SEQ)
    nc = _NC_CACHE[SEQ]
    maps = host_inputs(SEQ, inputs)
    res = run_bass_kernel_spmd(nc, maps, core_ids=list(range(8)))
    return assemble(SEQ, res.results)
```
